# Optimizing a Trainium2 kernel written in Bass

```python
import jax, jax.numpy as jnp
from jax import lax
import numpy as np

D_MODEL = 1024
BATCH = 16
SEQ = 2048
DEPTH = 4

CHUNK = 64
N_MIXERS = 3
HEAD_DIM = 64
N_HEADS = D_MODEL // HEAD_DIM
Q_BLOCK = 128
SG_CHUNK = 128
SG_GROUPS = 8
SG_HALF = 3 * D_MODEL
CA_PREV = 8
CA_BAND = (CA_PREV + 1) * CHUNK
REL_CLIP = 128
D_FF = 2816
N_EXPERTS = 8
TOP_K = 2
D_FF_EXPERT = 3584
EPS = 1e-6

N_SB = (DEPTH + 2) // 3
N_SG = (DEPTH + 1) // 3
N_CA = DEPTH // 3
N_DENSE = (DEPTH + 1) // 2
N_MOE = DEPTH // 2

kernel_name = 'hybrid_chunk_causal_sb_sgu_relattn_moe'


def rms_norm(x, g):
    xf = x.astype(jnp.float32)
    y = xf * lax.rsqrt(jnp.mean(xf * xf, axis=-1, keepdims=True) + EPS)
    return (y * g.astype(jnp.float32)).astype(x.dtype)


def stick_breaking_mixer(h, w_qkv, q_g, k_g, w_o):
    b, s, _ = h.shape
    qkv = (h @ w_qkv).reshape(b, s, 3, N_HEADS, HEAD_DIM)
    q = rms_norm(qkv[:, :, 0], q_g)
    k = rms_norm(qkv[:, :, 1], k_g)
    v = qkv[:, :, 2]
    scale = HEAD_DIM ** -0.5
    outs = []
    for blk in range(s // Q_BLOCK):
        q0 = blk * Q_BLOCK
        kn = q0 + Q_BLOCK
        z = jnp.einsum('bqhd,bkhd->bhqk', q[:, q0:kn], k[:, :kn]).astype(jnp.float32) * scale
        qpos = q0 + jnp.arange(Q_BLOCK)[:, None]
        kpos = jnp.arange(kn)[None, :]
        strict = kpos < qpos
        log_fail = jnp.where(strict, jax.nn.log_sigmoid(-z), 0.0)
        after = lax.cumsum(log_fail, axis=3, reverse=True) - log_fail
        a = jnp.where(strict, jnp.exp(jax.nn.log_sigmoid(z) + after), 0.0)
        outs.append(jnp.einsum('bhqk,bkhd->bqhd', a.astype(v.dtype), v[:, :kn]))
    o = jnp.concatenate(outs, axis=1).reshape(b, s, D_MODEL)
    return o @ w_o


def spatial_gating_mixer(h, w_in, v_g, w_s, b_s, w_out):
    b, s, _ = h.shape
    z = jax.nn.gelu(h @ w_in)
    u = z[..., :SG_HALF]
    v = rms_norm(z[..., SG_HALF:], v_g)
    n = s // SG_CHUNK
    vc = v.reshape(b, n, SG_CHUNK, SG_GROUPS, SG_HALF // SG_GROUPS)
    pos = jnp.arange(SG_CHUNK)
    mask = (pos[None, :] // CHUNK) <= (pos[:, None] // CHUNK)
    ws = jnp.where(mask[None], w_s, 0.0)
    mixed = jnp.einsum('gij,bnjgc->bnigc', ws.astype(vc.dtype), vc) + b_s.T[None, None, :, :, None]
    y = u * mixed.reshape(b, s, SG_HALF)
    return y @ w_out


def chunk_attention_mixer(h, w_qkv, q_g, k_g, rel_bias, w_o):
    b, s, _ = h.shape
    qkv = (h @ w_qkv).reshape(b, s, 3, N_HEADS, HEAD_DIM)
    q = rms_norm(qkv[:, :, 0], q_g) * (HEAD_DIM ** -0.5)
    k = rms_norm(qkv[:, :, 1], k_g)
    v = qkv[:, :, 2]
    pad = CA_PREV * CHUNK
    kp = jnp.pad(k, ((0, 0), (pad, 0), (0, 0), (0, 0)))
    vp = jnp.pad(v, ((0, 0), (pad, 0), (0, 0), (0, 0)))
    qi = jnp.arange(CHUNK)[:, None]
    kj = jnp.arange(CA_BAND)[None, :]
    rel = qi + pad - kj
    idx = jnp.clip(rel, -REL_CLIP, REL_CLIP) + REL_CLIP
    bias = rel_bias[:, idx].astype(jnp.float32)

    def one_chunk(n):
        start = n * CHUNK
        qn = lax.dynamic_slice_in_dim(q, start, CHUNK, axis=1)
        kn = lax.dynamic_slice_in_dim(kp, start, CA_BAND, axis=1)
        vn = lax.dynamic_slice_in_dim(vp, start, CA_BAND, axis=1)
        logits = jnp.einsum('bqhd,bkhd->bhqk', qn, kn).astype(jnp.float32) + bias
        valid = (start - pad + kj) >= 0
        logits = jnp.where(valid, logits, -jnp.inf)
        p = jax.nn.softmax(logits, axis=-1)
        return jnp.einsum('bhqk,bkhd->bqhd', p.astype(vn.dtype), vn)

    o = lax.map(one_chunk, jnp.arange(s // CHUNK))
    o = jnp.moveaxis(o, 0, 1).reshape(b, s, D_MODEL)
    return o @ w_o


def swiglu(h, w13, w2):
    a, g = jnp.split(h @ w13, 2, axis=-1)
    return (jax.nn.silu(a) * g) @ w2


def moe_swiglu(h, w_r, b_r, w1, w3, w2):
    logits = (h @ w_r).astype(jnp.float32) + b_r.astype(jnp.float32)
    top_v, top_i = lax.top_k(logits, TOP_K)
    top_w = jax.nn.softmax(top_v, axis=-1)
    gates = jnp.sum(jax.nn.one_hot(top_i, N_EXPERTS, dtype=jnp.float32) * top_w[..., None], axis=-2)
    out = jnp.zeros_like(h)
    for e in range(N_EXPERTS):
        y = (jax.nn.silu(h @ w1[e]) * (h @ w3[e])) @ w2[e]
        out = out + gates[..., e:e + 1].astype(h.dtype) * y
    return out


def setup_inputs(seed: int = 0) -> dict:
    key = jax.random.key(seed)
    ks = jax.random.split(key, 26)
    nrm = jax.random.normal
    f32 = jnp.float32
    d = D_MODEL
    return {
        'x': nrm(ks[0], (BATCH, SEQ, d), f32),
        'c': nrm(ks[1], (BATCH, d), f32),
        'norm_g': 1.0 + 0.02 * nrm(ks[2], (DEPTH, 2, d), f32),
        'ada_w': nrm(ks[3], (DEPTH, d, 6 * d), f32) * (0.5 * d ** -0.5),
        'ada_b': 0.02 * nrm(ks[4], (DEPTH, 6 * d), f32),
        'sb_wqkv': nrm(ks[5], (N_SB, d, 3 * d), f32) * d ** -0.5,
        'sb_qg': 1.0 + 0.02 * nrm(ks[6], (N_SB, HEAD_DIM), f32),
        'sb_kg': 1.0 + 0.02 * nrm(ks[7], (N_SB, HEAD_DIM), f32),
        'sb_wo': nrm(ks[8], (N_SB, d, d), f32) * d ** -0.5,
        'sg_win': nrm(ks[9], (N_SG, d, 2 * SG_HALF), f32) * d ** -0.5,
        'sg_vg': 1.0 + 0.02 * nrm(ks[10], (N_SG, SG_HALF), f32),
        'sg_ws': nrm(ks[11], (N_SG, SG_GROUPS, SG_CHUNK, SG_CHUNK), f32) * SG_CHUNK ** -0.5,
        'sg_bs': 1.0 + 0.02 * nrm(ks[12], (N_SG, SG_GROUPS, SG_CHUNK), f32),
        'sg_wout': nrm(ks[13], (N_SG, SG_HALF, d), f32) * SG_HALF ** -0.5,
        'ca_wqkv': nrm(ks[14], (N_CA, d, 3 * d), f32) * d ** -0.5,
        'ca_qg': 1.0 + 0.02 * nrm(ks[15], (N_CA, HEAD_DIM), f32),
        'ca_kg': 1.0 + 0.02 * nrm(ks[16], (N_CA, HEAD_DIM), f32),
        'ca_relb': 0.2 * nrm(ks[17], (N_CA, N_HEADS, 2 * REL_CLIP + 1), f32),
        'ca_wo': nrm(ks[18], (N_CA, d, d), f32) * d ** -0.5,
        'ff_w13': nrm(ks[19], (N_DENSE, d, 2 * D_FF), f32) * d ** -0.5,
        'ff_w2': nrm(ks[20], (N_DENSE, D_FF, d), f32) * D_FF ** -0.5,
        'moe_wr': nrm(ks[21], (N_MOE, d, N_EXPERTS), f32) * d ** -0.5,
        'moe_br': 0.01 * nrm(ks[22], (N_MOE, N_EXPERTS), f32),
        'moe_w1': nrm(ks[23], (N_MOE, N_EXPERTS, d, D_FF_EXPERT), f32) * d ** -0.5,
        'moe_w3': nrm(ks[24], (N_MOE, N_EXPERTS, d, D_FF_EXPERT), f32) * d ** -0.5,
        'moe_w2': nrm(ks[25], (N_MOE, N_EXPERTS, D_FF_EXPERT, d), f32) * D_FF_EXPERT ** -0.5,
    }


def reference(x, c, norm_g, ada_w, ada_b, sb_wqkv, sb_qg, sb_kg, sb_wo, sg_win, sg_vg, sg_ws, sg_bs,
              sg_wout, ca_wqkv, ca_qg, ca_kg, ca_relb, ca_wo, ff_w13, ff_w2, moe_wr, moe_br,
              moe_w1, moe_w3, moe_w2):
    cond = jax.nn.silu(c)
    for i in range(DEPTH):
        mod = (cond @ ada_w[i] + ada_b[i])[:, None, :]
        sh1, sc1, g1, sh2, sc2, g2 = jnp.split(mod, 6, axis=-1)
        h = rms_norm(x, norm_g[i, 0]) * (1 + sc1) + sh1
        kind = i % N_MIXERS
        j = i // N_MIXERS
        if kind == 0:
            y = stick_breaking_mixer(h, sb_wqkv[j], sb_qg[j], sb_kg[j], sb_wo[j])
        elif kind == 1:
            y = spatial_gating_mixer(h, sg_win[j], sg_vg[j], sg_ws[j], sg_bs[j], sg_wout[j])
        else:
            y = chunk_attention_mixer(h, ca_wqkv[j], ca_qg[j], ca_kg[j], ca_relb[j], ca_wo[j])
        x = x + g1 * y
        h = rms_norm(x, norm_g[i, 1]) * (1 + sc2) + sh2
        m = i // 2
        if i % 2 == 0:
            y = swiglu(h, ff_w13[m], ff_w2[m])
        else:
            y = moe_swiglu(h, moe_wr[m], moe_br[m], moe_w1[m], moe_w3[m], moe_w2[m])
        x = x + g2 * y
    return x
```

```python
import numpy as np
import concourse.bass as bass
import concourse.mybir as mybir
from concourse.bass_utils import run_bass_kernel_spmd
from contextlib import ExitStack

F32 = mybir.dt.float32
BF16 = mybir.dt.bfloat16
AF = mybir.ActivationFunctionType
ALU = mybir.AluOpType
AX = mybir.AxisListType

ENGS = ("pe", "act", "dve", "pool", "sp")
SAME_ENGINE_SYNC = True


class Trk:
    __slots__ = ("w", "r", "name")

    def __init__(self, name=""):
        self.w = None
        self.r = []
        self.name = name


class T:
    def __init__(self, h, name):
        self.h = h
        self.trk = Trk(name)

    def __getitem__(self, k):
        return self.h[k]


class Op:
    __slots__ = ("eng", "fn", "waits", "kind", "key", "val", "needed", "idx")


class Prog:
    def __init__(self, nc, stack):
        self.nc = nc
        self.stack = stack
        self.sems = {e: stack.enter_context(nc.semaphore("s_" + e)) for e in ENGS}
        self.dma_sems = {}
        self.ops = {e: [] for e in ENGS}
        self.nops = {e: 0 for e in ENGS}
        self.allops = {e: [] for e in ENGS}
        self.seen = {e: {} for e in ENGS}
        self.inc_count = {e: 0 for e in ENGS}
        self.uid = 0
        self.last_c = {}

    def sbuf(self, st, name, shape, dtype):
        self.uid += 1
        h = st.enter_context(self.nc.sbuf_tensor(f"{name}_{self.uid}", list(shape), dtype))
        return T(h, name)

    def psum(self, st, name, shape, dtype=F32):
        self.uid += 1
        h = st.enter_context(self.nc.psum_tensor(f"{name}_{self.uid}", list(shape), dtype))
        return T(h, name)

    def _deps(self, reads, writes):
        evs = []
        for t in reads:
            trk = t.trk if isinstance(t, T) else t
            if trk.w is not None:
                evs.append(trk.w)
        for t in writes:
            trk = t.trk if isinstance(t, T) else t
            if trk.w is not None:
                evs.append(trk.w)
            evs.extend(trk.r)
        return evs

    def _mkwaits(self, eng, evs, pe_accum=False, force_self=False):
        waits = []
        seen = self.seen[eng]
        for ev in evs:
            kind, src, v = ev[0], ev[1], ev[2]
            if kind == "eng":
                if src == eng and not force_self and (not SAME_ENGINE_SYNC or pe_accum or eng == "pe"):
                    continue
                if seen.get(("e", src), -1) >= v:
                    continue
                seen[("e", src)] = v
                waits.append(ev)
                self.allops[src][v].needed = True
            else:
                if seen.get(("d", src), -1) >= v:
                    continue
                seen[("d", src)] = v
                waits.append(ev)
        return waits

    def _update(self, ev, reads, writes):
        for t in writes:
            trk = t.trk if isinstance(t, T) else t
            trk.w = ev
            trk.r = []
        for t in reads:
            trk = t.trk if isinstance(t, T) else t
            if trk in [(x.trk if isinstance(x, T) else x) for x in writes]:
                continue
            trk.r.append(ev)
            if len(trk.r) > 64:
                last = {}
                for e in trk.r:
                    k = (e[0], e[1])
                    if k not in last or last[k][2] < e[2]:
                        last[k] = e
                trk.r = list(last.values())

    def op(self, eng, fn, reads=(), writes=(), accum=False):
        o = Op()
        o.eng = eng
        o.fn = fn
        o.kind = "c"
        o.needed = False
        o.idx = self.nops[eng]
        o.waits = self._mkwaits(eng, self._deps(reads, writes), pe_accum=accum)
        self.nops[eng] += 1
        self.ops[eng].append(o)
        self.allops[eng].append(o)
        ev = ("eng", eng, o.idx)
        self.last_c[eng] = o.idx
        self._update(ev, reads, writes)
        return o

    def dma(self, q, out_ap, in_ap, reads=(), writes=(), key=None, **kw):
        if key is None:
            w0 = writes[0]
            key = (w0.trk if isinstance(w0, T) else w0).name
        if key not in self.dma_sems:
            self.dma_sems[key] = [self.stack.enter_context(self.nc.semaphore("d_" + key)), 0]
        ent = self.dma_sems[key]
        ent[1] += 16
        val = ent[1]
        o = Op()
        o.eng = q
        o.kind = "d"
        o.key = key
        o.val = val
        o.needed = False
        o.idx = self.nops[q]
        sem = ent[0]
        o.fn = lambda e: e.dma_start(out=out_ap, in_=in_ap, **kw).then_inc(sem, 16)
        evs = self._deps(reads, writes)
        if val > 16:
            evs.append(("dma", key, val - 16))
        o.waits = self._mkwaits(q, evs)
        self.nops[q] += 1
        self.ops[q].append(o)
        self.allops[q].append(o)
        ev = ("dma", key, val)
        self._update(ev, reads, writes)
        return o

    def _emit_engine(self, eng, e, ops, incvals):
        sem_self = self.sems[eng]
        for o in ops:
            for ev in o.waits:
                if ev[0] == "eng":
                    e.wait_ge(self.sems[ev[1]], incvals[ev[1]][ev[2]])
                else:
                    e.wait_ge(self.dma_sems[ev[1]][0], ev[2])
            if o.fn is None:
                continue
            ins = o.fn(e)
            if o.kind == "c" and o.needed:
                ins.then_inc(sem_self, 1)

    def flush(self):
        incvals = getattr(self, "_incvals", None)
        if incvals is None:
            incvals = {e: [] for e in ENGS}
            self._incvals = incvals
            self._inc_run = {e: 0 for e in ENGS}
        for eng in ENGS:
            lst = self.allops[eng]
            iv = incvals[eng]
            run = self._inc_run[eng]
            for i in range(len(iv), len(lst)):
                o = lst[i]
                if o.kind == "c" and o.needed:
                    run += 1
                iv.append(run)
            self._inc_run[eng] = run
        ops = self.ops
        self.ops = {e: [] for e in ENGS}
        prog = self
        with self.nc.Block() as block:
            def mk(eng):
                def body(e):
                    prog._emit_engine(eng, e, ops[eng], incvals)
                return body

            block.tensor(mk("pe"))
            block.scalar(mk("act"))
            block.vector(mk("dve"))
            block.gpsimd(mk("pool"))
            block.sync(mk("sp"))

    def barrier_ops(self):
        last = {}
        for eng in ENGS:
            lc = self.last_c.get(eng)
            if lc is not None:
                last[eng] = ("eng", eng, lc)
        dmas = [("dma", k, v[1]) for k, v in self.dma_sems.items() if v[1] > 0]
        for eng in ENGS:
            evs = [ev for s_, ev in last.items()] + dmas
            waits = self._mkwaits(eng, evs, force_self=True)
            if not waits:
                continue
            o = Op()
            o.eng = eng
            o.kind = "b"
            o.needed = False
            o.idx = self.nops[eng]
            o.waits = waits
            o.fn = None
            self.nops[eng] += 1
            self.ops[eng].append(o)
            self.allops[eng].append(o)


D = 1024
SEQ = 2048
NSEQ = 2
TPS = 16
DFF = 2816
DFE = 3584
NE = 8
EPS = 1e-6


class K:
    pass


def mm(P, out_ap, lhsT, rhs, start, stop, reads, writes, **kw):
    P.op("pe", lambda e: e.matmul(out_ap, lhsT=lhsT, rhs=rhs, start=start, stop=stop, **kw),
         reads=reads, writes=writes)


def alloc_common(P, st, k, hb2=False):
    c = K()
    c.ident = P.sbuf(st, "ident", [128, 128], BF16)
    P.op("pool", lambda e: e.memset(c.ident[:], 1.0), writes=[c.ident])
    P.op("pool", lambda e: e.affine_select(out=c.ident[:], in_=c.ident[:], pattern=[[-1, 128]],
                                           compare_op=ALU.is_equal, fill=0.0, base=0, channel_multiplier=1),
         reads=[c.ident], writes=[c.ident])
    c.xt = [P.sbuf(st, f"xt{j}", [128, D], F32) for j in range(2)]
    c.junk = P.sbuf(st, "junk", [128, D], BF16)
    c.t1 = P.sbuf(st, "t1", [128, D], F32)
    c.hb = P.sbuf(st, "hb", [128, D], BF16)
    c.hbs = [c.hb] + ([P.sbuf(st, "hb2", [128, D], BF16)] if hb2 else [])
    c.hbcnt = 0
    c.ssq = P.sbuf(st, "ssq", [128, 2], F32)
    c.rstd = P.sbuf(st, "rstd", [128, 2], F32)
    c.A = P.sbuf(st, "modA", [128, D], F32)
    c.sh = P.sbuf(st, "modsh", [128, D], F32)
    c.g = P.sbuf(st, "modg", [128, D], F32)
    c.psT = P.psum(st, "psT", [128, D], BF16)
    k.epsb = P.sbuf(st, "epsb", [128, 1], F32)
    P.op("pool", lambda e: e.memset(k.epsb[:], EPS), writes=[k.epsb])
    c.cnt = 0
    return c


def load_mod(P, k, c, layer, sub, b):
    o = 3 * sub * D
    P.dma("sp", c.sh[:], k.modd[layer, b:b + 1, o:o + D].partition_broadcast(128), reads=[k.modtrk], writes=[c.sh])
    P.dma("sp", c.t1[:], k.modd[layer, b:b + 1, o + D:o + 2 * D].partition_broadcast(128), reads=[k.modtrk], writes=[c.t1])
    P.dma("sp", c.g[:], k.modd[layer, b:b + 1, o + 2 * D:o + 3 * D].partition_broadcast(128), reads=[k.modtrk], writes=[c.g])
    P.dma("sp", c.A[:], k.norm_g[layer, sub:sub + 1, :].partition_broadcast(128), writes=[c.A])
    P.op("dve", lambda e: e.tensor_tensor(out=c.A[:], in0=c.A[:], in1=c.t1[:], op=ALU.mult),
         reads=[c.A, c.t1], writes=[c.A])


def norm_mod(P, k, c, src_ap, src_trk, out_tile, out_ap):
    xt = c.xt[c.cnt % 2]
    col = c.cnt % 2
    c.cnt += 1
    P.dma("sp", xt[:], src_ap, reads=[src_trk], writes=[xt])
    P.op("act", lambda e: e.activation(out=c.junk[:], in_=xt[:], func=AF.Square, accum_out=c.ssq[:, col:col + 1]),
         reads=[xt], writes=[c.junk, c.ssq])
    P.op("act", lambda e: e.activation(out=c.rstd[:, col:col + 1], in_=c.ssq[:, col:col + 1], func=AF.Sqrt, scale=1.0 / D, bias=k.epsb[:, 0:1]),
         reads=[c.ssq, k.epsb], writes=[c.rstd])
    P.op("dve", lambda e: e.reciprocal(out=c.rstd[:, col:col + 1], in_=c.rstd[:, col:col + 1]), reads=[c.rstd], writes=[c.rstd])
    P.op("dve", lambda e: e.scalar_tensor_tensor(out=c.t1[:], in0=xt[:], scalar=c.rstd[:, col:col + 1], in1=c.A[:],
                                                 op0=ALU.mult, op1=ALU.mult), reads=[xt, c.rstd, c.A], writes=[c.t1])
    P.op("dve", lambda e: e.tensor_tensor(out=out_ap, in0=c.t1[:], in1=c.sh[:], op=ALU.add),
         reads=[c.t1, c.sh], writes=[out_tile])
    return xt


def prologue_tile(P, k, c, src_ap, src_trk, hT, col0, pend=None):
    hb = c.hbs[c.hbcnt % len(c.hbs)]
    c.hbcnt += 1
    norm_mod(P, k, c, src_ap, src_trk, hb, hb[:])

    def stageB():
        for kk in range(8):
            P.op("pe", lambda e, kk=kk: e.transpose(out=c.psT[:, kk * 128:(kk + 1) * 128], in_=hb[:, kk * 128:(kk + 1) * 128],
                                                    identity=c.ident[:]), reads=[hb, c.ident], writes=[c.psT])
        P.op("act", lambda e: e.copy(out=hT[:, :, col0:col0 + 128], in_=c.psT[:].rearrange("p (k t) -> p k t", k=8)),
             reads=[c.psT], writes=[hT])

    if pend is None or len(c.hbs) < 2:
        stageB()
    else:
        if pend:
            pend.pop(0)()
        pend.append(stageB)


def flush_pend(pend):
    while pend:
        pend.pop(0)()


def epilogue_half(P, k, c, xt, y_ap, y_trk, half):
    hs = slice(half * 512, (half + 1) * 512)
    P.op("dve", lambda e: e.tensor_tensor(out=c.t1[:, hs], in0=y_ap, in1=c.g[:, hs], op=ALU.mult),
         reads=[y_trk, c.g], writes=[c.t1])
    P.op("pool", lambda e: e.tensor_tensor(out=xt[:, hs], in0=c.t1[:, hs], in1=xt[:, hs], op=ALU.add),
         reads=[c.t1, xt], writes=[xt])


def xsrc(k, first, gt):
    if first:
        return k.x_in[gt * 128:(gt + 1) * 128, :], k.xin_trk
    return k.y[gt * 128:(gt + 1) * 128, :], k.ytrk[gt]


def store_x(P, k, xt, gt):
    P.dma("sp", k.y[gt * 128:(gt + 1) * 128, :], xt[:], reads=[xt], writes=[k.ytrk[gt]], key=f"yst{gt % 4}")


def phase_mod(P, k):
    with ExitStack() as st:
        cT = P.sbuf(st, "cT", [128, 8, 2], F32)
        condT = P.sbuf(st, "condT", [128, 8, 2], F32)
        wbuf = [P.sbuf(st, f"adaw{j}", [128, 8, 512], F32) for j in range(2)]
        bias2 = P.sbuf(st, "bias2", [2, 6 * D], F32)
        modsb = P.sbuf(st, "modsb", [2, 6 * D], F32)
        ps = [P.psum(st, f"psm{j}", [128, 512], F32) for j in range(2)]
        for b in range(2):
            P.dma("sp", cT[:, :, b], k.c[b].rearrange("(k p) -> p k", p=128), writes=[cT], allow_slow_non_contiguous=True)
        P.op("act", lambda e: e.activation(out=condT[:], in_=cT[:], func=AF.Silu), reads=[cT], writes=[condT])
        it = 0
        for i in range(4):
            P.dma("sp", bias2[:], k.ada_b[i:i + 1, :].partition_broadcast(2), writes=[bias2])
            wv = k.ada_w[i].rearrange("(k p) n -> p k n", p=128)
            for n in range(12):
                wb = wbuf[it % 2]
                pst = ps[it % 2]
                it += 1
                P.dma("sp", wb[:], wv[:, :, n * 512:(n + 1) * 512], writes=[wb])
                for kk in range(8):
                    mm(P, pst[0:2, :], condT[:, kk, :], wb[:, kk, :], kk == 0, kk == 7, [condT, wb], [pst])
                P.op("dve", lambda e, n=n, pst=pst: e.tensor_tensor(out=modsb[:, n * 512:(n + 1) * 512], in0=pst[0:2, :],
                                                                   in1=bias2[:, n * 512:(n + 1) * 512], op=ALU.add),
                     reads=[pst, bias2], writes=[modsb])
            for o in (D, 4 * D):
                P.op("dve", lambda e, o=o: e.tensor_scalar_add(out=modsb[:, o:o + D], in0=modsb[:, o:o + D], scalar1=1.0),
                     reads=[modsb], writes=[modsb])
            P.dma("sp", k.modd[i], modsb[:], reads=[modsb], writes=[k.modtrk], key="modst")
        P.barrier_ops()
        P.flush()


def ffn_body(P, k, c, b, hT, acc, actb, w13b, w2b, psA, psO, wa_ap, wg_ap, w2_ap, F, G, first_acc, gate_ap, st_cnt):
    nch = F // 128
    wav = wa_ap.rearrange("(k p) f -> p k f", p=128)
    wgv = wg_ap.rearrange("(k p) f -> p k f", p=128)
    w2v = w2_ap.rearrange("(c p) n -> p c n", p=128)
    c0 = 0
    while c0 < nch:
        g = min(G, nch - c0)
        w13 = w13b[st_cnt[0] % 2]
        w2 = w2b[st_cnt[0] % 2]
        st_cnt[0] += 1
        P.dma("pool", w13[:, :, 0, 0:g * 128], wav[:, :, c0 * 128:(c0 + g) * 128], writes=[w13])
        P.dma("pool", w13[:, :, 1, 0:g * 128], wgv[:, :, c0 * 128:(c0 + g) * 128], writes=[w13])
        P.dma("pool", w2[:, 0:g, :], w2v[:, c0:c0 + g, :], writes=[w2])
        for cc in range(g):
            for tt in range(4):
                pa = psA[st_cnt[1] % 4]
                pg = psA[(st_cnt[1] + 1) % 4]
                st_cnt[1] += 2
                for kk in range(8):
                    mm(P, pa[:], w13[:, kk, 0, cc * 128:(cc + 1) * 128], hT[:, kk, tt * 512:(tt + 1) * 512], kk == 0, kk == 7, [w13, hT], [pa])
                for kk in range(8):
                    mm(P, pg[:], w13[:, kk, 1, cc * 128:(cc + 1) * 128], hT[:, kk, tt * 512:(tt + 1) * 512], kk == 0, kk == 7, [w13, hT], [pg])
                sa = k.sa[st_cnt[1] // 2 % 2]
                P.op("act", lambda e, sa=sa, pa=pa: e.activation(out=sa[:], in_=pa[:], func=AF.Silu), reads=[pa], writes=[sa])
                P.op("dve", lambda e, sa=sa, pg=pg, cc=cc, tt=tt: e.tensor_tensor(out=actb[:, cc, tt * 512:(tt + 1) * 512], in0=pg[:], in1=sa[:],
                                                                               op=ALU.mult), reads=[pg, sa], writes=[k.act_trk[cc][tt]])
        for t in range(TPS):
            for half in range(2):
                po = psO[st_cnt[2] % 3]
                st_cnt[2] += 1
                for cc in range(g):
                    mm(P, po[:], actb[:, cc, t * 128:(t + 1) * 128], w2[:, cc, half * 512:(half + 1) * 512], cc == 0, cc == g - 1, [k.act_trk[cc][t // 4], w2], [po])
                dst = acc[:, t, half * 512:(half + 1) * 512]
                if first_acc and c0 == 0:
                    if gate_ap is None:
                        P.op("dve", lambda e, dst=dst, po=po: e.tensor_copy(out=dst, in_=po[:]), reads=[po], writes=[k.acc_trk[t][half]])
                    else:
                        P.op("dve", lambda e, dst=dst, po=po, t=t: e.tensor_scalar(out=dst, in0=po[:], scalar1=gate_ap(t), scalar2=None, op0=ALU.mult),
                             reads=[po, k.gates_sb], writes=[k.acc_trk[t][half]])
                else:
                    if gate_ap is None:
                        P.op("dve", lambda e, dst=dst, po=po: e.tensor_tensor(out=dst, in0=po[:], in1=dst, op=ALU.add), reads=[po, k.acc_trk[t][half]], writes=[k.acc_trk[t][half]])
                    else:
                        P.op("dve", lambda e, dst=dst, po=po, t=t: e.scalar_tensor_tensor(out=dst, in0=po[:], scalar=gate_ap(t), in1=dst,
                                                                                      op0=ALU.mult, op1=ALU.add),
                             reads=[po, k.acc_trk[t][half], k.gates_sb], writes=[k.acc_trk[t][half]])
        c0 += g


def phase_ffn(P, k, layer, moe):
    first = False
    m = layer // 2
    G = 4
    with ExitStack() as st:
        c = alloc_common(P, st, k, hb2=True)
        hT = P.sbuf(st, "hT", [128, 8, SEQ], BF16)
        acc = P.sbuf(st, "acc", [128, TPS, D], F32)
        k.acc_trk = [[Trk(f"acc{t}_{h}") for h in range(2)] for t in range(TPS)]
        k.act_trk = [[Trk(f"act{cc}_{tt}") for tt in range(4)] for cc in range(G)]
        actb = P.sbuf(st, "actb", [128, G, SEQ], BF16)
        w13b = [P.sbuf(st, f"w13b{j}", [128, 8, 2, G * 128], BF16) for j in range(2)]
        w2b = [P.sbuf(st, f"w2b{j}", [128, G, D], BF16) for j in range(2)]
        k.sa = [P.sbuf(st, f"sa{j}", [128, 512], F32) for j in range(2)]
        psA = [P.psum(st, f"psA{j}", [128, 512], F32) for j in range(4)]
        psO = [P.psum(st, f"psO{j}", [128, 512], F32) for j in range(3)]
        k.gates_sb = P.sbuf(st, "gates_sb", [128, TPS, NE], F32)
        st_cnt = [0, 0, 0]
        for b in range(NSEQ):
            load_mod(P, k, c, layer, 1, b)
            pend = []
            for t in range(TPS):
                gt = b * TPS + t
                ap, trk = xsrc(k, first, gt)
                prologue_tile(P, k, c, ap, trk, hT, t * 128, pend)
            flush_pend(pend)
            if not moe:
                ffn_body(P, k, c, b, hT, acc, actb, w13b, w2b, psA, psO,
                         k.ff_w13[m][:, 0:DFF], k.ff_w13[m][:, DFF:2 * DFF], k.ff_w2[m], DFF, G, True, None, st_cnt)
            else:
                P.dma("sp", k.gates_sb[:], k.gates_d[b * SEQ:(b + 1) * SEQ, :].rearrange("(t p) e -> p t e", p=128),
                      reads=[k.gates_trk], writes=[k.gates_sb])
                for ex in range(NE):
                    ffn_body(P, k, c, b, hT, acc, actb, w13b, w2b, psA, psO,
                             k.moe_w1[m, ex], k.moe_w3[m, ex], k.moe_w2[m, ex], DFE, G, ex == 0,
                             (lambda t, ex=ex: k.gates_sb[:, t, ex:ex + 1]), st_cnt)
            for t in range(TPS):
                gt = b * TPS + t
                ap, trk = xsrc(k, first, gt)
                xt = c.xt[c.cnt % 2]
                c.cnt += 1
                P.dma("sp", xt[:], ap, reads=[trk], writes=[xt])
                for half in range(2):
                    epilogue_half(P, k, c, xt, acc[:, t, half * 512:(half + 1) * 512], k.acc_trk[t][half], half)
                store_x(P, k, xt, gt)
        P.barrier_ops()
        P.flush()


def phase_router(P, k, layer):
    m = layer // 2
    with ExitStack() as st:
        c = alloc_common(P, st, k)
        wr = P.sbuf(st, "wr", [128, NE, D], F32)
        br = P.sbuf(st, "br", [128, TPS, NE], F32)
        hfs = [P.sbuf(st, f"hf{i}", [128, D], F32) for i in range(2)]
        jk = P.sbuf(st, "jk", [128, D], F32)
        lg = P.sbuf(st, "lg", [128, TPS, NE], F32)
        v1 = P.sbuf(st, "v1", [128, TPS], F32)
        v2 = P.sbuf(st, "v2", [128, TPS], F32)
        ee = P.sbuf(st, "ee", [128, TPS], F32)
        w1 = P.sbuf(st, "w1", [128, TPS], F32)
        w2 = P.sbuf(st, "w2", [128, TPS], F32)
        m1 = P.sbuf(st, "m1", [128, TPS, NE], F32)
        m2 = P.sbuf(st, "m2", [128, TPS, NE], F32)
        l2 = P.sbuf(st, "l2", [128, TPS, NE], F32)
        gt_sb = P.sbuf(st, "gt_sb", [128, TPS, NE], F32)
        for ex in range(NE):
            P.dma("sp", wr[:, ex, :], k.moe_wr[m, ex:ex + 1, :].partition_broadcast(128), writes=[wr])
        for t in range(TPS):
            P.dma("sp", br[:, t, :], k.moe_br[m:m + 1, :].partition_broadcast(128), writes=[br])
        bc = lambda ap: ap.unsqueeze(2).to_broadcast([128, TPS, NE])
        for b in range(NSEQ):
            load_mod(P, k, c, layer, 1, b)
            for t in range(TPS):
                gt = b * TPS + t
                ap, trk = xsrc(k, False, gt)
                hf = hfs[t % 2]
                norm_mod(P, k, c, ap, trk, hf, hf[:])
                for ex in range(NE):
                    P.op("dve", lambda e, ex=ex, hf=hf, t=t: e.scalar_tensor_tensor(out=jk[:], in0=hf[:], scalar=1.0, in1=wr[:, ex, :], op0=ALU.mult, op1=ALU.mult,
                                                                                 accum_out=lg[:, t, ex:ex + 1]), reads=[hf, wr], writes=[jk, lg])
            P.op("dve", lambda e: e.tensor_tensor(out=lg[:], in0=lg[:], in1=br[:], op=ALU.add), reads=[lg, br], writes=[lg])
            P.op("dve", lambda e: e.tensor_reduce(out=v1[:], in_=lg[:], axis=AX.X, op=ALU.max), reads=[lg], writes=[v1])
            P.op("dve", lambda e: e.tensor_tensor(out=m1[:], in0=lg[:], in1=bc(v1[:, :]), op=ALU.is_equal), reads=[lg, v1], writes=[m1])
            P.op("dve", lambda e: e.scalar_tensor_tensor(out=l2[:], in0=m1[:], scalar=-1e30, in1=lg[:], op0=ALU.mult, op1=ALU.add),
                 reads=[m1, lg], writes=[l2])
            P.op("dve", lambda e: e.tensor_reduce(out=v2[:], in_=l2[:], axis=AX.X, op=ALU.max), reads=[l2], writes=[v2])
            P.op("dve", lambda e: e.tensor_tensor(out=m2[:], in0=l2[:], in1=bc(v2[:, :]), op=ALU.is_equal), reads=[l2, v2], writes=[m2])
            P.op("dve", lambda e: e.tensor_tensor(out=ee[:], in0=v2[:], in1=v1[:], op=ALU.subtract), reads=[v1, v2], writes=[ee])
            P.op("act", lambda e: e.activation(out=ee[:], in_=ee[:], func=AF.Exp), reads=[ee], writes=[ee])
            P.op("dve", lambda e: e.tensor_scalar_add(out=w1[:], in0=ee[:], scalar1=1.0), reads=[ee], writes=[w1])
            P.op("dve", lambda e: e.reciprocal(out=w1[:], in_=w1[:]), reads=[w1], writes=[w1])
            P.op("dve", lambda e: e.tensor_tensor(out=w2[:], in0=ee[:], in1=w1[:], op=ALU.mult), reads=[ee, w1], writes=[w2])
            P.op("dve", lambda e: e.tensor_tensor(out=m1[:], in0=m1[:], in1=bc(w1[:, :]), op=ALU.mult), reads=[m1, w1], writes=[m1])
            P.op("dve", lambda e: e.tensor_tensor(out=m2[:], in0=m2[:], in1=bc(w2[:, :]), op=ALU.mult), reads=[m2, w2], writes=[m2])
            P.op("dve", lambda e: e.tensor_tensor(out=gt_sb[:], in0=m1[:], in1=m2[:], op=ALU.add), reads=[m1, m2], writes=[gt_sb])
            P.dma("sp", k.gates_d[b * SEQ:(b + 1) * SEQ, :].rearrange("(t p) e -> p t e", p=128), gt_sb[:], reads=[gt_sb], writes=[k.gates_trk], key="gst")
        P.barrier_ops()
        P.flush()


def phase_attn(P, k, layer, kind):
    first = (layer == 0)
    sbk = (kind == "sb")
    j = layer // 3
    if sbk:
        wqkv, qg, kg, wo = k.sb_wqkv[j], k.sb_qg, k.sb_kg, k.sb_wo[j]
    else:
        wqkv, qg, kg, wo = k.ca_wqkv[0], k.ca_qg, k.ca_kg, k.ca_wo[0]
    wqv = wqkv.rearrange("(k p) n -> p k n", p=128)
    wov = wo.rearrange("(k p) n -> p k n", p=128)
    with ExitStack() as st:
        c = alloc_common(P, st, k)
        hT = P.sbuf(st, "hT", [128, 8, SEQ], BF16)
        oT = hT
        qT = P.sbuf(st, "qT", [128, 8, SEQ], BF16)
        kT = P.sbuf(st, "kT", [128, 8, SEQ], BF16)
        v_sb = P.sbuf(st, "v_sb", [128, TPS, D], BF16)
        wb = [P.sbuf(st, f"wb{i}", [128, 8, 512], BF16) for i in range(2)]
        gq = P.sbuf(st, "gq", [128, 512], F32)
        gk = P.sbuf(st, "gk", [128, 512], F32)
        sq_ = [P.sbuf(st, f"sq{i}", [128, 512], F32) for i in range(2)]
        tmp_ = [P.sbuf(st, f"tmp{i}", [128, 512], F32) for i in range(2)]
        qn_ = [P.sbuf(st, f"qn{i}", [128, 512], BF16) for i in range(2)]
        ssqh_ = [P.sbuf(st, f"ssqh{i}", [128, 8], F32) for i in range(2)]
        rsth_ = [P.sbuf(st, f"rsth{i}", [128, 8], F32) for i in range(2)]
        psP = [P.psum(st, f"psP{i}", [128, 512], F32) for i in range(2)]
        P.dma("sp", gq[:].rearrange("p (h d) -> p h d", h=8), bass.AP(qg, j * 64, [[0, 128], [0, 8], [1, 64]]), writes=[gq])
        P.dma("sp", gk[:].rearrange("p (h d) -> p h d", h=8), bass.AP(kg, j * 64, [[0, 128], [0, 8], [1, 64]]), writes=[gk])
        P.op("dve", lambda e: e.tensor_scalar_mul(out=gq[:], in0=gq[:], scalar1=0.125), reads=[gq], writes=[gq])
        if sbk:
            esb = [P.sbuf(st, f"esb{i}", [128, 512], F32) for i in range(2)]
            spb = [P.sbuf(st, f"spb{i}", [128, 512], BF16) for i in range(2)]
            Lbb = [P.sbuf(st, f"Lb{i}", [128, 512], BF16) for i in range(2)]
            efb = [P.sbuf(st, f"efb{i}", [128, 512], F32) for i in range(2)]
            Ab = [P.sbuf(st, f"Ab{i}", [128, 512], BF16) for i in range(3)]
            masks = P.sbuf(st, "masks", [128, 4, 512], BF16)
            negtri = P.sbuf(st, "negtri", [128, 128], BF16)
            negones = P.sbuf(st, "negones", [128, 128], BF16)
            zer = P.sbuf(st, "zer", [128, 64], BF16)
            psZ = [P.psum(st, f"psZ{i}", [128, 512], F32) for i in range(4)]
            psOT = P.psum(st, "psOT", [128, 512], F32)
            P.op("pool", lambda e: e.memset(masks[:], 1.0), writes=[masks])
            for d in range(4):
                P.op("pool", lambda e, d=d: e.affine_select(out=masks[:, d, :], in_=masks[:, d, :], pattern=[[1, 512]],
                                                           compare_op=ALU.is_gt, fill=0.0, base=-128 * d, channel_multiplier=-1),
                     reads=[masks], writes=[masks])
            P.op("pool", lambda e: e.memset(negtri[:], -1.0), writes=[negtri])
            P.op("pool", lambda e: e.affine_select(out=negtri[:], in_=negtri[:], pattern=[[-1, 128]], compare_op=ALU.is_ge, fill=0.0,
                                                   base=0, channel_multiplier=1), reads=[negtri], writes=[negtri])
            P.op("pool", lambda e: e.memset(negones[:], -1.0), writes=[negones])
            P.op("pool", lambda e: e.memset(zer[:], 0.0), writes=[zer])
        else:
            BM = P.sbuf(st, "BM", [128, 16, 2, 128], BF16)
            mask0 = P.sbuf(st, "mask0", [128, 128], F32)
            c256 = P.sbuf(st, "c256", [128, 16], F32)
            dmy = BM
            Esb = P.sbuf(st, "Esb", [16, 384], F32)
            ssb = [P.sbuf(st, f"ssb{i}", [128, 128], F32) for i in range(2)]
            Ab = [P.sbuf(st, f"Ab{i}", [128, 128], BF16) for i in range(4)]
            vaug = [P.sbuf(st, f"vaug{a}", [128, TPS, 128], BF16) for a in range(2)]
            rec = [P.sbuf(st, f"rec{i}", [128, 128], F32) for i in range(2)]
            psZ = [P.psum(st, f"psZ{i}", [128, 128], F32) for i in range(3)]
            psOT = [P.psum(st, f"psOT{i}", [128, 128], F32) for i in range(2)]
            P.dma("sp", Esb[:, 0:256], k.ca_relb[0, :, 1:257], writes=[Esb])
            P.op("dve", lambda e: e.tensor_copy(out=Esb[:, 256:384], in_=Esb[:, 255:256].to_broadcast([16, 128])), reads=[Esb], writes=[Esb])
            P.dma("sp", k.Fd[:, :, :], Esb[:, 0:383].unsqueeze(1).to_broadcast([16, 128, 383]), reads=[Esb], writes=[k.Ftrk], key="Fst")
            for jj in (3, 4):
                off = (4 - jj) * 128 + 127
                P.dma("pool", BM[:, :, jj - 3, :], bass.AP(k.Fd, off, [[382, 128], [128 * 383, 16], [1, 128]]), reads=[k.Ftrk], writes=[BM])
            P.dma("sp", c256[:], k.ca_relb[0:1, :, 256:257].rearrange("o h x -> o (h x)").partition_broadcast(128), writes=[c256],
                  allow_slow_non_contiguous=True)
            P.op("dve", lambda e: e.memset(BM[64:128, :, 1, 0:64], -30000.0), reads=[BM], writes=[BM])
            P.op("pool", lambda e: e.memset(mask0[:], 1.0), writes=[mask0])
            P.op("pool", lambda e: e.memset(mask0[0:64, 64:128], 0.0), reads=[mask0], writes=[mask0])
            for a_ in range(2):
                P.op("pool", lambda e, a_=a_: e.memset(vaug[a_][:], 1.0), writes=[vaug[a_]])

        cnt = [0, 0, 0, 0]
        k.psT_trk = [Trk("psTa"), Trk("psTb")]
        for b in range(NSEQ):
            load_mod(P, k, c, layer, 0, b)
            P.op("act", lambda e: e.copy(out=c.junk[:, 0:8], in_=c.junk[:, 8:16]), reads=[k.psT_trk[0], k.psT_trk[1]], writes=[c.psT, c.junk])
            for t in range(TPS):
                gt = b * TPS + t
                ap, trk = xsrc(k, first, gt)
                prologue_tile(P, k, c, ap, trk, hT, t * 128)
            P.op("act", lambda e: e.copy(out=c.junk[:, 0:8], in_=c.psT[:, 0:8]), reads=[c.psT], writes=[c.junk, k.psT_trk[0], k.psT_trk[1]])
            pendB = []
            for n in range(6):
                w = wb[cnt[0] % 2]
                cnt[0] += 1
                P.dma("pool", w[:], wqv[:, :, n * 512:(n + 1) * 512], writes=[w])
                for t in range(TPS):
                    ps = psP[cnt[1] % 2]
                    cnt[1] += 1
                    for kk in range(8):
                        mm(P, ps[:], hT[:, kk, t * 128:(t + 1) * 128], w[:, kk, :], kk == 0, kk == 7, [hT, w], [ps])
                    if n < 4:
                        gain = gq if n < 2 else gk
                        dstT = qT if n < 2 else kT
                        bi = cnt[1] % 2
                        sq, tmp, qn, ssqh, rsth = sq_[bi], tmp_[bi], qn_[bi], ssqh_[bi], rsth_[bi]
                        pso = bi * 512
                        P.op("act", lambda e, ps=ps, sq=sq: e.activation(out=sq[:], in_=ps[:], func=AF.Square), reads=[ps], writes=[sq])
                        P.op("dve", lambda e, sq=sq, ssqh=ssqh: e.tensor_reduce(out=ssqh[:], in_=sq[:].rearrange("p (h d) -> p h d", h=8), axis=AX.X, op=ALU.add),
                             reads=[sq], writes=[ssqh])
                        P.op("act", lambda e, ssqh=ssqh, rsth=rsth: e.activation(out=rsth[:], in_=ssqh[:], func=AF.Sqrt, scale=1.0 / 64, bias=k.epsb[:, 0:1]),
                             reads=[ssqh, k.epsb], writes=[rsth])
                        P.op("dve", lambda e, rsth=rsth: e.reciprocal(out=rsth[:], in_=rsth[:]), reads=[rsth], writes=[rsth])
                        P.op("dve", lambda e, ps=ps, tmp=tmp, rsth=rsth: e.tensor_tensor(out=tmp[:].rearrange("p (h d) -> p h d", h=8),
                                                                                      in0=ps[:].rearrange("p (h d) -> p h d", h=8),
                                                                                      in1=rsth[:, :].unsqueeze(2).to_broadcast([128, 8, 64]), op=ALU.mult),
                             reads=[ps, rsth], writes=[tmp])
                        P.op("pool", lambda e, gain=gain, qn=qn, tmp=tmp: e.tensor_tensor(out=qn[:], in0=tmp[:], in1=gain[:], op=ALU.mult),
                             reads=[tmp, gain], writes=[qn])
                        def stageB(qn=qn, pso=pso, bi=bi, dstT=dstT, t=t, n=n):
                            for pp in range(4):
                                P.op("pe", lambda e, pp=pp: e.transpose(out=c.psT[:, pso + pp * 128:pso + (pp + 1) * 128], in_=qn[:, pp * 128:(pp + 1) * 128],
                                                                        identity=c.ident[:]), reads=[qn, c.ident], writes=[k.psT_trk[bi]])
                            p0 = (n % 2) * 4
                            P.op("act", lambda e: e.copy(out=dstT[:, p0:p0 + 4, t * 128:(t + 1) * 128],
                                                         in_=c.psT[:, pso:pso + 512].rearrange("p (k t) -> p k t", k=4)),
                                 reads=[k.psT_trk[bi]], writes=[dstT])
                        if pendB:
                            pendB.pop(0)()
                        pendB.append(stageB)
                    else:
                        P.op("act", lambda e, ps=ps, t=t, n=n: e.copy(out=v_sb[:, t, (n - 4) * 512:(n - 3) * 512], in_=ps[:]),
                             reads=[ps], writes=[v_sb])
            while pendB:
                pendB.pop(0)()
            if sbk:
                units = [(h, Q, ci, cc) for h in range(16) for Q in range(4) for ci, cc in enumerate(range(4 * Q + 3, -1, -1))]

                def geom(i):
                    h, Q, ci, cc = units[i]
                    d = cc - 4 * Q
                    c_lo = 128 * d if d > 0 else 0
                    return h, Q, ci, cc, d, slice(c_lo, 512), slice(Q * 512 + c_lo, (Q + 1) * 512), h // 2, (h % 2) * 64

                def stage1(i):
                    h, Q, ci, cc, d, cs, qs, p, base = geom(i)
                    pz, es, sp = psZ[i % 4], esb[i % 2], spb[i % 2]
                    mm(P, pz[:, cs], kT[base:base + 64, p, cc * 128:(cc + 1) * 128], qT[base:base + 64, p, qs], True, False, [kT, qT], [pz])
                    P.op("act", lambda e: e.activation(out=es[:, cs], in_=pz[:, cs], func=AF.Exp), reads=[pz], writes=[es])
                    P.op("act", lambda e: e.activation(out=sp[:, cs], in_=es[:, cs], func=AF.Ln, bias=1.0), reads=[es], writes=[sp])
                    if d >= 0:
                        P.op("dve", lambda e: e.tensor_tensor(out=sp[:, cs], in0=sp[:, cs], in1=masks[:, d, cs], op=ALU.mult),
                             reads=[sp, masks], writes=[sp])

                def stage2(i):
                    h, Q, ci, cc, d, cs, qs, p, base = geom(i)
                    paf, A, sp, ef = psZ[i % 4], Ab[i % 3], spb[i % 2], efb[i % 2]
                    mm(P, paf[:, cs], negtri[:], sp[:, cs], False, ci == 0, [negtri, sp], [paf])
                    Lo, Ln_ = Lbb[(ci + 1) % 2], Lbb[ci % 2]
                    if ci > 0:
                        mm(P, paf[:, cs], negones[:], Lo[:, cs], False, True, [negones, Lo], [paf])
                        P.op("pool", lambda e: e.tensor_tensor(out=Ln_[:, cs], in0=Lo[:, cs], in1=sp[:, cs], op=ALU.add), reads=[Lo, sp], writes=[Ln_])
                    else:
                        P.op("pool", lambda e: e.memset(Lbb[0][:], 0.0), writes=[Lbb[0]])
                        P.op("pool", lambda e: e.memset(Lbb[1][:], 0.0), writes=[Lbb[1]])
                        P.op("pool", lambda e: e.tensor_copy(out=Ln_[:, cs], in_=sp[:, cs]), reads=[sp, Ln_], writes=[Ln_])
                    for _ in range(N_DUMMY):
                        mm(P, psP[0][:], negones[:], masks[:, 0, :], True, True, [negones, masks], [psP[0]])
                    if d >= 0:
                        P.op("act", lambda e: e.activation(out=ef[:, cs], in_=paf[:, cs], func=AF.Exp), reads=[paf], writes=[ef])
                        P.op("dve", lambda e: e.tensor_tensor(out=A[:, cs], in0=ef[:, cs], in1=masks[:, d, cs], op=ALU.mult), reads=[ef, masks], writes=[A])
                    else:
                        P.op("act", lambda e: e.activation(out=A[:], in_=paf[:], func=AF.Exp), reads=[paf], writes=[A])

                def stage3(i):
                    h, Q, ci, cc, d, cs, qs, p, base = geom(i)
                    A = Ab[i % 3]
                    kwo = dict(tile_position=(0, 64)) if base == 64 else {}
                    if ci == 0:
                        mm(P, psOT[base:base + 64, :], zer[:], masks[:, 0, :], True, False, [zer, masks], [psOT], **kwo)
                    mm(P, psOT[base:base + 64, cs], v_sb[:, cc, h * 64:(h + 1) * 64], A[:, cs], False, cc == 0, [v_sb, A], [psOT], **kwo)
                    if cc == 0:
                        P.op("act", lambda e: e.copy(out=oT[base:base + 64, p, Q * 512:(Q + 1) * 512], in_=psOT[base:base + 64, :]),
                             reads=[psOT], writes=[oT])

                n_u = len(units)
                stage1(0)
                for i in range(n_u):
                    if i + 1 < n_u:
                        stage1(i + 1)
                    stage2(i)
                    if i >= 1:
                        stage3(i - 1)
                stage3(n_u - 1)
            else:
                unitsc = [(h, t, ji, jj, len([x for x in range(5) if t - 4 + x >= 0]))
                          for h in range(16) for t in range(TPS) for ji, jj in enumerate([x for x in range(5) if t - 4 + x >= 0])]

                def cstage1(i):
                    h, t, ji, jj, nj = unitsc[i]
                    p, base = h // 2, (h % 2) * 64
                    kt = t - 4 + jj
                    pz, A = psZ[i % 3], Ab[i % 4]
                    if t == 0 and ji == 0:
                        va = vaug[h % 2]
                        P.op("pool", lambda e: e.tensor_copy(out=va[:, :, base:base + 64], in_=v_sb[:, :, h * 64:(h + 1) * 64]), reads=[v_sb, va], writes=[va])
                    mm(P, pz[:], kT[base:base + 64, p, kt * 128:(kt + 1) * 128], qT[base:base + 64, p, t * 128:(t + 1) * 128], True, True, [kT, qT], [pz])
                    if jj >= 3:
                        sb_ = ssb[i % 2]
                        P.op("dve", lambda e: e.tensor_tensor(out=sb_[:], in0=pz[:], in1=BM[:, h, jj - 3, :], op=ALU.add), reads=[pz, BM], writes=[sb_])
                        P.op("act", lambda e: e.activation(out=A[:], in_=sb_[:], func=AF.Exp), reads=[sb_], writes=[A])
                    elif jj == 0:
                        sb_ = ssb[i % 2]
                        P.op("act", lambda e: e.activation(out=sb_[:], in_=pz[:], func=AF.Exp, bias=c256[:, h:h + 1]), reads=[pz, c256], writes=[sb_])
                        P.op("dve", lambda e: e.tensor_tensor(out=A[:], in0=sb_[:], in1=mask0[:], op=ALU.mult), reads=[sb_, mask0], writes=[A])
                    else:
                        P.op("act", lambda e: e.activation(out=A[:], in_=pz[:], func=AF.Exp, bias=c256[:, h:h + 1]), reads=[pz, c256], writes=[A])

                def cstage2(i):
                    h, t, ji, jj, nj = unitsc[i]
                    p, base = h // 2, (h % 2) * 64
                    kt = t - 4 + jj
                    A = Ab[i % 4]
                    po = psOT[(h * TPS + t) % 2]
                    va = vaug[h % 2]
                    mm(P, po[:], va[:, kt, :], A[:], ji == 0, ji == nj - 1, [va, A], [po])
                    for _ in range(N_DUMMY_CA):
                        mm(P, psP[0][:], c.ident[:], BM[:, 0:2, :, :].rearrange("p a b c -> p (a b c)"), True, True, [c.ident, BM], [psP[0]])
                    if ji == nj - 1:
                        ob = 64 - base
                        rc = rec[(h * TPS + t) % 2]
                        P.op("dve", lambda e: e.reciprocal(out=rc[ob:ob + 64, :], in_=po[ob:ob + 64, :]), reads=[po], writes=[rc])
                        P.op("dve", lambda e: e.tensor_copy(out=rc[base:base + 64, :], in_=rc[ob:ob + 64, :]), reads=[rc], writes=[rc])
                        P.op("dve", lambda e: e.tensor_tensor(out=oT[base:base + 64, p, t * 128:(t + 1) * 128], in0=po[base:base + 64, :],
                                                              in1=rc[base:base + 64, :], op=ALU.mult), reads=[po, rc], writes=[oT])

                n_c = len(unitsc)
                cstage1(0)
                cstage1(1)
                for i in range(n_c):
                    if i + 2 < n_c:
                        cstage1(i + 2)
                    cstage2(i)
            for half in range(2):
                P.dma("pool", wb[half][:], wov[:, :, half * 512:(half + 1) * 512], writes=[wb[half]])
            for t in range(TPS):
                gt = b * TPS + t
                ap, trk = xsrc(k, first, gt)
                xt = c.xt[c.cnt % 2]
                c.cnt += 1
                P.dma("sp", xt[:], ap, reads=[trk], writes=[xt])
                for half in range(2):
                    ps = psP[cnt[1] % 2]
                    cnt[1] += 1
                    for kk in range(8):
                        mm(P, ps[:], oT[:, kk, t * 128:(t + 1) * 128], wb[half][:, kk, :], kk == 0, kk == 7, [oT, wb[half]], [ps])
                    epilogue_half(P, k, c, xt, ps[:], ps, half)
                store_x(P, k, xt, gt)
        P.barrier_ops()
        P.flush()


GELU_FUNC = AF.Gelu_apprx_tanh
N_DUMMY = 2
N_DUMMY_CA = 1


def phase_sg(P, k, layer):
    win = k.sg_win[0].rearrange("(k p) n -> p k n", p=128)
    wout = k.sg_wout[0].rearrange("(k p) n -> p k n", p=128)
    H3 = 3 * D
    with ExitStack() as st:
        c = alloc_common(P, st, k, hb2=True)
        hT = P.sbuf(st, "hT", [128, 8, 512], BF16)
        zb = P.sbuf(st, "zb", [128, 4, 2 * H3], BF16)
        yT = P.sbuf(st, "yT", [128, 24, 512], BF16)
        winb = [P.sbuf(st, f"winb{i}", [128, 8, 512], BF16) for i in range(2)]
        woutb = [P.sbuf(st, f"woutb{i}", [128, 24, 512], BF16) for i in range(2)]
        vgb = P.sbuf(st, "vgb", [128, H3], F32)
        vn = P.sbuf(st, "vn", [128, H3], BF16)
        yb = P.sbuf(st, "yb", [128, H3], BF16)
        wsr = P.sbuf(st, "wsr", [128, 8, 128], BF16)
        wsT = P.sbuf(st, "wsT", [128, 8, 128], BF16)
        bs = P.sbuf(st, "bs", [128, 8], F32)
        ssv = P.sbuf(st, "ssv", [128, 4, 8], F32)
        rs = P.sbuf(st, "rs", [128, 2], F32)
        psZ = [P.psum(st, f"psZ{i}", [128, 512], F32) for i in range(3)]
        psM = [P.psum(st, f"psM{i}", [128, 512], F32) for i in range(2)]
        psY = [P.psum(st, f"psY{i}", [128, 512], F32) for i in range(2)]
        P.dma("sp", vgb[:], k.sg_vg[0:1, :].partition_broadcast(128), writes=[vgb])
        P.dma("pool", wsr[:], k.sg_ws[0].rearrange("g i j -> i g j"), writes=[wsr])
        P.dma("sp", bs[:], k.sg_bs[0].rearrange("g i -> i g"), writes=[bs], allow_slow_non_contiguous=True)
        for g in range(8):
            P.op("pe", lambda e, g=g: e.transpose(out=c.psT[:, g * 128:(g + 1) * 128], in_=wsr[:, g, :], identity=c.ident[:]),
                 reads=[wsr, c.ident], writes=[c.psT])
        P.op("act", lambda e: e.copy(out=wsT[:], in_=c.psT[:].rearrange("p (g i) -> p g i", g=8)), reads=[c.psT], writes=[wsT])
        P.op("dve", lambda e: e.memset(wsT[64:128, :, 0:64], 0.0), reads=[wsT], writes=[wsT])
        cnt = [0, 0, 0, 0]
        cur_b = -1
        for blk in range(8):
            b = blk // 4
            if b != cur_b:
                load_mod(P, k, c, layer, 0, b)
                cur_b = b
            pend = []
            for tt in range(4):
                gt = blk * 4 + tt
                ap, trk = xsrc(k, False, gt)
                prologue_tile(P, k, c, ap, trk, hT, tt * 128, pend)
            flush_pend(pend)
            for n in range(12):
                w = winb[cnt[0] % 2]
                cnt[0] += 1
                P.dma("pool", w[:], win[:, :, n * 512:(n + 1) * 512], writes=[w])
                for tt in range(4):
                    ps = psZ[cnt[1] % 3]
                    cnt[1] += 1
                    for kk in range(8):
                        mm(P, ps[:], hT[:, kk, tt * 128:(tt + 1) * 128], w[:, kk, :], kk == 0, kk == 7, [hT, w], [ps])
                    P.op("act", lambda e, ps=ps, tt=tt, n=n: e.activation(out=zb[:, tt, n * 512:(n + 1) * 512], in_=ps[:], func=GELU_FUNC),
                         reads=[ps], writes=[zb])
                    if n >= 6:
                        P.op("act", lambda e, tt=tt, n=n: e.activation(out=c.junk[:, 0:512], in_=zb[:, tt, n * 512:(n + 1) * 512], func=AF.Square,
                                                                      accum_out=ssv[:, tt, n - 6:n - 5]), reads=[zb], writes=[c.junk, ssv])
            for tt in range(4):
                col = tt % 2
                P.op("dve", lambda e, tt=tt, col=col: e.tensor_reduce(out=rs[:, col:col + 1], in_=ssv[:, tt, 0:6], axis=AX.X, op=ALU.add),
                     reads=[ssv], writes=[rs])
                P.op("act", lambda e, col=col: e.activation(out=rs[:, col:col + 1], in_=rs[:, col:col + 1], func=AF.Sqrt, scale=1.0 / H3, bias=k.epsb[:, 0:1]),
                     reads=[rs, k.epsb], writes=[rs])
                P.op("dve", lambda e, col=col: e.reciprocal(out=rs[:, col:col + 1], in_=rs[:, col:col + 1]), reads=[rs], writes=[rs])
                P.op("dve", lambda e, tt=tt, col=col: e.scalar_tensor_tensor(out=vn[:], in0=zb[:, tt, H3:2 * H3], scalar=rs[:, col:col + 1], in1=vgb[:],
                                                                            op0=ALU.mult, op1=ALU.mult), reads=[zb, rs, vgb], writes=[vn])
                for g in range(8):
                    pm = psM[cnt[2] % 2]
                    cnt[2] += 1
                    mm(P, pm[:, 0:384], wsT[:, g, :], vn[:, g * 384:(g + 1) * 384], True, True, [wsT, vn], [pm])
                    P.op("dve", lambda e, pm=pm, g=g, tt=tt: e.scalar_tensor_tensor(out=yb[:, g * 384:(g + 1) * 384], in0=pm[:, 0:384], scalar=bs[:, g:g + 1],
                                                                                  in1=zb[:, tt, g * 384:(g + 1) * 384], op0=ALU.add, op1=ALU.mult),
                         reads=[pm, bs, zb], writes=[yb])
                for k0 in range(0, 24, 8):
                    for kk in range(8):
                        P.op("pe", lambda e, kk=kk, k0=k0: e.transpose(out=c.psT[:, kk * 128:(kk + 1) * 128], in_=yb[:, (k0 + kk) * 128:(k0 + kk + 1) * 128],
                                                                      identity=c.ident[:]), reads=[yb, c.ident], writes=[c.psT])
                    P.op("act", lambda e, k0=k0, tt=tt: e.copy(out=yT[:, k0:k0 + 8, tt * 128:(tt + 1) * 128], in_=c.psT[:].rearrange("p (k t) -> p k t", k=8)),
                         reads=[c.psT], writes=[yT])
            for half in range(2):
                P.dma("pool", woutb[half][:], wout[:, :, half * 512:(half + 1) * 512], writes=[woutb[half]])
            for tt in range(4):
                gt = blk * 4 + tt
                ap, trk = xsrc(k, False, gt)
                xt = c.xt[c.cnt % 2]
                c.cnt += 1
                P.dma("sp", xt[:], ap, reads=[trk], writes=[xt])
                for half in range(2):
                    ps = psY[cnt[3] % 2]
                    cnt[3] += 1
                    for kk in range(24):
                        mm(P, ps[:], yT[:, kk, tt * 128:(tt + 1) * 128], woutb[half][:, kk, :], kk == 0, kk == 23, [yT, woutb[half]], [ps])
                    epilogue_half(P, k, c, xt, ps[:], ps, half)
                store_x(P, k, xt, gt)
        P.barrier_ops()
        P.flush()


PHASES_ALL = ["mod", "a0", "f0", "a1", "r1", "f1", "a2", "f2", "a3", "r3", "f3"]


def build_program(phases=None):
    phases = PHASES_ALL if phases is None else phases
    nc = bass.Bass("TRN2", target_bir_lowering=False)
    k = K()
    T_ = D * 0 + NSEQ * SEQ

    def inp(name, shape):
        return nc.dram_tensor(name, list(shape), F32, kind="ExternalInput")

    k.x_in = inp("x", [T_, D])
    k.c = inp("c", [NSEQ, D])
    k.norm_g = inp("norm_g", [4, 2, D])
    k.ada_w = inp("ada_w", [4, D, 6 * D])
    k.ada_b = inp("ada_b", [4, 6 * D])
    k.sb_wqkv = inp("sb_wqkv", [2, D, 3 * D])
    k.sb_qg = inp("sb_qg", [2, 64])
    k.sb_kg = inp("sb_kg", [2, 64])
    k.sb_wo = inp("sb_wo", [2, D, D])
    k.sg_win = inp("sg_win", [1, D, 6 * D])
    k.sg_vg = inp("sg_vg", [1, 3 * D])
    k.sg_ws = inp("sg_ws", [1, 8, 128, 128])
    k.sg_bs = inp("sg_bs", [1, 8, 128])
    k.sg_wout = inp("sg_wout", [1, 3 * D, D])
    k.ca_wqkv = inp("ca_wqkv", [1, D, 3 * D])
    k.ca_qg = inp("ca_qg", [1, 64])
    k.ca_kg = inp("ca_kg", [1, 64])
    k.ca_relb = inp("ca_relb", [1, 16, 257])
    k.ca_wo = inp("ca_wo", [1, D, D])
    k.ff_w13 = inp("ff_w13", [2, D, 2 * DFF])
    k.ff_w2 = inp("ff_w2", [2, DFF, D])
    k.moe_wr = inp("moe_wrT", [2, NE, D])
    k.moe_br = inp("moe_br", [2, NE])
    k.moe_w1 = inp("moe_w1", [2, NE, D, DFE])
    k.moe_w3 = inp("moe_w3", [2, NE, D, DFE])
    k.moe_w2 = inp("moe_w2", [2, NE, DFE, D])
    k.y = nc.dram_tensor("y", [T_, D], F32, kind="ExternalOutput")
    k.modd = nc.dram_tensor("modd", [4, NSEQ, 6 * D], F32)
    k.gates_d = nc.dram_tensor("gates_d", [T_, NE], F32)
    k.Fd = nc.dram_tensor("Fd", [16, 128, 383], F32)
    k.modtrk = Trk("modtrk")
    k.gates_trk = Trk("gates_trk")
    k.Ftrk = Trk("Ftrk")
    k.xin_trk = Trk("xin")
    k.ytrk = [Trk(f"y{t}") for t in range(NSEQ * TPS)]
    with ExitStack() as st:
        P = Prog(nc, st)
        for ph in phases:
            if ph == "mod":
                phase_mod(P, k)
            elif ph == "cp":
                for gt in range(NSEQ * TPS):
                    P.dma("sp", k.y[gt * 128:(gt + 1) * 128, :], k.x_in[gt * 128:(gt + 1) * 128, :], writes=[k.ytrk[gt]], key=f"yst{gt % 4}")
                P.barrier_ops()
                P.flush()
            elif ph[0] == "a":
                layer = int(ph[1])
                kind = ["sb", "sg", "ca"][layer % 3]
                if kind == "sg":
                    phase_sg(P, k, layer)
                else:
                    phase_attn(P, k, layer, kind)
            elif ph[0] == "r":
                phase_router(P, k, int(ph[1]))
            elif ph[0] == "f":
                layer = int(ph[1])
                phase_ffn(P, k, layer, layer % 2 == 1)
    return nc


INPUT_NAMES = ["x", "c", "norm_g", "ada_w", "ada_b", "sb_wqkv", "sb_qg", "sb_kg", "sb_wo", "sg_win", "sg_vg", "sg_ws", "sg_bs",
               "sg_wout", "ca_wqkv", "ca_qg", "ca_kg", "ca_relb", "ca_wo", "ff_w13", "ff_w2", "moe_wr", "moe_br",
               "moe_w1", "moe_w3", "moe_w2"]


def make_in_maps(inputs, n_cores=8):
    f = lambda a: np.ascontiguousarray(np.asarray(a, dtype=np.float32))
    shared = {}
    for name in INPUT_NAMES:
        if name in ("x", "c"):
            continue
        if name == "moe_wr":
            shared["moe_wrT"] = f(np.asarray(inputs[name]).transpose(0, 2, 1))
        else:
            shared[name] = f(inputs[name])
    x = np.asarray(inputs["x"], dtype=np.float32)
    c = np.asarray(inputs["c"], dtype=np.float32)
    maps = []
    for i in range(n_cores):
        m = dict(shared)
        m["x"] = f(x[NSEQ * i:NSEQ * (i + 1)].reshape(NSEQ * SEQ, D))
        m["c"] = f(c[NSEQ * i:NSEQ * (i + 1)])
        maps.append(m)
    return maps


def kernel(**inputs):
    nc = build_program()
    maps = make_in_maps(inputs, 8)
    res = run_bass_kernel_spmd(nc, maps, core_ids=list(range(8)))
    out = np.stack([np.asarray(r["y"], dtype=np.float32).reshape(NSEQ, SEQ, D) for r in res.results], axis=0)
    return out.reshape(8 * NSEQ, SEQ, D)
```

```python
import numpy as np
import concourse.bass as bass
import concourse.mybir as mybir
from concourse.bass_utils import run_bass_kernel_spmd
from contextlib import ExitStack

F32 = mybir.dt.float32
BF16 = mybir.dt.bfloat16
AF = mybir.ActivationFunctionType
ALU = mybir.AluOpType
AX = mybir.AxisListType

ENGS = ("pe", "act", "dve", "pool", "sp")
SAME_ENGINE_SYNC = True


class Trk:
    __slots__ = ("w", "r", "name")

    def __init__(self, name=""):
        self.w = None
        self.r = []
        self.name = name


class T:
    def __init__(self, h, name):
        self.h = h
        self.trk = Trk(name)

    def __getitem__(self, k):
        return self.h[k]


class Op:
    __slots__ = ("eng", "fn", "waits", "kind", "key", "val", "needed", "idx")


class Prog:
    def __init__(self, nc, stack):
        self.nc = nc
        self.stack = stack
        self.sems = {e: stack.enter_context(nc.semaphore("s_" + e)) for e in ENGS}
        self.dma_sems = {}
        self.ops = {e: [] for e in ENGS}
        self.nops = {e: 0 for e in ENGS}
        self.allops = {e: [] for e in ENGS}
        self.seen = {e: {} for e in ENGS}
        self.inc_count = {e: 0 for e in ENGS}
        self.uid = 0
        self.last_c = {}

    def sbuf(self, st, name, shape, dtype):
        self.uid += 1
        h = st.enter_context(self.nc.sbuf_tensor(f"{name}_{self.uid}", list(shape), dtype))
        return T(h, name)

    def psum(self, st, name, shape, dtype=F32):
        self.uid += 1
        h = st.enter_context(self.nc.psum_tensor(f"{name}_{self.uid}", list(shape), dtype))
        return T(h, name)

    def _deps(self, reads, writes):
        evs = []
        for t in reads:
            trk = t.trk if isinstance(t, T) else t
            if trk.w is not None:
                evs.append(trk.w)
        for t in writes:
            trk = t.trk if isinstance(t, T) else t
            if trk.w is not None:
                evs.append(trk.w)
            evs.extend(trk.r)
        return evs

    def _mkwaits(self, eng, evs, pe_accum=False, force_self=False):
        waits = []
        seen = self.seen[eng]
        for ev in evs:
            kind, src, v = ev[0], ev[1], ev[2]
            if kind == "eng":
                if src == eng and not force_self and (not SAME_ENGINE_SYNC or pe_accum or eng == "pe"):
                    continue
                if seen.get(("e", src), -1) >= v:
                    continue
                seen[("e", src)] = v
                waits.append(ev)
                self.allops[src][v].needed = True
            else:
                if seen.get(("d", src), -1) >= v:
                    continue
                seen[("d", src)] = v
                waits.append(ev)
        return waits

    def _update(self, ev, reads, writes):
        for t in writes:
            trk = t.trk if isinstance(t, T) else t
            trk.w = ev
            trk.r = []
        for t in reads:
            trk = t.trk if isinstance(t, T) else t
            if trk in [(x.trk if isinstance(x, T) else x) for x in writes]:
                continue
            trk.r.append(ev)
            if len(trk.r) > 64:
                last = {}
                for e in trk.r:
                    k = (e[0], e[1])
                    if k not in last or last[k][2] < e[2]:
                        last[k] = e
                trk.r = list(last.values())

    def op(self, eng, fn, reads=(), writes=(), accum=False):
        o = Op()
        o.eng = eng
        o.fn = fn
        o.kind = "c"
        o.needed = False
        o.idx = self.nops[eng]
        o.waits = self._mkwaits(eng, self._deps(reads, writes), pe_accum=accum)
        self.nops[eng] += 1
        self.ops[eng].append(o)
        self.allops[eng].append(o)
        ev = ("eng", eng, o.idx)
        self.last_c[eng] = o.idx
        self._update(ev, reads, writes)
        return o

    def dma(self, q, out_ap, in_ap, reads=(), writes=(), key=None, **kw):
        if key is None:
            w0 = writes[0]
            key = (w0.trk if isinstance(w0, T) else w0).name
        if key not in self.dma_sems:
            self.dma_sems[key] = [self.stack.enter_context(self.nc.semaphore("d_" + key)), 0]
        ent = self.dma_sems[key]
        ent[1] += 16
        val = ent[1]
        o = Op()
        o.eng = q
        o.kind = "d"
        o.key = key
        o.val = val
        o.needed = False
        o.idx = self.nops[q]
        sem = ent[0]
        o.fn = lambda e: e.dma_start(out=out_ap, in_=in_ap, **kw).then_inc(sem, 16)
        evs = self._deps(reads, writes)
        if val > 16:
            evs.append(("dma", key, val - 16))
        o.waits = self._mkwaits(q, evs)
        self.nops[q] += 1
        self.ops[q].append(o)
        self.allops[q].append(o)
        ev = ("dma", key, val)
        self._update(ev, reads, writes)
        return o

    def _emit_engine(self, eng, e, ops, incvals):
        sem_self = self.sems[eng]
        for o in ops:
            for ev in o.waits:
                if ev[0] == "eng":
                    e.wait_ge(self.sems[ev[1]], incvals[ev[1]][ev[2]])
                else:
                    e.wait_ge(self.dma_sems[ev[1]][0], ev[2])
            if o.fn is None:
                continue
            ins = o.fn(e)
            if o.kind == "c" and o.needed:
                ins.then_inc(sem_self, 1)

    def flush(self):
        incvals = getattr(self, "_incvals", None)
        if incvals is None:
            incvals = {e: [] for e in ENGS}
            self._incvals = incvals
            self._inc_run = {e: 0 for e in ENGS}
        for eng in ENGS:
            lst = self.allops[eng]
            iv = incvals[eng]
            run = self._inc_run[eng]
            for i in range(len(iv), len(lst)):
                o = lst[i]
                if o.kind == "c" and o.needed:
                    run += 1
                iv.append(run)
            self._inc_run[eng] = run
        ops = self.ops
        self.ops = {e: [] for e in ENGS}
        prog = self
        with self.nc.Block() as block:
            def mk(eng):
                def body(e):
                    prog._emit_engine(eng, e, ops[eng], incvals)
                return body

            block.tensor(mk("pe"))
            block.scalar(mk("act"))
            block.vector(mk("dve"))
            block.gpsimd(mk("pool"))
            block.sync(mk("sp"))

    def barrier_ops(self):
        last = {}
        for eng in ENGS:
            lc = self.last_c.get(eng)
            if lc is not None:
                last[eng] = ("eng", eng, lc)
        dmas = [("dma", k, v[1]) for k, v in self.dma_sems.items() if v[1] > 0]
        for eng in ENGS:
            evs = [ev for s_, ev in last.items()] + dmas
            waits = self._mkwaits(eng, evs, force_self=True)
            if not waits:
                continue
            o = Op()
            o.eng = eng
            o.kind = "b"
            o.needed = False
            o.idx = self.nops[eng]
            o.waits = waits
            o.fn = None
            self.nops[eng] += 1
            self.ops[eng].append(o)
            self.allops[eng].append(o)


D = 1024
SEQ = 2048
NSEQ = 2
TPS = 16
DFF = 2816
DFE = 3584
NE = 8
EPS = 1e-6


class K:
    pass


def mm(P, out_ap, lhsT, rhs, start, stop, reads, writes, **kw):
    P.op("pe", lambda e: e.matmul(out_ap, lhsT=lhsT, rhs=rhs, start=start, stop=stop, **kw),
         reads=reads, writes=writes)


def alloc_common(P, st, k, hb2=False):
    c = K()
    c.ident = P.sbuf(st, "ident", [128, 128], BF16)
    P.op("pool", lambda e: e.memset(c.ident[:], 1.0), writes=[c.ident])
    P.op("pool", lambda e: e.affine_select(out=c.ident[:], in_=c.ident[:], pattern=[[-1, 128]],
                                           compare_op=ALU.is_equal, fill=0.0, base=0, channel_multiplier=1),
         reads=[c.ident], writes=[c.ident])
    c.xt = [P.sbuf(st, f"xt{j}", [128, D], F32) for j in range(2)]
    c.junk = P.sbuf(st, "junk", [128, D], BF16)
    c.t1 = P.sbuf(st, "t1", [128, D], F32)
    c.hb = P.sbuf(st, "hb", [128, D], BF16)
    c.hbs = [c.hb] + ([P.sbuf(st, "hb2", [128, D], BF16)] if hb2 else [])
    c.hbcnt = 0
    c.ssq = P.sbuf(st, "ssq", [128, 2], F32)
    c.rstd = P.sbuf(st, "rstd", [128, 2], F32)
    c.A = P.sbuf(st, "modA", [128, D], F32)
    c.sh = P.sbuf(st, "modsh", [128, D], F32)
    c.g = P.sbuf(st, "modg", [128, D], F32)
    c.psT = P.psum(st, "psT", [128, D], BF16)
    k.epsb = P.sbuf(st, "epsb", [128, 1], F32)
    P.op("pool", lambda e: e.memset(k.epsb[:], EPS), writes=[k.epsb])
    c.cnt = 0
    return c


def load_mod(P, k, c, layer, sub, b):
    o = 3 * sub * D
    P.dma("sp", c.sh[:], k.modd[layer, b:b + 1, o:o + D].partition_broadcast(128), reads=[k.modtrk], writes=[c.sh])
    P.dma("sp", c.t1[:], k.modd[layer, b:b + 1, o + D:o + 2 * D].partition_broadcast(128), reads=[k.modtrk], writes=[c.t1])
    P.dma("sp", c.g[:], k.modd[layer, b:b + 1, o + 2 * D:o + 3 * D].partition_broadcast(128), reads=[k.modtrk], writes=[c.g])
    P.dma("sp", c.A[:], k.norm_g[layer, sub:sub + 1, :].partition_broadcast(128), writes=[c.A])
    P.op("dve", lambda e: e.tensor_tensor(out=c.A[:], in0=c.A[:], in1=c.t1[:], op=ALU.mult),
         reads=[c.A, c.t1], writes=[c.A])


def norm_mod(P, k, c, src_ap, src_trk, out_tile, out_ap):
    xt = c.xt[c.cnt % 2]
    col = c.cnt % 2
    c.cnt += 1
    P.dma("sp", xt[:], src_ap, reads=[src_trk], writes=[xt])
    P.op("act", lambda e: e.activation(out=c.junk[:], in_=xt[:], func=AF.Square, accum_out=c.ssq[:, col:col + 1]),
         reads=[xt], writes=[c.junk, c.ssq])
    P.op("act", lambda e: e.activation(out=c.rstd[:, col:col + 1], in_=c.ssq[:, col:col + 1], func=AF.Sqrt, scale=1.0 / D, bias=k.epsb[:, 0:1]),
         reads=[c.ssq, k.epsb], writes=[c.rstd])
    P.op("dve", lambda e: e.reciprocal(out=c.rstd[:, col:col + 1], in_=c.rstd[:, col:col + 1]), reads=[c.rstd], writes=[c.rstd])
    P.op("dve", lambda e: e.scalar_tensor_tensor(out=c.t1[:], in0=xt[:], scalar=c.rstd[:, col:col + 1], in1=c.A[:],
                                                 op0=ALU.mult, op1=ALU.mult), reads=[xt, c.rstd, c.A], writes=[c.t1])
    P.op("dve", lambda e: e.tensor_tensor(out=out_ap, in0=c.t1[:], in1=c.sh[:], op=ALU.add),
         reads=[c.t1, c.sh], writes=[out_tile])
    return xt


def prologue_tile(P, k, c, src_ap, src_trk, hT, col0, pend=None):
    hb = c.hbs[c.hbcnt % len(c.hbs)]
    c.hbcnt += 1
    norm_mod(P, k, c, src_ap, src_trk, hb, hb[:])

    def stageB():
        for kk in range(8):
            P.op("pe", lambda e, kk=kk: e.transpose(out=c.psT[:, kk * 128:(kk + 1) * 128], in_=hb[:, kk * 128:(kk + 1) * 128],
                                                    identity=c.ident[:]), reads=[hb, c.ident], writes=[c.psT])
        P.op("act", lambda e: e.copy(out=hT[:, :, col0:col0 + 128], in_=c.psT[:].rearrange("p (k t) -> p k t", k=8)),
             reads=[c.psT], writes=[hT])

    if pend is None or len(c.hbs) < 2:
        stageB()
    else:
        if pend:
            pend.pop(0)()
        pend.append(stageB)


def flush_pend(pend):
    while pend:
        pend.pop(0)()


def epilogue_half(P, k, c, xt, y_ap, y_trk, half):
    hs = slice(half * 512, (half + 1) * 512)
    P.op("dve", lambda e: e.tensor_tensor(out=c.t1[:, hs], in0=y_ap, in1=c.g[:, hs], op=ALU.mult),
         reads=[y_trk, c.g], writes=[c.t1])
    P.op("pool", lambda e: e.tensor_tensor(out=xt[:, hs], in0=c.t1[:, hs], in1=xt[:, hs], op=ALU.add),
         reads=[c.t1, xt], writes=[xt])


def xsrc(k, first, gt):
    if first:
        return k.x_in[gt * 128:(gt + 1) * 128, :], k.xin_trk
    return k.y[gt * 128:(gt + 1) * 128, :], k.ytrk[gt]


def store_x(P, k, xt, gt):
    P.dma("sp", k.y[gt * 128:(gt + 1) * 128, :], xt[:], reads=[xt], writes=[k.ytrk[gt]], key=f"yst{gt % 4}")


def phase_mod(P, k):
    with ExitStack() as st:
        cT = P.sbuf(st, "cT", [128, 8, 2], F32)
        condT = P.sbuf(st, "condT", [128, 8, 2], F32)
        wbuf = [P.sbuf(st, f"adaw{j}", [128, 8, 512], F32) for j in range(2)]
        bias2 = P.sbuf(st, "bias2", [2, 6 * D], F32)
        modsb = P.sbuf(st, "modsb", [2, 6 * D], F32)
        ps = [P.psum(st, f"psm{j}", [128, 512], F32) for j in range(2)]
        for b in range(2):
            P.dma("sp", cT[:, :, b], k.c[b].rearrange("(k p) -> p k", p=128), writes=[cT], allow_slow_non_contiguous=True)
        P.op("act", lambda e: e.activation(out=condT[:], in_=cT[:], func=AF.Silu), reads=[cT], writes=[condT])
        it = 0
        for i in range(4):
            P.dma("sp", bias2[:], k.ada_b[i:i + 1, :].partition_broadcast(2), writes=[bias2])
            wv = k.ada_w[i].rearrange("(k p) n -> p k n", p=128)
            for n in range(12):
                wb = wbuf[it % 2]
                pst = ps[it % 2]
                it += 1
                P.dma("sp", wb[:], wv[:, :, n * 512:(n + 1) * 512], writes=[wb])
                for kk in range(8):
                    mm(P, pst[0:2, :], condT[:, kk, :], wb[:, kk, :], kk == 0, kk == 7, [condT, wb], [pst])
                P.op("dve", lambda e, n=n, pst=pst: e.tensor_tensor(out=modsb[:, n * 512:(n + 1) * 512], in0=pst[0:2, :],
                                                                   in1=bias2[:, n * 512:(n + 1) * 512], op=ALU.add),
                     reads=[pst, bias2], writes=[modsb])
            for o in (D, 4 * D):
                P.op("dve", lambda e, o=o: e.tensor_scalar_add(out=modsb[:, o:o + D], in0=modsb[:, o:o + D], scalar1=1.0),
                     reads=[modsb], writes=[modsb])
            P.dma("sp", k.modd[i], modsb[:], reads=[modsb], writes=[k.modtrk], key="modst")
        P.barrier_ops()
        P.flush()


def ffn_body(P, k, c, b, hT, acc, actb, w13b, w2b, psA, psO, wa_ap, wg_ap, w2_ap, F, G, first_acc, gate_ap, st_cnt):
    nch = F // 128
    wav = wa_ap.rearrange("(k p) f -> p k f", p=128)
    wgv = wg_ap.rearrange("(k p) f -> p k f", p=128)
    w2v = w2_ap.rearrange("(c p) n -> p c n", p=128)
    c0 = 0
    while c0 < nch:
        g = min(G, nch - c0)
        w13 = w13b[st_cnt[0] % 2]
        w2 = w2b[st_cnt[0] % 2]
        st_cnt[0] += 1
        P.dma("pool", w13[:, :, 0, 0:g * 128], wav[:, :, c0 * 128:(c0 + g) * 128], writes=[w13])
        P.dma("pool", w13[:, :, 1, 0:g * 128], wgv[:, :, c0 * 128:(c0 + g) * 128], writes=[w13])
        P.dma("pool", w2[:, 0:g, :], w2v[:, c0:c0 + g, :], writes=[w2])
        for cc in range(g):
            for tt in range(4):
                pa = psA[st_cnt[1] % 4]
                pg = psA[(st_cnt[1] + 1) % 4]
                st_cnt[1] += 2
                for kk in range(8):
                    mm(P, pa[:], w13[:, kk, 0, cc * 128:(cc + 1) * 128], hT[:, kk, tt * 512:(tt + 1) * 512], kk == 0, kk == 7, [w13, hT], [pa])
                for kk in range(8):
                    mm(P, pg[:], w13[:, kk, 1, cc * 128:(cc + 1) * 128], hT[:, kk, tt * 512:(tt + 1) * 512], kk == 0, kk == 7, [w13, hT], [pg])
                sa = k.sa[st_cnt[1] // 2 % 2]
                P.op("act", lambda e, sa=sa, pa=pa: e.activation(out=sa[:], in_=pa[:], func=AF.Silu), reads=[pa], writes=[sa])
                P.op("dve", lambda e, sa=sa, pg=pg, cc=cc, tt=tt: e.tensor_tensor(out=actb[:, cc, tt * 512:(tt + 1) * 512], in0=pg[:], in1=sa[:],
                                                                               op=ALU.mult), reads=[pg, sa], writes=[k.act_trk[cc][tt]])
        for t in range(TPS):
            for half in range(2):
                po = psO[st_cnt[2] % 3]
                st_cnt[2] += 1
                for cc in range(g):
                    mm(P, po[:], actb[:, cc, t * 128:(t + 1) * 128], w2[:, cc, half * 512:(half + 1) * 512], cc == 0, cc == g - 1, [k.act_trk[cc][t // 4], w2], [po])
                dst = acc[:, t, half * 512:(half + 1) * 512]
                if first_acc and c0 == 0:
                    if gate_ap is None:
                        P.op("dve", lambda e, dst=dst, po=po: e.tensor_copy(out=dst, in_=po[:]), reads=[po], writes=[k.acc_trk[t][half]])
                    else:
                        P.op("dve", lambda e, dst=dst, po=po, t=t: e.tensor_scalar(out=dst, in0=po[:], scalar1=gate_ap(t), scalar2=None, op0=ALU.mult),
                             reads=[po, k.gates_sb], writes=[k.acc_trk[t][half]])
                else:
                    if gate_ap is None:
                        P.op("dve", lambda e, dst=dst, po=po: e.tensor_tensor(out=dst, in0=po[:], in1=dst, op=ALU.add), reads=[po, k.acc_trk[t][half]], writes=[k.acc_trk[t][half]])
                    else:
                        P.op("dve", lambda e, dst=dst, po=po, t=t: e.scalar_tensor_tensor(out=dst, in0=po[:], scalar=gate_ap(t), in1=dst,
                                                                                      op0=ALU.mult, op1=ALU.add),
                             reads=[po, k.acc_trk[t][half], k.gates_sb], writes=[k.acc_trk[t][half]])
        c0 += g


def phase_ffn(P, k, layer, moe):
    first = False
    m = layer // 2
    G = 4
    with ExitStack() as st:
        c = alloc_common(P, st, k, hb2=True)
        hT = P.sbuf(st, "hT", [128, 8, SEQ], BF16)
        acc = P.sbuf(st, "acc", [128, TPS, D], F32)
        k.acc_trk = [[Trk(f"acc{t}_{h}") for h in range(2)] for t in range(TPS)]
        k.act_trk = [[Trk(f"act{cc}_{tt}") for tt in range(4)] for cc in range(G)]
        actb = P.sbuf(st, "actb", [128, G, SEQ], BF16)
        w13b = [P.sbuf(st, f"w13b{j}", [128, 8, 2, G * 128], BF16) for j in range(2)]
        w2b = [P.sbuf(st, f"w2b{j}", [128, G, D], BF16) for j in range(2)]
        k.sa = [P.sbuf(st, f"sa{j}", [128, 512], F32) for j in range(2)]
        psA = [P.psum(st, f"psA{j}", [128, 512], F32) for j in range(4)]
        psO = [P.psum(st, f"psO{j}", [128, 512], F32) for j in range(3)]
        k.gates_sb = P.sbuf(st, "gates_sb", [128, TPS, NE], F32)
        st_cnt = [0, 0, 0]
        for b in range(NSEQ):
            load_mod(P, k, c, layer, 1, b)
            pend = []
            for t in range(TPS):
                gt = b * TPS + t
                ap, trk = xsrc(k, first, gt)
                prologue_tile(P, k, c, ap, trk, hT, t * 128, pend)
            flush_pend(pend)
            if not moe:
                ffn_body(P, k, c, b, hT, acc, actb, w13b, w2b, psA, psO,
                         k.ff_w13[m][:, 0:DFF], k.ff_w13[m][:, DFF:2 * DFF], k.ff_w2[m], DFF, G, True, None, st_cnt)
            else:
                P.dma("sp", k.gates_sb[:], k.gates_d[b * SEQ:(b + 1) * SEQ, :].rearrange("(t p) e -> p t e", p=128),
                      reads=[k.gates_trk], writes=[k.gates_sb])
                for ex in range(NE):
                    ffn_body(P, k, c, b, hT, acc, actb, w13b, w2b, psA, psO,
                             k.moe_w1[m, ex], k.moe_w3[m, ex], k.moe_w2[m, ex], DFE, G, ex == 0,
                             (lambda t, ex=ex: k.gates_sb[:, t, ex:ex + 1]), st_cnt)
            for t in range(TPS):
                gt = b * TPS + t
                ap, trk = xsrc(k, first, gt)
                xt = c.xt[c.cnt % 2]
                c.cnt += 1
                P.dma("sp", xt[:], ap, reads=[trk], writes=[xt])
                for half in range(2):
                    epilogue_half(P, k, c, xt, acc[:, t, half * 512:(half + 1) * 512], k.acc_trk[t][half], half)
                store_x(P, k, xt, gt)
        P.barrier_ops()
        P.flush()


def phase_router(P, k, layer):
    m = layer // 2
    with ExitStack() as st:
        c = alloc_common(P, st, k)
        wr = P.sbuf(st, "wr", [128, NE, D], F32)
        br = P.sbuf(st, "br", [128, TPS, NE], F32)
        hfs = [P.sbuf(st, f"hf{i}", [128, D], F32) for i in range(2)]
        jk = P.sbuf(st, "jk", [128, D], F32)
        lg = P.sbuf(st, "lg", [128, TPS, NE], F32)
        v1 = P.sbuf(st, "v1", [128, TPS], F32)
        v2 = P.sbuf(st, "v2", [128, TPS], F32)
        ee = P.sbuf(st, "ee", [128, TPS], F32)
        w1 = P.sbuf(st, "w1", [128, TPS], F32)
        w2 = P.sbuf(st, "w2", [128, TPS], F32)
        m1 = P.sbuf(st, "m1", [128, TPS, NE], F32)
        m2 = P.sbuf(st, "m2", [128, TPS, NE], F32)
        l2 = P.sbuf(st, "l2", [128, TPS, NE], F32)
        gt_sb = P.sbuf(st, "gt_sb", [128, TPS, NE], F32)
        for ex in range(NE):
            P.dma("sp", wr[:, ex, :], k.moe_wr[m, ex:ex + 1, :].partition_broadcast(128), writes=[wr])
        for t in range(TPS):
            P.dma("sp", br[:, t, :], k.moe_br[m:m + 1, :].partition_broadcast(128), writes=[br])
        bc = lambda ap: ap.unsqueeze(2).to_broadcast([128, TPS, NE])
        for b in range(NSEQ):
            load_mod(P, k, c, layer, 1, b)
            for t in range(TPS):
                gt = b * TPS + t
                ap, trk = xsrc(k, False, gt)
                hf = hfs[t % 2]
                norm_mod(P, k, c, ap, trk, hf, hf[:])
                for ex in range(NE):
                    P.op("dve", lambda e, ex=ex, hf=hf, t=t: e.scalar_tensor_tensor(out=jk[:], in0=hf[:], scalar=1.0, in1=wr[:, ex, :], op0=ALU.mult, op1=ALU.mult,
                                                                                 accum_out=lg[:, t, ex:ex + 1]), reads=[hf, wr], writes=[jk, lg])
            P.op("dve", lambda e: e.tensor_tensor(out=lg[:], in0=lg[:], in1=br[:], op=ALU.add), reads=[lg, br], writes=[lg])
            P.op("dve", lambda e: e.tensor_reduce(out=v1[:], in_=lg[:], axis=AX.X, op=ALU.max), reads=[lg], writes=[v1])
            P.op("dve", lambda e: e.tensor_tensor(out=m1[:], in0=lg[:], in1=bc(v1[:, :]), op=ALU.is_equal), reads=[lg, v1], writes=[m1])
            P.op("dve", lambda e: e.scalar_tensor_tensor(out=l2[:], in0=m1[:], scalar=-1e30, in1=lg[:], op0=ALU.mult, op1=ALU.add),
                 reads=[m1, lg], writes=[l2])
            P.op("dve", lambda e: e.tensor_reduce(out=v2[:], in_=l2[:], axis=AX.X, op=ALU.max), reads=[l2], writes=[v2])
            P.op("dve", lambda e: e.tensor_tensor(out=m2[:], in0=l2[:], in1=bc(v2[:, :]), op=ALU.is_equal), reads=[l2, v2], writes=[m2])
            P.op("dve", lambda e: e.tensor_tensor(out=ee[:], in0=v2[:], in1=v1[:], op=ALU.subtract), reads=[v1, v2], writes=[ee])
            P.op("act", lambda e: e.activation(out=ee[:], in_=ee[:], func=AF.Exp), reads=[ee], writes=[ee])
            P.op("dve", lambda e: e.tensor_scalar_add(out=w1[:], in0=ee[:], scalar1=1.0), reads=[ee], writes=[w1])
            P.op("dve", lambda e: e.reciprocal(out=w1[:], in_=w1[:]), reads=[w1], writes=[w1])
            P.op("dve", lambda e: e.tensor_tensor(out=w2[:], in0=ee[:], in1=w1[:], op=ALU.mult), reads=[ee, w1], writes=[w2])
            P.op("dve", lambda e: e.tensor_tensor(out=m1[:], in0=m1[:], in1=bc(w1[:, :]), op=ALU.mult), reads=[m1, w1], writes=[m1])
            P.op("dve", lambda e: e.tensor_tensor(out=m2[:], in0=m2[:], in1=bc(w2[:, :]), op=ALU.mult), reads=[m2, w2], writes=[m2])
            P.op("dve", lambda e: e.tensor_tensor(out=gt_sb[:], in0=m1[:], in1=m2[:], op=ALU.add), reads=[m1, m2], writes=[gt_sb])
            P.dma("sp", k.gates_d[b * SEQ:(b + 1) * SEQ, :].rearrange("(t p) e -> p t e", p=128), gt_sb[:], reads=[gt_sb], writes=[k.gates_trk], key="gst")
        P.barrier_ops()
        P.flush()


def phase_attn(P, k, layer, kind):
    first = (layer == 0)
    sbk = (kind == "sb")
    j = layer // 3
    if sbk:
        wqkv, qg, kg, wo = k.sb_wqkv[j], k.sb_qg, k.sb_kg, k.sb_wo[j]
    else:
        wqkv, qg, kg, wo = k.ca_wqkv[0], k.ca_qg, k.ca_kg, k.ca_wo[0]
    wqv = wqkv.rearrange("(k p) n -> p k n", p=128)
    wov = wo.rearrange("(k p) n -> p k n", p=128)
    with ExitStack() as st:
        c = alloc_common(P, st, k)
        hT = P.sbuf(st, "hT", [128, 8, SEQ], BF16)
        oT = hT
        qT = P.sbuf(st, "qT", [128, 8, SEQ], BF16)
        kT = P.sbuf(st, "kT", [128, 8, SEQ], BF16)
        v_sb = P.sbuf(st, "v_sb", [128, TPS, D], BF16)
        wb = [P.sbuf(st, f"wb{i}", [128, 8, 512], BF16) for i in range(2)]
        gq = P.sbuf(st, "gq", [128, 512], F32)
        gk = P.sbuf(st, "gk", [128, 512], F32)
        sq_ = [P.sbuf(st, f"sq{i}", [128, 512], F32) for i in range(2)]
        tmp_ = [P.sbuf(st, f"tmp{i}", [128, 512], F32) for i in range(2)]
        qn_ = [P.sbuf(st, f"qn{i}", [128, 512], BF16) for i in range(2)]
        ssqh_ = [P.sbuf(st, f"ssqh{i}", [128, 8], F32) for i in range(2)]
        rsth_ = [P.sbuf(st, f"rsth{i}", [128, 8], F32) for i in range(2)]
        psP = [P.psum(st, f"psP{i}", [128, 512], F32) for i in range(2)]
        P.dma("sp", gq[:].rearrange("p (h d) -> p h d", h=8), bass.AP(qg, j * 64, [[0, 128], [0, 8], [1, 64]]), writes=[gq])
        P.dma("sp", gk[:].rearrange("p (h d) -> p h d", h=8), bass.AP(kg, j * 64, [[0, 128], [0, 8], [1, 64]]), writes=[gk])
        P.op("dve", lambda e: e.tensor_scalar_mul(out=gq[:], in0=gq[:], scalar1=0.125), reads=[gq], writes=[gq])
        if sbk:
            esb = [P.sbuf(st, f"esb{i}", [128, 512], F32) for i in range(2)]
            spb = [P.sbuf(st, f"spb{i}", [128, 512], BF16) for i in range(2)]
            Lbb = [P.sbuf(st, f"Lb{i}", [128, 512], BF16) for i in range(2)]
            efb = [P.sbuf(st, f"efb{i}", [128, 512], F32) for i in range(2)]
            Ab = [P.sbuf(st, f"Ab{i}", [128, 512], BF16) for i in range(3)]
            masks = P.sbuf(st, "masks", [128, 4, 512], BF16)
            negtri = P.sbuf(st, "negtri", [128, 128], BF16)
            negones = P.sbuf(st, "negones", [128, 128], BF16)
            zer = P.sbuf(st, "zer", [128, 64], BF16)
            psZ = [P.psum(st, f"psZ{i}", [128, 512], F32) for i in range(4)]
            psOT = P.psum(st, "psOT", [128, 512], F32)
            P.op("pool", lambda e: e.memset(masks[:], 1.0), writes=[masks])
            for d in range(4):
                P.op("pool", lambda e, d=d: e.affine_select(out=masks[:, d, :], in_=masks[:, d, :], pattern=[[1, 512]],
                                                           compare_op=ALU.is_gt, fill=0.0, base=-128 * d, channel_multiplier=-1),
                     reads=[masks], writes=[masks])
            P.op("pool", lambda e: e.memset(negtri[:], -1.0), writes=[negtri])
            P.op("pool", lambda e: e.affine_select(out=negtri[:], in_=negtri[:], pattern=[[-1, 128]], compare_op=ALU.is_ge, fill=0.0,
                                                   base=0, channel_multiplier=1), reads=[negtri], writes=[negtri])
            P.op("pool", lambda e: e.memset(negones[:], -1.0), writes=[negones])
            P.op("pool", lambda e: e.memset(zer[:], 0.0), writes=[zer])
        else:
            BM = P.sbuf(st, "BM", [128, 16, 2, 128], BF16)
            mask0 = P.sbuf(st, "mask0", [128, 128], F32)
            c256 = P.sbuf(st, "c256", [128, 16], F32)
            Esb = P.sbuf(st, "Esb", [16, 384], F32)
            ssb = [P.sbuf(st, f"ssb{i}", [128, 128], F32) for i in range(2)]
            Ab = [P.sbuf(st, f"Ab{i}", [128, 128], BF16) for i in range(4)]
            vaug = [P.sbuf(st, f"vaug{a}", [128, TPS, 128], BF16) for a in range(2)]
            rec = [P.sbuf(st, f"rec{i}", [128, 128], F32) for i in range(2)]
            psZ = [P.psum(st, f"psZ{i}", [128, 128], F32) for i in range(3)]
            psOT = [P.psum(st, f"psOT{i}", [128, 128], F32) for i in range(2)]
            P.dma("sp", Esb[:, 0:256], k.ca_relb[0, :, 1:257], writes=[Esb])
            P.op("dve", lambda e: e.tensor_copy(out=Esb[:, 256:384], in_=Esb[:, 255:256].to_broadcast([16, 128])), reads=[Esb], writes=[Esb])
            P.dma("sp", k.Fd[:, :, :], Esb[:, 0:383].unsqueeze(1).to_broadcast([16, 128, 383]), reads=[Esb], writes=[k.Ftrk], key="Fst")
            for jj in (3, 4):
                off = (4 - jj) * 128 + 127
                P.dma("pool", BM[:, :, jj - 3, :], bass.AP(k.Fd, off, [[382, 128], [128 * 383, 16], [1, 128]]), reads=[k.Ftrk], writes=[BM])
            P.dma("sp", c256[:], k.ca_relb[0:1, :, 256:257].rearrange("o h x -> o (h x)").partition_broadcast(128), writes=[c256],
                  allow_slow_non_contiguous=True)
            P.op("dve", lambda e: e.memset(BM[64:128, :, 1, 0:64], -30000.0), reads=[BM], writes=[BM])
            P.op("pool", lambda e: e.memset(mask0[:], 1.0), writes=[mask0])
            P.op("pool", lambda e: e.memset(mask0[0:64, 64:128], 0.0), reads=[mask0], writes=[mask0])
            for a_ in range(2):
                P.op("pool", lambda e, a_=a_: e.memset(vaug[a_][:], 1.0), writes=[vaug[a_]])

        cnt = [0, 0, 0, 0]
        k.psT_trk = [Trk("psTa"), Trk("psTb")]
        for b in range(NSEQ):
            load_mod(P, k, c, layer, 0, b)
            P.op("act", lambda e: e.copy(out=c.junk[:, 0:8], in_=c.ident[:, 0:8]), reads=[k.psT_trk[0], k.psT_trk[1], c.ident], writes=[c.psT, c.junk])
            for t in range(TPS):
                gt = b * TPS + t
                ap, trk = xsrc(k, first, gt)
                prologue_tile(P, k, c, ap, trk, hT, t * 128)
            P.op("act", lambda e: e.copy(out=c.junk[:, 0:8], in_=c.psT[:, 0:8]), reads=[c.psT], writes=[c.junk, k.psT_trk[0], k.psT_trk[1]])
            pendB = []
            for n in range(6):
                w = wb[cnt[0] % 2]
                cnt[0] += 1
                P.dma("pool", w[:], wqv[:, :, n * 512:(n + 1) * 512], writes=[w])
                for t in range(TPS):
                    ps = psP[cnt[1] % 2]
                    cnt[1] += 1
                    for kk in range(8):
                        mm(P, ps[:], hT[:, kk, t * 128:(t + 1) * 128], w[:, kk, :], kk == 0, kk == 7, [hT, w], [ps])
                    if n < 4:
                        gain = gq if n < 2 else gk
                        dstT = qT if n < 2 else kT
                        bi = cnt[1] % 2
                        sq, tmp, qn, ssqh, rsth = sq_[bi], tmp_[bi], qn_[bi], ssqh_[bi], rsth_[bi]
                        pso = bi * 512
                        P.op("act", lambda e, ps=ps, sq=sq: e.activation(out=sq[:], in_=ps[:], func=AF.Square), reads=[ps], writes=[sq])
                        P.op("dve", lambda e, sq=sq, ssqh=ssqh: e.tensor_reduce(out=ssqh[:], in_=sq[:].rearrange("p (h d) -> p h d", h=8), axis=AX.X, op=ALU.add),
                             reads=[sq], writes=[ssqh])
                        P.op("act", lambda e, ssqh=ssqh, rsth=rsth: e.activation(out=rsth[:], in_=ssqh[:], func=AF.Sqrt, scale=1.0 / 64, bias=k.epsb[:, 0:1]),
                             reads=[ssqh, k.epsb], writes=[rsth])
                        P.op("dve", lambda e, rsth=rsth: e.reciprocal(out=rsth[:], in_=rsth[:]), reads=[rsth], writes=[rsth])
                        P.op("dve", lambda e, ps=ps, tmp=tmp, rsth=rsth: e.tensor_tensor(out=tmp[:].rearrange("p (h d) -> p h d", h=8),
                                                                                      in0=ps[:].rearrange("p (h d) -> p h d", h=8),
                                                                                      in1=rsth[:, :].unsqueeze(2).to_broadcast([128, 8, 64]), op=ALU.mult),
                             reads=[ps, rsth], writes=[tmp])
                        P.op("pool", lambda e, gain=gain, qn=qn, tmp=tmp: e.tensor_tensor(out=qn[:], in0=tmp[:], in1=gain[:], op=ALU.mult),
                             reads=[tmp, gain], writes=[qn])
                        def stageB(qn=qn, pso=pso, bi=bi, dstT=dstT, t=t, n=n):
                            for pp in range(4):
                                P.op("pe", lambda e, pp=pp: e.transpose(out=c.psT[:, pso + pp * 128:pso + (pp + 1) * 128], in_=qn[:, pp * 128:(pp + 1) * 128],
                                                                        identity=c.ident[:]), reads=[qn, c.ident], writes=[k.psT_trk[bi]])
                            p0 = (n % 2) * 4
                            P.op("act", lambda e: e.copy(out=dstT[:, p0:p0 + 4, t * 128:(t + 1) * 128],
                                                         in_=c.psT[:, pso:pso + 512].rearrange("p (k t) -> p k t", k=4)),
                                 reads=[k.psT_trk[bi]], writes=[dstT])
                        if pendB:
                            pendB.pop(0)()
                        pendB.append(stageB)
                    else:
                        P.op("act", lambda e, ps=ps, t=t, n=n: e.copy(out=v_sb[:, t, (n - 4) * 512:(n - 3) * 512], in_=ps[:]),
                             reads=[ps], writes=[v_sb])
            while pendB:
                pendB.pop(0)()
            if sbk:
                units = [(h, Q, ci, cc) for h in range(16) for Q in range(4) for ci, cc in enumerate(range(4 * Q + 3, -1, -1))]

                def geom(i):
                    h, Q, ci, cc = units[i]
                    d = cc - 4 * Q
                    c_lo = 128 * d if d > 0 else 0
                    return h, Q, ci, cc, d, slice(c_lo, 512), slice(Q * 512 + c_lo, (Q + 1) * 512), h // 2, (h % 2) * 64

                def stage1(i):
                    h, Q, ci, cc, d, cs, qs, p, base = geom(i)
                    pz, es, sp = psZ[i % 4], esb[i % 2], spb[i % 2]
                    mm(P, pz[:, cs], kT[base:base + 64, p, cc * 128:(cc + 1) * 128], qT[base:base + 64, p, qs], True, False, [kT, qT], [pz])
                    P.op("act", lambda e: e.activation(out=es[:, cs], in_=pz[:, cs], func=AF.Exp), reads=[pz], writes=[es])
                    P.op("act", lambda e: e.activation(out=sp[:, cs], in_=es[:, cs], func=AF.Ln, bias=1.0), reads=[es], writes=[sp])
                    if d >= 0:
                        P.op("dve", lambda e: e.tensor_tensor(out=sp[:, cs], in0=sp[:, cs], in1=masks[:, d, cs], op=ALU.mult),
                             reads=[sp, masks], writes=[sp])

                def stage2(i):
                    h, Q, ci, cc, d, cs, qs, p, base = geom(i)
                    paf, A, sp, ef = psZ[i % 4], Ab[i % 3], spb[i % 2], efb[i % 2]
                    mm(P, paf[:, cs], negtri[:], sp[:, cs], False, ci == 0, [negtri, sp], [paf])
                    Lo, Ln_ = Lbb[(ci + 1) % 2], Lbb[ci % 2]
                    if ci > 0:
                        mm(P, paf[:, cs], negones[:], Lo[:, cs], False, True, [negones, Lo], [paf])
                        P.op("pool", lambda e: e.tensor_tensor(out=Ln_[:, cs], in0=Lo[:, cs], in1=sp[:, cs], op=ALU.add), reads=[Lo, sp], writes=[Ln_])
                    else:
                        P.op("pool", lambda e: e.memset(Lbb[0][:], 0.0), writes=[Lbb[0]])
                        P.op("pool", lambda e: e.memset(Lbb[1][:], 0.0), writes=[Lbb[1]])
                        P.op("pool", lambda e: e.tensor_copy(out=Ln_[:, cs], in_=sp[:, cs]), reads=[sp, Ln_], writes=[Ln_])
                    if d >= 0:
                        P.op("act", lambda e: e.activation(out=ef[:, cs], in_=paf[:, cs], func=AF.Exp), reads=[paf], writes=[ef])
                        P.op("dve", lambda e: e.tensor_tensor(out=A[:, cs], in0=ef[:, cs], in1=masks[:, d, cs], op=ALU.mult), reads=[ef, masks], writes=[A])
                    else:
                        P.op("act", lambda e: e.activation(out=A[:], in_=paf[:], func=AF.Exp), reads=[paf], writes=[A])

                def stage3(i):
                    h, Q, ci, cc, d, cs, qs, p, base = geom(i)
                    A = Ab[i % 3]
                    kwo = dict(tile_position=(0, 64)) if base == 64 else {}
                    if ci == 0:
                        mm(P, psOT[base:base + 64, :], zer[:], masks[:, 0, :], True, False, [zer, masks], [psOT], **kwo)
                    mm(P, psOT[base:base + 64, cs], v_sb[:, cc, h * 64:(h + 1) * 64], A[:, cs], False, cc == 0, [v_sb, A], [psOT], **kwo)
                    if cc == 0:
                        P.op("act", lambda e: e.copy(out=oT[base:base + 64, p, Q * 512:(Q + 1) * 512], in_=psOT[base:base + 64, :]),
                             reads=[psOT], writes=[oT])

                n_u = len(units)
                stage1(0)
                for i in range(n_u):
                    if i + 1 < n_u:
                        stage1(i + 1)
                    stage2(i)
                    if i >= 1:
                        stage3(i - 1)
                stage3(n_u - 1)
            else:
                unitsc = [(h, t, ji, jj, len([x for x in range(5) if t - 4 + x >= 0]))
                          for h in range(16) for t in range(TPS) for ji, jj in enumerate([x for x in range(5) if t - 4 + x >= 0])]

                def cstage1(i):
                    h, t, ji, jj, nj = unitsc[i]
                    p, base = h // 2, (h % 2) * 64
                    kt = t - 4 + jj
                    pz, A = psZ[i % 3], Ab[i % 4]
                    if t == 0 and ji == 0:
                        va = vaug[h % 2]
                        P.op("pool", lambda e: e.tensor_copy(out=va[:, :, base:base + 64], in_=v_sb[:, :, h * 64:(h + 1) * 64]), reads=[v_sb, va], writes=[va])
                    mm(P, pz[:], kT[base:base + 64, p, kt * 128:(kt + 1) * 128], qT[base:base + 64, p, t * 128:(t + 1) * 128], True, True, [kT, qT], [pz])
                    if jj >= 3:
                        sb_ = ssb[i % 2]
                        P.op("dve", lambda e: e.tensor_tensor(out=sb_[:], in0=pz[:], in1=BM[:, h, jj - 3, :], op=ALU.add), reads=[pz, BM], writes=[sb_])
                        P.op("act", lambda e: e.activation(out=A[:], in_=sb_[:], func=AF.Exp), reads=[sb_], writes=[A])
                    elif jj == 0:
                        sb_ = ssb[i % 2]
                        P.op("act", lambda e: e.activation(out=sb_[:], in_=pz[:], func=AF.Exp, bias=c256[:, h:h + 1]), reads=[pz, c256], writes=[sb_])
                        P.op("dve", lambda e: e.tensor_tensor(out=A[:], in0=sb_[:], in1=mask0[:], op=ALU.mult), reads=[sb_, mask0], writes=[A])
                    else:
                        P.op("act", lambda e: e.activation(out=A[:], in_=pz[:], func=AF.Exp, bias=c256[:, h:h + 1]), reads=[pz, c256], writes=[A])

                def cstage2(i):
                    h, t, ji, jj, nj = unitsc[i]
                    p, base = h // 2, (h % 2) * 64
                    kt = t - 4 + jj
                    A = Ab[i % 4]
                    po = psOT[(h * TPS + t) % 2]
                    va = vaug[h % 2]
                    mm(P, po[:], va[:, kt, :], A[:], ji == 0, ji == nj - 1, [va, A], [po])
                    if ji == nj - 1:
                        ob = 64 - base
                        rc = rec[(h * TPS + t) % 2]
                        P.op("dve", lambda e: e.reciprocal(out=rc[ob:ob + 64, :], in_=po[ob:ob + 64, :]), reads=[po], writes=[rc])
                        P.op("dve", lambda e: e.tensor_copy(out=rc[base:base + 64, :], in_=rc[ob:ob + 64, :]), reads=[rc], writes=[rc])
                        P.op("dve", lambda e: e.tensor_tensor(out=oT[base:base + 64, p, t * 128:(t + 1) * 128], in0=po[base:base + 64, :],
                                                              in1=rc[base:base + 64, :], op=ALU.mult), reads=[po, rc], writes=[oT])

                n_c = len(unitsc)
                cstage1(0)
                cstage1(1)
                for i in range(n_c):
                    if i + 2 < n_c:
                        cstage1(i + 2)
                    cstage2(i)
            for half in range(2):
                P.dma("pool", wb[half][:], wov[:, :, half * 512:(half + 1) * 512], writes=[wb[half]])
            for t in range(TPS):
                gt = b * TPS + t
                ap, trk = xsrc(k, first, gt)
                xt = c.xt[c.cnt % 2]
                c.cnt += 1
                P.dma("sp", xt[:], ap, reads=[trk], writes=[xt])
                for half in range(2):
                    ps = psP[cnt[1] % 2]
                    cnt[1] += 1
                    for kk in range(8):
                        mm(P, ps[:], oT[:, kk, t * 128:(t + 1) * 128], wb[half][:, kk, :], kk == 0, kk == 7, [oT, wb[half]], [ps])
                    epilogue_half(P, k, c, xt, ps[:], ps, half)
                store_x(P, k, xt, gt)
        P.barrier_ops()
        P.flush()


GELU_FUNC = AF.Gelu_apprx_tanh


def phase_sg(P, k, layer):
    win = k.sg_win[0].rearrange("(k p) n -> p k n", p=128)
    wout = k.sg_wout[0].rearrange("(k p) n -> p k n", p=128)
    H3 = 3 * D
    with ExitStack() as st:
        c = alloc_common(P, st, k, hb2=True)
        hT = P.sbuf(st, "hT", [128, 8, 512], BF16)
        zb = P.sbuf(st, "zb", [128, 4, 2 * H3], BF16)
        yT = P.sbuf(st, "yT", [128, 24, 512], BF16)
        winb = [P.sbuf(st, f"winb{i}", [128, 8, 512], BF16) for i in range(2)]
        woutb = [P.sbuf(st, f"woutb{i}", [128, 24, 512], BF16) for i in range(2)]
        vgb = P.sbuf(st, "vgb", [128, H3], F32)
        vn = P.sbuf(st, "vn", [128, H3], BF16)
        yb = P.sbuf(st, "yb", [128, H3], BF16)
        wsr = P.sbuf(st, "wsr", [128, 8, 128], BF16)
        wsT = P.sbuf(st, "wsT", [128, 8, 128], BF16)
        bs = P.sbuf(st, "bs", [128, 8], F32)
        ssv = P.sbuf(st, "ssv", [128, 4, 8], F32)
        rs = P.sbuf(st, "rs", [128, 2], F32)
        psZ = [P.psum(st, f"psZ{i}", [128, 512], F32) for i in range(3)]
        psM = [P.psum(st, f"psM{i}", [128, 512], F32) for i in range(2)]
        psY = [P.psum(st, f"psY{i}", [128, 512], F32) for i in range(2)]
        P.dma("sp", vgb[:], k.sg_vg[0:1, :].partition_broadcast(128), writes=[vgb])
        P.dma("pool", wsr[:], k.sg_ws[0].rearrange("g i j -> i g j"), writes=[wsr])
        P.dma("sp", bs[:], k.sg_bs[0].rearrange("g i -> i g"), writes=[bs], allow_slow_non_contiguous=True)
        for g in range(8):
            P.op("pe", lambda e, g=g: e.transpose(out=c.psT[:, g * 128:(g + 1) * 128], in_=wsr[:, g, :], identity=c.ident[:]),
                 reads=[wsr, c.ident], writes=[c.psT])
        P.op("act", lambda e: e.copy(out=wsT[:], in_=c.psT[:].rearrange("p (g i) -> p g i", g=8)), reads=[c.psT], writes=[wsT])
        P.op("dve", lambda e: e.memset(wsT[64:128, :, 0:64], 0.0), reads=[wsT], writes=[wsT])
        cnt = [0, 0, 0, 0]
        cur_b = -1
        for blk in range(8):
            b = blk // 4
            if b != cur_b:
                load_mod(P, k, c, layer, 0, b)
                cur_b = b
            pend = []
            for tt in range(4):
                gt = blk * 4 + tt
                ap, trk = xsrc(k, False, gt)
                prologue_tile(P, k, c, ap, trk, hT, tt * 128, pend)
            flush_pend(pend)
            for n in range(12):
                w = winb[cnt[0] % 2]
                cnt[0] += 1
                P.dma("pool", w[:], win[:, :, n * 512:(n + 1) * 512], writes=[w])
                for tt in range(4):
                    ps = psZ[cnt[1] % 3]
                    cnt[1] += 1
                    for kk in range(8):
                        mm(P, ps[:], hT[:, kk, tt * 128:(tt + 1) * 128], w[:, kk, :], kk == 0, kk == 7, [hT, w], [ps])
                    P.op("act", lambda e, ps=ps, tt=tt, n=n: e.activation(out=zb[:, tt, n * 512:(n + 1) * 512], in_=ps[:], func=GELU_FUNC),
                         reads=[ps], writes=[zb])
                    if n >= 6:
                        P.op("act", lambda e, tt=tt, n=n: e.activation(out=c.junk[:, 0:512], in_=zb[:, tt, n * 512:(n + 1) * 512], func=AF.Square,
                                                                      accum_out=ssv[:, tt, n - 6:n - 5]), reads=[zb], writes=[c.junk, ssv])
            for tt in range(4):
                col = tt % 2
                P.op("dve", lambda e, tt=tt, col=col: e.tensor_reduce(out=rs[:, col:col + 1], in_=ssv[:, tt, 0:6], axis=AX.X, op=ALU.add),
                     reads=[ssv], writes=[rs])
                P.op("act", lambda e, col=col: e.activation(out=rs[:, col:col + 1], in_=rs[:, col:col + 1], func=AF.Sqrt, scale=1.0 / H3, bias=k.epsb[:, 0:1]),
                     reads=[rs, k.epsb], writes=[rs])
                P.op("dve", lambda e, col=col: e.reciprocal(out=rs[:, col:col + 1], in_=rs[:, col:col + 1]), reads=[rs], writes=[rs])
                P.op("dve", lambda e, tt=tt, col=col: e.scalar_tensor_tensor(out=vn[:], in0=zb[:, tt, H3:2 * H3], scalar=rs[:, col:col + 1], in1=vgb[:],
                                                                            op0=ALU.mult, op1=ALU.mult), reads=[zb, rs, vgb], writes=[vn])
                for g in range(8):
                    pm = psM[cnt[2] % 2]
                    cnt[2] += 1
                    mm(P, pm[:, 0:384], wsT[:, g, :], vn[:, g * 384:(g + 1) * 384], True, True, [wsT, vn], [pm])
                    P.op("dve", lambda e, pm=pm, g=g, tt=tt: e.scalar_tensor_tensor(out=yb[:, g * 384:(g + 1) * 384], in0=pm[:, 0:384], scalar=bs[:, g:g + 1],
                                                                                  in1=zb[:, tt, g * 384:(g + 1) * 384], op0=ALU.add, op1=ALU.mult),
                         reads=[pm, bs, zb], writes=[yb])
                for k0 in range(0, 24, 8):
                    for kk in range(8):
                        P.op("pe", lambda e, kk=kk, k0=k0: e.transpose(out=c.psT[:, kk * 128:(kk + 1) * 128], in_=yb[:, (k0 + kk) * 128:(k0 + kk + 1) * 128],
                                                                      identity=c.ident[:]), reads=[yb, c.ident], writes=[c.psT])
                    P.op("act", lambda e, k0=k0, tt=tt: e.copy(out=yT[:, k0:k0 + 8, tt * 128:(tt + 1) * 128], in_=c.psT[:].rearrange("p (k t) -> p k t", k=8)),
                         reads=[c.psT], writes=[yT])
            for half in range(2):
                P.dma("pool", woutb[half][:], wout[:, :, half * 512:(half + 1) * 512], writes=[woutb[half]])
            for tt in range(4):
                gt = blk * 4 + tt
                ap, trk = xsrc(k, False, gt)
                xt = c.xt[c.cnt % 2]
                c.cnt += 1
                P.dma("sp", xt[:], ap, reads=[trk], writes=[xt])
                for half in range(2):
                    ps = psY[cnt[3] % 2]
                    cnt[3] += 1
                    for kk in range(24):
                        mm(P, ps[:], yT[:, kk, tt * 128:(tt + 1) * 128], woutb[half][:, kk, :], kk == 0, kk == 23, [yT, woutb[half]], [ps])
                    epilogue_half(P, k, c, xt, ps[:], ps, half)
                store_x(P, k, xt, gt)
        P.barrier_ops()
        P.flush()


PHASES_ALL = ["mod", "a0", "f0", "a1", "r1", "f1", "a2", "f2", "a3", "r3", "f3"]


def build_program(phases=None):
    phases = PHASES_ALL if phases is None else phases
    nc = bass.Bass("TRN2", target_bir_lowering=False)
    k = K()
    T_ = D * 0 + NSEQ * SEQ

    def inp(name, shape):
        return nc.dram_tensor(name, list(shape), F32, kind="ExternalInput")

    k.x_in = inp("x", [T_, D])
    k.c = inp("c", [NSEQ, D])
    k.norm_g = inp("norm_g", [4, 2, D])
    k.ada_w = inp("ada_w", [4, D, 6 * D])
    k.ada_b = inp("ada_b", [4, 6 * D])
    k.sb_wqkv = inp("sb_wqkv", [2, D, 3 * D])
    k.sb_qg = inp("sb_qg", [2, 64])
    k.sb_kg = inp("sb_kg", [2, 64])
    k.sb_wo = inp("sb_wo", [2, D, D])
    k.sg_win = inp("sg_win", [1, D, 6 * D])
    k.sg_vg = inp("sg_vg", [1, 3 * D])
    k.sg_ws = inp("sg_ws", [1, 8, 128, 128])
    k.sg_bs = inp("sg_bs", [1, 8, 128])
    k.sg_wout = inp("sg_wout", [1, 3 * D, D])
    k.ca_wqkv = inp("ca_wqkv", [1, D, 3 * D])
    k.ca_qg = inp("ca_qg", [1, 64])
    k.ca_kg = inp("ca_kg", [1, 64])
    k.ca_relb = inp("ca_relb", [1, 16, 257])
    k.ca_wo = inp("ca_wo", [1, D, D])
    k.ff_w13 = inp("ff_w13", [2, D, 2 * DFF])
    k.ff_w2 = inp("ff_w2", [2, DFF, D])
    k.moe_wr = inp("moe_wrT", [2, NE, D])
    k.moe_br = inp("moe_br", [2, NE])
    k.moe_w1 = inp("moe_w1", [2, NE, D, DFE])
    k.moe_w3 = inp("moe_w3", [2, NE, D, DFE])
    k.moe_w2 = inp("moe_w2", [2, NE, DFE, D])
    k.y = nc.dram_tensor("y", [T_, D], F32, kind="ExternalOutput")
    k.modd = nc.dram_tensor("modd", [4, NSEQ, 6 * D], F32)
    k.gates_d = nc.dram_tensor("gates_d", [T_, NE], F32)
    k.Fd = nc.dram_tensor("Fd", [16, 128, 383], F32)
    k.modtrk = Trk("modtrk")
    k.gates_trk = Trk("gates_trk")
    k.Ftrk = Trk("Ftrk")
    k.xin_trk = Trk("xin")
    k.ytrk = [Trk(f"y{t}") for t in range(NSEQ * TPS)]
    with ExitStack() as st:
        P = Prog(nc, st)
        for ph in phases:
            if ph == "mod":
                phase_mod(P, k)
            elif ph == "cp":
                for gt in range(NSEQ * TPS):
                    P.dma("sp", k.y[gt * 128:(gt + 1) * 128, :], k.x_in[gt * 128:(gt + 1) * 128, :], writes=[k.ytrk[gt]], key=f"yst{gt % 4}")
                P.barrier_ops()
                P.flush()
            elif ph[0] == "a":
                layer = int(ph[1])
                kind = ["sb", "sg", "ca"][layer % 3]
                if kind == "sg":
                    phase_sg(P, k, layer)
                else:
                    phase_attn(P, k, layer, kind)
            elif ph[0] == "r":
                phase_router(P, k, int(ph[1]))
            elif ph[0] == "f":
                layer = int(ph[1])
                phase_ffn(P, k, layer, layer % 2 == 1)
    return nc


INPUT_NAMES = ["x", "c", "norm_g", "ada_w", "ada_b", "sb_wqkv", "sb_qg", "sb_kg", "sb_wo", "sg_win", "sg_vg", "sg_ws", "sg_bs",
               "sg_wout", "ca_wqkv", "ca_qg", "ca_kg", "ca_relb", "ca_wo", "ff_w13", "ff_w2", "moe_wr", "moe_br",
               "moe_w1", "moe_w3", "moe_w2"]


def make_in_maps(inputs, n_cores=8):
    f = lambda a: np.ascontiguousarray(np.asarray(a, dtype=np.float32))
    shared = {}
    for name in INPUT_NAMES:
        if name in ("x", "c"):
            continue
        if name == "moe_wr":
            shared["moe_wrT"] = f(np.asarray(inputs[name]).transpose(0, 2, 1))
        else:
            shared[name] = f(inputs[name])
    x = np.asarray(inputs["x"], dtype=np.float32)
    c = np.asarray(inputs["c"], dtype=np.float32)
    maps = []
    for i in range(n_cores):
        m = dict(shared)
        m["x"] = f(x[NSEQ * i:NSEQ * (i + 1)].reshape(NSEQ * SEQ, D))
        m["c"] = f(c[NSEQ * i:NSEQ * (i + 1)])
        maps.append(m)
    return maps


def kernel(**inputs):
    nc = build_program()
    maps = make_in_maps(inputs, 8)
    res = run_bass_kernel_spmd(nc, maps, core_ids=list(range(8)))
    out = np.stack([np.asarray(r["y"], dtype=np.float32).reshape(NSEQ, SEQ, D) for r in res.results], axis=0)
    return out.reshape(8 * NSEQ, SEQ, D)
```

```python
import numpy as np
import concourse.bass as bass
import concourse.mybir as mybir
from concourse.bass_utils import run_bass_kernel_spmd
from contextlib import ExitStack

F32 = mybir.dt.float32
BF16 = mybir.dt.bfloat16
AF = mybir.ActivationFunctionType
ALU = mybir.AluOpType
AX = mybir.AxisListType

ENGS = ("pe", "act", "dve", "pool", "sp")
SAME_ENGINE_SYNC = True


class Trk:
    __slots__ = ("w", "r", "name")

    def __init__(self, name=""):
        self.w = None
        self.r = []
        self.name = name


class T:
    def __init__(self, h, name):
        self.h = h
        self.trk = Trk(name)

    def __getitem__(self, k):
        return self.h[k]


class Op:
    __slots__ = ("eng", "fn", "waits", "kind", "key", "val", "needed", "idx")


class Prog:
    def __init__(self, nc, stack):
        self.nc = nc
        self.stack = stack
        self.sems = {e: stack.enter_context(nc.semaphore("s_" + e)) for e in ENGS}
        self.dma_sems = {}
        self.ops = {e: [] for e in ENGS}
        self.nops = {e: 0 for e in ENGS}
        self.allops = {e: [] for e in ENGS}
        self.seen = {e: {} for e in ENGS}
        self.inc_count = {e: 0 for e in ENGS}
        self.uid = 0
        self.last_c = {}

    def sbuf(self, st, name, shape, dtype):
        self.uid += 1
        h = st.enter_context(self.nc.sbuf_tensor(f"{name}_{self.uid}", list(shape), dtype))
        return T(h, name)

    def psum(self, st, name, shape, dtype=F32):
        self.uid += 1
        h = st.enter_context(self.nc.psum_tensor(f"{name}_{self.uid}", list(shape), dtype))
        return T(h, name)

    def _deps(self, reads, writes):
        evs = []
        for t in reads:
            trk = t.trk if isinstance(t, T) else t
            if trk.w is not None:
                evs.append(trk.w)
        for t in writes:
            trk = t.trk if isinstance(t, T) else t
            if trk.w is not None:
                evs.append(trk.w)
            evs.extend(trk.r)
        return evs

    def _mkwaits(self, eng, evs, pe_accum=False, force_self=False):
        waits = []
        seen = self.seen[eng]
        for ev in evs:
            kind, src, v = ev[0], ev[1], ev[2]
            if kind == "eng":
                if src == eng and not force_self and (not SAME_ENGINE_SYNC or pe_accum or eng == "pe"):
                    continue
                if seen.get(("e", src), -1) >= v:
                    continue
                seen[("e", src)] = v
                waits.append(ev)
                self.allops[src][v].needed = True
            else:
                if seen.get(("d", src), -1) >= v:
                    continue
                seen[("d", src)] = v
                waits.append(ev)
        return waits

    def _update(self, ev, reads, writes):
        for t in writes:
            trk = t.trk if isinstance(t, T) else t
            trk.w = ev
            trk.r = []
        for t in reads:
            trk = t.trk if isinstance(t, T) else t
            if trk in [(x.trk if isinstance(x, T) else x) for x in writes]:
                continue
            trk.r.append(ev)
            if len(trk.r) > 64:
                last = {}
                for e in trk.r:
                    k = (e[0], e[1])
                    if k not in last or last[k][2] < e[2]:
                        last[k] = e
                trk.r = list(last.values())

    def op(self, eng, fn, reads=(), writes=(), accum=False, noself=False):
        o = Op()
        o.eng = eng
        o.fn = fn
        o.kind = "c"
        o.needed = False
        o.idx = self.nops[eng]
        evs_ = self._deps(reads, writes)
        if noself:
            evs_ = [ev for ev in evs_ if not (ev[0] == "eng" and ev[1] == eng)]
        o.waits = self._mkwaits(eng, evs_, pe_accum=accum)
        self.nops[eng] += 1
        self.ops[eng].append(o)
        self.allops[eng].append(o)
        ev = ("eng", eng, o.idx)
        self.last_c[eng] = o.idx
        self._update(ev, reads, writes)
        return o

    def dma(self, q, out_ap, in_ap, reads=(), writes=(), key=None, **kw):
        if key is None:
            w0 = writes[0]
            key = (w0.trk if isinstance(w0, T) else w0).name
        if key not in self.dma_sems:
            self.dma_sems[key] = [self.stack.enter_context(self.nc.semaphore("d_" + key)), 0]
        ent = self.dma_sems[key]
        ent[1] += 16
        val = ent[1]
        o = Op()
        o.eng = q
        o.kind = "d"
        o.key = key
        o.val = val
        o.needed = False
        o.idx = self.nops[q]
        sem = ent[0]
        o.fn = lambda e: e.dma_start(out=out_ap, in_=in_ap, **kw).then_inc(sem, 16)
        evs = self._deps(reads, writes)
        if val > 16:
            evs.append(("dma", key, val - 16))
        o.waits = self._mkwaits(q, evs)
        self.nops[q] += 1
        self.ops[q].append(o)
        self.allops[q].append(o)
        ev = ("dma", key, val)
        self._update(ev, reads, writes)
        return o

    def _emit_engine(self, eng, e, ops, incvals):
        sem_self = self.sems[eng]
        for o in ops:
            for ev in o.waits:
                if ev[0] == "eng":
                    e.wait_ge(self.sems[ev[1]], incvals[ev[1]][ev[2]])
                else:
                    e.wait_ge(self.dma_sems[ev[1]][0], ev[2])
            if o.fn is None:
                continue
            ins = o.fn(e)
            if o.kind == "c" and o.needed:
                ins.then_inc(sem_self, 1)

    def flush(self):
        incvals = getattr(self, "_incvals", None)
        if incvals is None:
            incvals = {e: [] for e in ENGS}
            self._incvals = incvals
            self._inc_run = {e: 0 for e in ENGS}
        for eng in ENGS:
            lst = self.allops[eng]
            iv = incvals[eng]
            run = self._inc_run[eng]
            for i in range(len(iv), len(lst)):
                o = lst[i]
                if o.kind == "c" and o.needed:
                    run += 1
                iv.append(run)
            self._inc_run[eng] = run
        ops = self.ops
        self.ops = {e: [] for e in ENGS}
        prog = self
        with self.nc.Block() as block:
            def mk(eng):
                def body(e):
                    prog._emit_engine(eng, e, ops[eng], incvals)
                return body

            block.tensor(mk("pe"))
            block.scalar(mk("act"))
            block.vector(mk("dve"))
            block.gpsimd(mk("pool"))
            block.sync(mk("sp"))

    def barrier_ops(self):
        last = {}
        for eng in ENGS:
            lc = self.last_c.get(eng)
            if lc is not None:
                last[eng] = ("eng", eng, lc)
        dmas = [("dma", k, v[1]) for k, v in self.dma_sems.items() if v[1] > 0]
        for eng in ENGS:
            evs = [ev for s_, ev in last.items()] + dmas
            waits = self._mkwaits(eng, evs, force_self=True)
            if not waits:
                continue
            o = Op()
            o.eng = eng
            o.kind = "b"
            o.needed = False
            o.idx = self.nops[eng]
            o.waits = waits
            o.fn = None
            self.nops[eng] += 1
            self.ops[eng].append(o)
            self.allops[eng].append(o)


D = 1024
SEQ = 2048
NSEQ = 2
TPS = 16
DFF = 2816
DFE = 3584
NE = 8
EPS = 1e-6


class K:
    pass


def mm(P, out_ap, lhsT, rhs, start, stop, reads, writes, **kw):
    P.op("pe", lambda e: e.matmul(out_ap, lhsT=lhsT, rhs=rhs, start=start, stop=stop, **kw),
         reads=reads, writes=writes)


def alloc_common(P, st, k, hb2=False):
    c = K()
    c.ident = P.sbuf(st, "ident", [128, 128], BF16)
    P.op("pool", lambda e: e.memset(c.ident[:], 1.0), writes=[c.ident])
    P.op("pool", lambda e: e.affine_select(out=c.ident[:], in_=c.ident[:], pattern=[[-1, 128]],
                                           compare_op=ALU.is_equal, fill=0.0, base=0, channel_multiplier=1),
         reads=[c.ident], writes=[c.ident])
    c.xt = [P.sbuf(st, f"xt{j}", [128, D], F32) for j in range(2)]
    c.junk = P.sbuf(st, "junk", [128, D], BF16)
    c.t1 = P.sbuf(st, "t1", [128, D], F32)
    c.hb = P.sbuf(st, "hb", [128, D], BF16)
    c.hbs = [c.hb] + ([P.sbuf(st, "hb2", [128, D], BF16)] if hb2 else [])
    c.hbcnt = 0
    c.ssq = P.sbuf(st, "ssq", [128, 2], F32)
    c.rstd = P.sbuf(st, "rstd", [128, 2], F32)
    c.A = P.sbuf(st, "modA", [128, D], F32)
    c.sh = P.sbuf(st, "modsh", [128, D], F32)
    c.g = P.sbuf(st, "modg", [128, D], F32)
    c.psT = P.psum(st, "psT", [128, D], BF16)
    k.epsb = P.sbuf(st, "epsb", [128, 1], F32)
    P.op("pool", lambda e: e.memset(k.epsb[:], EPS), writes=[k.epsb])
    c.cnt = 0
    return c


def load_mod(P, k, c, layer, sub, b):
    o = 3 * sub * D
    P.dma("sp", c.sh[:], k.modd[layer, b:b + 1, o:o + D].partition_broadcast(128), reads=[k.modtrk], writes=[c.sh])
    P.dma("sp", c.t1[:], k.modd[layer, b:b + 1, o + D:o + 2 * D].partition_broadcast(128), reads=[k.modtrk], writes=[c.t1])
    P.dma("sp", c.g[:], k.modd[layer, b:b + 1, o + 2 * D:o + 3 * D].partition_broadcast(128), reads=[k.modtrk], writes=[c.g])
    P.dma("sp", c.A[:], k.norm_g[layer, sub:sub + 1, :].partition_broadcast(128), writes=[c.A])
    P.op("dve", lambda e: e.tensor_tensor(out=c.A[:], in0=c.A[:], in1=c.t1[:], op=ALU.mult),
         reads=[c.A, c.t1], writes=[c.A])


def norm_mod(P, k, c, src_ap, src_trk, out_tile, out_ap):
    xt = c.xt[c.cnt % 2]
    col = c.cnt % 2
    c.cnt += 1
    P.dma("sp", xt[:], src_ap, reads=[src_trk], writes=[xt])
    P.op("act", lambda e: e.activation(out=c.junk[:], in_=xt[:], func=AF.Square, accum_out=c.ssq[:, col:col + 1]),
         reads=[xt], writes=[c.junk, c.ssq])
    P.op("act", lambda e: e.activation(out=c.rstd[:, col:col + 1], in_=c.ssq[:, col:col + 1], func=AF.Sqrt, scale=1.0 / D, bias=k.epsb[:, 0:1]),
         reads=[c.ssq, k.epsb], writes=[c.rstd])
    P.op("dve", lambda e: e.reciprocal(out=c.rstd[:, col:col + 1], in_=c.rstd[:, col:col + 1]), reads=[c.rstd], writes=[c.rstd])
    P.op("dve", lambda e: e.scalar_tensor_tensor(out=c.t1[:], in0=xt[:], scalar=c.rstd[:, col:col + 1], in1=c.A[:],
                                                 op0=ALU.mult, op1=ALU.mult), reads=[xt, c.rstd, c.A], writes=[c.t1])
    P.op("dve", lambda e: e.tensor_tensor(out=out_ap, in0=c.t1[:], in1=c.sh[:], op=ALU.add),
         reads=[c.t1, c.sh], writes=[out_tile])
    return xt


def prologue_tile(P, k, c, src_ap, src_trk, hT, col0, pend=None):
    hb = c.hbs[c.hbcnt % len(c.hbs)]
    c.hbcnt += 1
    norm_mod(P, k, c, src_ap, src_trk, hb, hb[:])

    def stageB():
        for kk in range(8):
            P.op("pe", lambda e, kk=kk: e.transpose(out=c.psT[:, kk * 128:(kk + 1) * 128], in_=hb[:, kk * 128:(kk + 1) * 128],
                                                    identity=c.ident[:]), reads=[hb, c.ident], writes=[c.psT])
        P.op("act", lambda e: e.copy(out=hT[:, :, col0:col0 + 128], in_=c.psT[:].rearrange("p (k t) -> p k t", k=8)),
             reads=[c.psT], writes=[hT])

    if pend is None or len(c.hbs) < 2:
        stageB()
    else:
        if pend:
            pend.pop(0)()
        pend.append(stageB)


def flush_pend(pend):
    while pend:
        pend.pop(0)()


def epilogue_half(P, k, c, xt, y_ap, y_trk, half):
    hs = slice(half * 512, (half + 1) * 512)
    P.op("dve", lambda e: e.tensor_tensor(out=c.t1[:, hs], in0=y_ap, in1=c.g[:, hs], op=ALU.mult),
         reads=[y_trk, c.g], writes=[c.t1])
    P.op("pool", lambda e: e.tensor_tensor(out=xt[:, hs], in0=c.t1[:, hs], in1=xt[:, hs], op=ALU.add),
         reads=[c.t1, xt], writes=[xt])


def xsrc(k, first, gt):
    if first:
        return k.x_in[gt * 128:(gt + 1) * 128, :], k.xin_trk
    return k.y[gt * 128:(gt + 1) * 128, :], k.ytrk[gt]


def store_x(P, k, xt, gt):
    P.dma("sp", k.y[gt * 128:(gt + 1) * 128, :], xt[:], reads=[xt], writes=[k.ytrk[gt]], key=f"yst{gt % 4}")


def phase_mod(P, k):
    with ExitStack() as st:
        cT = P.sbuf(st, "cT", [128, 8, 2], F32)
        condT = P.sbuf(st, "condT", [128, 8, 2], F32)
        wbuf = [P.sbuf(st, f"adaw{j}", [128, 8, 512], F32) for j in range(2)]
        bias2 = P.sbuf(st, "bias2", [2, 6 * D], F32)
        modsb = P.sbuf(st, "modsb", [2, 6 * D], F32)
        ps = [P.psum(st, f"psm{j}", [128, 512], F32) for j in range(2)]
        for b in range(2):
            P.dma("sp", cT[:, :, b], k.c[b].rearrange("(k p) -> p k", p=128), writes=[cT], allow_slow_non_contiguous=True)
        P.op("act", lambda e: e.activation(out=condT[:], in_=cT[:], func=AF.Silu), reads=[cT], writes=[condT])
        it = 0
        for i in range(4):
            P.dma("sp", bias2[:], k.ada_b[i:i + 1, :].partition_broadcast(2), writes=[bias2])
            wv = k.ada_w[i].rearrange("(k p) n -> p k n", p=128)
            for n in range(12):
                wb = wbuf[it % 2]
                pst = ps[it % 2]
                it += 1
                P.dma("sp", wb[:], wv[:, :, n * 512:(n + 1) * 512], writes=[wb])
                for kk in range(8):
                    mm(P, pst[0:2, :], condT[:, kk, :], wb[:, kk, :], kk == 0, kk == 7, [condT, wb], [pst])
                P.op("dve", lambda e, n=n, pst=pst: e.tensor_tensor(out=modsb[:, n * 512:(n + 1) * 512], in0=pst[0:2, :],
                                                                   in1=bias2[:, n * 512:(n + 1) * 512], op=ALU.add),
                     reads=[pst, bias2], writes=[modsb])
            for o in (D, 4 * D):
                P.op("dve", lambda e, o=o: e.tensor_scalar_add(out=modsb[:, o:o + D], in0=modsb[:, o:o + D], scalar1=1.0),
                     reads=[modsb], writes=[modsb])
            P.dma("sp", k.modd[i], modsb[:], reads=[modsb], writes=[k.modtrk], key="modst")
        P.barrier_ops()
        P.flush()


def ffn_body(P, k, c, b, hT, acc, actb, w13b, w2b, psA, psO, wa_ap, wg_ap, w2_ap, F, G, first_acc, gate_ap, st_cnt):
    nch = F // 128
    wav = wa_ap.rearrange("(k p) f -> p k f", p=128)
    wgv = wg_ap.rearrange("(k p) f -> p k f", p=128)
    w2v = w2_ap.rearrange("(c p) n -> p c n", p=128)
    c0 = 0
    while c0 < nch:
        g = min(G, nch - c0)
        w13 = w13b[st_cnt[0] % 2]
        w2 = w2b[st_cnt[0] % 2]
        st_cnt[0] += 1
        P.dma("pool", w13[:, :, 0, 0:g * 128], wav[:, :, c0 * 128:(c0 + g) * 128], writes=[w13])
        P.dma("pool", w13[:, :, 1, 0:g * 128], wgv[:, :, c0 * 128:(c0 + g) * 128], writes=[w13])
        P.dma("pool", w2[:, 0:g, :], w2v[:, c0:c0 + g, :], writes=[w2])
        for cc in range(g):
            for tt in range(4):
                pa = psA[st_cnt[1] % 4]
                pg = psA[(st_cnt[1] + 1) % 4]
                st_cnt[1] += 2
                for kk in range(8):
                    mm(P, pa[:], w13[:, kk, 0, cc * 128:(cc + 1) * 128], hT[:, kk, tt * 512:(tt + 1) * 512], kk == 0, kk == 7, [w13, hT], [pa])
                for kk in range(8):
                    mm(P, pg[:], w13[:, kk, 1, cc * 128:(cc + 1) * 128], hT[:, kk, tt * 512:(tt + 1) * 512], kk == 0, kk == 7, [w13, hT], [pg])
                sa = k.sa[st_cnt[1] // 2 % 2]
                P.op("act", lambda e, sa=sa, pa=pa: e.activation(out=sa[:], in_=pa[:], func=AF.Silu), reads=[pa], writes=[sa])
                P.op("dve", lambda e, sa=sa, pg=pg, cc=cc, tt=tt: e.tensor_tensor(out=actb[:, cc, tt * 512:(tt + 1) * 512], in0=pg[:], in1=sa[:],
                                                                               op=ALU.mult), reads=[pg, sa], writes=[k.act_trk[cc][tt]])
        for t in range(TPS):
            for half in range(2):
                po = psO[st_cnt[2] % 3]
                st_cnt[2] += 1
                for cc in range(g):
                    mm(P, po[:], actb[:, cc, t * 128:(t + 1) * 128], w2[:, cc, half * 512:(half + 1) * 512], cc == 0, cc == g - 1, [k.act_trk[cc][t // 4], w2], [po])
                dst = acc[:, t, half * 512:(half + 1) * 512]
                if first_acc and c0 == 0:
                    if gate_ap is None:
                        P.op("dve", lambda e, dst=dst, po=po: e.tensor_copy(out=dst, in_=po[:]), reads=[po], writes=[k.acc_trk[t][half]])
                    else:
                        P.op("dve", lambda e, dst=dst, po=po, t=t: e.tensor_scalar(out=dst, in0=po[:], scalar1=gate_ap(t), scalar2=None, op0=ALU.mult),
                             reads=[po, k.gates_sb], writes=[k.acc_trk[t][half]])
                else:
                    if gate_ap is None:
                        P.op("dve", lambda e, dst=dst, po=po: e.tensor_tensor(out=dst, in0=po[:], in1=dst, op=ALU.add), reads=[po, k.acc_trk[t][half]], writes=[k.acc_trk[t][half]])
                    else:
                        P.op("dve", lambda e, dst=dst, po=po, t=t: e.scalar_tensor_tensor(out=dst, in0=po[:], scalar=gate_ap(t), in1=dst,
                                                                                      op0=ALU.mult, op1=ALU.add),
                             reads=[po, k.acc_trk[t][half], k.gates_sb], writes=[k.acc_trk[t][half]])
        c0 += g


def phase_ffn(P, k, layer, moe):
    first = False
    m = layer // 2
    G = 4
    with ExitStack() as st:
        c = alloc_common(P, st, k, hb2=True)
        hT = P.sbuf(st, "hT", [128, 8, SEQ], BF16)
        acc = P.sbuf(st, "acc", [128, TPS, D], F32)
        k.acc_trk = [[Trk(f"acc{t}_{h}") for h in range(2)] for t in range(TPS)]
        k.act_trk = [[Trk(f"act{cc}_{tt}") for tt in range(4)] for cc in range(G)]
        actb = P.sbuf(st, "actb", [128, G, SEQ], BF16)
        w13b = [P.sbuf(st, f"w13b{j}", [128, 8, 2, G * 128], BF16) for j in range(2)]
        w2b = [P.sbuf(st, f"w2b{j}", [128, G, D], BF16) for j in range(2)]
        k.sa = [P.sbuf(st, f"sa{j}", [128, 512], F32) for j in range(2)]
        psA = [P.psum(st, f"psA{j}", [128, 512], F32) for j in range(4)]
        psO = [P.psum(st, f"psO{j}", [128, 512], F32) for j in range(3)]
        k.gates_sb = P.sbuf(st, "gates_sb", [128, TPS, NE], F32)
        st_cnt = [0, 0, 0]
        for b in range(NSEQ):
            load_mod(P, k, c, layer, 1, b)
            pend = []
            for t in range(TPS):
                gt = b * TPS + t
                ap, trk = xsrc(k, first, gt)
                prologue_tile(P, k, c, ap, trk, hT, t * 128, pend)
            flush_pend(pend)
            if not moe:
                ffn_body(P, k, c, b, hT, acc, actb, w13b, w2b, psA, psO,
                         k.ff_w13[m][:, 0:DFF], k.ff_w13[m][:, DFF:2 * DFF], k.ff_w2[m], DFF, G, True, None, st_cnt)
            else:
                P.dma("sp", k.gates_sb[:], k.gates_d[b * SEQ:(b + 1) * SEQ, :].rearrange("(t p) e -> p t e", p=128),
                      reads=[k.gates_trk], writes=[k.gates_sb])
                for ex in range(NE):
                    ffn_body(P, k, c, b, hT, acc, actb, w13b, w2b, psA, psO,
                             k.moe_w1[m, ex], k.moe_w3[m, ex], k.moe_w2[m, ex], DFE, G, ex == 0,
                             (lambda t, ex=ex: k.gates_sb[:, t, ex:ex + 1]), st_cnt)
            for t in range(TPS):
                gt = b * TPS + t
                ap, trk = xsrc(k, first, gt)
                xt = c.xt[c.cnt % 2]
                c.cnt += 1
                P.dma("sp", xt[:], ap, reads=[trk], writes=[xt])
                for half in range(2):
                    epilogue_half(P, k, c, xt, acc[:, t, half * 512:(half + 1) * 512], k.acc_trk[t][half], half)
                store_x(P, k, xt, gt)
        P.barrier_ops()
        P.flush()


def phase_router(P, k, layer):
    m = layer // 2
    with ExitStack() as st:
        c = alloc_common(P, st, k)
        wr = P.sbuf(st, "wr", [128, NE, D], F32)
        br = P.sbuf(st, "br", [128, TPS, NE], F32)
        hfs = [P.sbuf(st, f"hf{i}", [128, D], F32) for i in range(2)]
        jk = P.sbuf(st, "jk", [128, D], F32)
        lg = P.sbuf(st, "lg", [128, TPS, NE], F32)
        v1 = P.sbuf(st, "v1", [128, TPS], F32)
        v2 = P.sbuf(st, "v2", [128, TPS], F32)
        ee = P.sbuf(st, "ee", [128, TPS], F32)
        w1 = P.sbuf(st, "w1", [128, TPS], F32)
        w2 = P.sbuf(st, "w2", [128, TPS], F32)
        m1 = P.sbuf(st, "m1", [128, TPS, NE], F32)
        m2 = P.sbuf(st, "m2", [128, TPS, NE], F32)
        l2 = P.sbuf(st, "l2", [128, TPS, NE], F32)
        gt_sb = P.sbuf(st, "gt_sb", [128, TPS, NE], F32)
        for ex in range(NE):
            P.dma("sp", wr[:, ex, :], k.moe_wr[m, ex:ex + 1, :].partition_broadcast(128), writes=[wr])
        for t in range(TPS):
            P.dma("sp", br[:, t, :], k.moe_br[m:m + 1, :].partition_broadcast(128), writes=[br])
        bc = lambda ap: ap.unsqueeze(2).to_broadcast([128, TPS, NE])
        for b in range(NSEQ):
            load_mod(P, k, c, layer, 1, b)
            for t in range(TPS):
                gt = b * TPS + t
                ap, trk = xsrc(k, False, gt)
                hf = hfs[t % 2]
                norm_mod(P, k, c, ap, trk, hf, hf[:])
                for ex in range(NE):
                    P.op("dve", lambda e, ex=ex, hf=hf, t=t: e.scalar_tensor_tensor(out=jk[:], in0=hf[:], scalar=1.0, in1=wr[:, ex, :], op0=ALU.mult, op1=ALU.mult,
                                                                                 accum_out=lg[:, t, ex:ex + 1]), reads=[hf, wr], writes=[jk, lg])
            P.op("dve", lambda e: e.tensor_tensor(out=lg[:], in0=lg[:], in1=br[:], op=ALU.add), reads=[lg, br], writes=[lg])
            P.op("dve", lambda e: e.tensor_reduce(out=v1[:], in_=lg[:], axis=AX.X, op=ALU.max), reads=[lg], writes=[v1])
            P.op("dve", lambda e: e.tensor_tensor(out=m1[:], in0=lg[:], in1=bc(v1[:, :]), op=ALU.is_equal), reads=[lg, v1], writes=[m1])
            P.op("dve", lambda e: e.scalar_tensor_tensor(out=l2[:], in0=m1[:], scalar=-1e30, in1=lg[:], op0=ALU.mult, op1=ALU.add),
                 reads=[m1, lg], writes=[l2])
            P.op("dve", lambda e: e.tensor_reduce(out=v2[:], in_=l2[:], axis=AX.X, op=ALU.max), reads=[l2], writes=[v2])
            P.op("dve", lambda e: e.tensor_tensor(out=m2[:], in0=l2[:], in1=bc(v2[:, :]), op=ALU.is_equal), reads=[l2, v2], writes=[m2])
            P.op("dve", lambda e: e.tensor_tensor(out=ee[:], in0=v2[:], in1=v1[:], op=ALU.subtract), reads=[v1, v2], writes=[ee])
            P.op("act", lambda e: e.activation(out=ee[:], in_=ee[:], func=AF.Exp), reads=[ee], writes=[ee])
            P.op("dve", lambda e: e.tensor_scalar_add(out=w1[:], in0=ee[:], scalar1=1.0), reads=[ee], writes=[w1])
            P.op("dve", lambda e: e.reciprocal(out=w1[:], in_=w1[:]), reads=[w1], writes=[w1])
            P.op("dve", lambda e: e.tensor_tensor(out=w2[:], in0=ee[:], in1=w1[:], op=ALU.mult), reads=[ee, w1], writes=[w2])
            P.op("dve", lambda e: e.tensor_tensor(out=m1[:], in0=m1[:], in1=bc(w1[:, :]), op=ALU.mult), reads=[m1, w1], writes=[m1])
            P.op("dve", lambda e: e.tensor_tensor(out=m2[:], in0=m2[:], in1=bc(w2[:, :]), op=ALU.mult), reads=[m2, w2], writes=[m2])
            P.op("dve", lambda e: e.tensor_tensor(out=gt_sb[:], in0=m1[:], in1=m2[:], op=ALU.add), reads=[m1, m2], writes=[gt_sb])
            P.dma("sp", k.gates_d[b * SEQ:(b + 1) * SEQ, :].rearrange("(t p) e -> p t e", p=128), gt_sb[:], reads=[gt_sb], writes=[k.gates_trk], key="gst")
        P.barrier_ops()
        P.flush()


def phase_attn(P, k, layer, kind):
    first = (layer == 0)
    sbk = (kind == "sb")
    j = layer // 3
    if sbk:
        wqkv, qg, kg, wo = k.sb_wqkv[j], k.sb_qg, k.sb_kg, k.sb_wo[j]
    else:
        wqkv, qg, kg, wo = k.ca_wqkv[0], k.ca_qg, k.ca_kg, k.ca_wo[0]
    wqv = wqkv.rearrange("(k p) n -> p k n", p=128)
    wov = wo.rearrange("(k p) n -> p k n", p=128)
    with ExitStack() as st:
        c = alloc_common(P, st, k)
        hT = P.sbuf(st, "hT", [128, 8, SEQ], BF16)
        oT = hT
        qT = P.sbuf(st, "qT", [128, 8, SEQ], BF16)
        kT = P.sbuf(st, "kT", [128, 8, SEQ], BF16)
        v_sb = P.sbuf(st, "v_sb", [128, TPS, D], BF16)
        wb = [P.sbuf(st, f"wb{i}", [128, 8, 512], BF16) for i in range(2)]
        gq = P.sbuf(st, "gq", [128, 512], F32)
        gk = P.sbuf(st, "gk", [128, 512], F32)
        sq_ = [P.sbuf(st, f"sq{i}", [128, 512], F32) for i in range(2)]
        tmp_ = [P.sbuf(st, f"tmp{i}", [128, 512], F32) for i in range(2)]
        qn_ = [P.sbuf(st, f"qn{i}", [128, 512], BF16) for i in range(2)]
        ssqh_ = [P.sbuf(st, f"ssqh{i}", [128, 8], F32) for i in range(2)]
        rsth_ = [P.sbuf(st, f"rsth{i}", [128, 8], F32) for i in range(2)]
        psP = [P.psum(st, f"psP{i}", [128, 512], F32) for i in range(2)]
        P.dma("sp", gq[:].rearrange("p (h d) -> p h d", h=8), bass.AP(qg, j * 64, [[0, 128], [0, 8], [1, 64]]), writes=[gq])
        P.dma("sp", gk[:].rearrange("p (h d) -> p h d", h=8), bass.AP(kg, j * 64, [[0, 128], [0, 8], [1, 64]]), writes=[gk])
        P.op("dve", lambda e: e.tensor_scalar_mul(out=gq[:], in0=gq[:], scalar1=0.125), reads=[gq], writes=[gq])
        if sbk:
            esb = [P.sbuf(st, f"esb{i}", [128, 512], F32) for i in range(2)]
            spb = [P.sbuf(st, f"spb{i}", [128, 512], BF16) for i in range(2)]
            Lbb = [P.sbuf(st, f"Lb{i}", [128, 512], BF16) for i in range(2)]
            efb = [P.sbuf(st, f"efb{i}", [128, 512], F32) for i in range(2)]
            Ab = [P.sbuf(st, f"Ab{i}", [128, 512], BF16) for i in range(3)]
            masks = P.sbuf(st, "masks", [128, 4, 512], BF16)
            negtri = P.sbuf(st, "negtri", [128, 128], BF16)
            negones = P.sbuf(st, "negones", [128, 128], BF16)
            zer = P.sbuf(st, "zer", [128, 64], BF16)
            psZ = [P.psum(st, f"psZ{i}", [128, 512], F32) for i in range(4)]
            psOT = P.psum(st, "psOT", [128, 512], F32)
            P.op("pool", lambda e: e.memset(masks[:], 1.0), writes=[masks])
            for d in range(4):
                P.op("pool", lambda e, d=d: e.affine_select(out=masks[:, d, :], in_=masks[:, d, :], pattern=[[1, 512]],
                                                           compare_op=ALU.is_gt, fill=0.0, base=-128 * d, channel_multiplier=-1),
                     reads=[masks], writes=[masks])
            P.op("pool", lambda e: e.memset(negtri[:], -1.0), writes=[negtri])
            P.op("pool", lambda e: e.affine_select(out=negtri[:], in_=negtri[:], pattern=[[-1, 128]], compare_op=ALU.is_ge, fill=0.0,
                                                   base=0, channel_multiplier=1), reads=[negtri], writes=[negtri])
            P.op("pool", lambda e: e.memset(negones[:], -1.0), writes=[negones])
            P.op("pool", lambda e: e.memset(zer[:], 0.0), writes=[zer])
        else:
            BM = P.sbuf(st, "BM", [128, 16, 2, 128], BF16)
            mask0 = P.sbuf(st, "mask0", [128, 128], F32)
            c256 = P.sbuf(st, "c256", [128, 16], F32)
            Esb = P.sbuf(st, "Esb", [16, 384], F32)
            ssb = [P.sbuf(st, f"ssb{i}", [128, 128], F32) for i in range(2)]
            Ab = [P.sbuf(st, f"Ab{i}", [128, 128], BF16) for i in range(4)]
            vaug = [P.sbuf(st, f"vaug{a}", [128, TPS, 128], BF16) for a in range(2)]
            rec = [P.sbuf(st, f"rec{i}", [128, 128], F32) for i in range(2)]
            psZ = [P.psum(st, f"psZ{i}", [128, 128], F32) for i in range(3)]
            psOT = [P.psum(st, f"psOT{i}", [128, 128], F32) for i in range(2)]
            P.dma("sp", Esb[:, 0:256], k.ca_relb[0, :, 1:257], writes=[Esb])
            P.op("dve", lambda e: e.tensor_copy(out=Esb[:, 256:384], in_=Esb[:, 255:256].to_broadcast([16, 128])), reads=[Esb], writes=[Esb])
            P.dma("sp", k.Fd[:, :, :], Esb[:, 0:383].unsqueeze(1).to_broadcast([16, 128, 383]), reads=[Esb], writes=[k.Ftrk], key="Fst")
            for jj in (3, 4):
                off = (4 - jj) * 128 + 127
                P.dma("pool", BM[:, :, jj - 3, :], bass.AP(k.Fd, off, [[382, 128], [128 * 383, 16], [1, 128]]), reads=[k.Ftrk], writes=[BM])
            P.dma("sp", c256[:], k.ca_relb[0:1, :, 256:257].rearrange("o h x -> o (h x)").partition_broadcast(128), writes=[c256],
                  allow_slow_non_contiguous=True)
            P.op("dve", lambda e: e.memset(BM[64:128, :, 1, 0:64], -30000.0), reads=[BM], writes=[BM])
            P.op("pool", lambda e: e.memset(mask0[:], 1.0), writes=[mask0])
            P.op("pool", lambda e: e.memset(mask0[0:64, 64:128], 0.0), reads=[mask0], writes=[mask0])
            for a_ in range(2):
                P.op("pool", lambda e, a_=a_: e.memset(vaug[a_][:], 1.0), writes=[vaug[a_]])

        cnt = [0, 0, 0, 0]
        k.psT_trk = [Trk("psTa"), Trk("psTb")]
        for b in range(NSEQ):
            load_mod(P, k, c, layer, 0, b)
            P.op("act", lambda e: e.copy(out=c.junk[:, 0:8], in_=c.junk[:, 8:16]), reads=[k.psT_trk[0], k.psT_trk[1]], writes=[c.psT, c.junk])
            for t in range(TPS):
                gt = b * TPS + t
                ap, trk = xsrc(k, first, gt)
                prologue_tile(P, k, c, ap, trk, hT, t * 128)
            P.op("act", lambda e: e.copy(out=c.junk[:, 0:8], in_=c.psT[:, 0:8]), reads=[c.psT], writes=[c.junk, k.psT_trk[0], k.psT_trk[1]])
            pendB = []
            for n in range(6):
                w = wb[cnt[0] % 2]
                cnt[0] += 1
                P.dma("pool", w[:], wqv[:, :, n * 512:(n + 1) * 512], writes=[w])
                for t in range(TPS):
                    ps = psP[cnt[1] % 2]
                    cnt[1] += 1
                    for kk in range(8):
                        mm(P, ps[:], hT[:, kk, t * 128:(t + 1) * 128], w[:, kk, :], kk == 0, kk == 7, [hT, w], [ps])
                    if n < 4:
                        gain = gq if n < 2 else gk
                        dstT = qT if n < 2 else kT
                        bi = cnt[1] % 2
                        sq, tmp, qn, ssqh, rsth = sq_[bi], tmp_[bi], qn_[bi], ssqh_[bi], rsth_[bi]
                        pso = bi * 512
                        P.op("act", lambda e, ps=ps, sq=sq: e.activation(out=sq[:], in_=ps[:], func=AF.Square), reads=[ps], writes=[sq])
                        P.op("dve", lambda e, sq=sq, ssqh=ssqh: e.tensor_reduce(out=ssqh[:], in_=sq[:].rearrange("p (h d) -> p h d", h=8), axis=AX.X, op=ALU.add),
                             reads=[sq], writes=[ssqh])
                        P.op("act", lambda e, ssqh=ssqh, rsth=rsth: e.activation(out=rsth[:], in_=ssqh[:], func=AF.Sqrt, scale=1.0 / 64, bias=k.epsb[:, 0:1]),
                             reads=[ssqh, k.epsb], writes=[rsth])
                        P.op("dve", lambda e, rsth=rsth: e.reciprocal(out=rsth[:], in_=rsth[:]), reads=[rsth], writes=[rsth])
                        P.op("dve", lambda e, ps=ps, tmp=tmp, rsth=rsth: e.tensor_tensor(out=tmp[:].rearrange("p (h d) -> p h d", h=8),
                                                                                      in0=ps[:].rearrange("p (h d) -> p h d", h=8),
                                                                                      in1=rsth[:, :].unsqueeze(2).to_broadcast([128, 8, 64]), op=ALU.mult),
                             reads=[ps, rsth], writes=[tmp])
                        P.op("pool", lambda e, gain=gain, qn=qn, tmp=tmp: e.tensor_tensor(out=qn[:], in0=tmp[:], in1=gain[:], op=ALU.mult),
                             reads=[tmp, gain], writes=[qn])
                        def stageB(qn=qn, pso=pso, bi=bi, dstT=dstT, t=t, n=n):
                            for pp in range(4):
                                P.op("pe", lambda e, pp=pp: e.transpose(out=c.psT[:, pso + pp * 128:pso + (pp + 1) * 128], in_=qn[:, pp * 128:(pp + 1) * 128],
                                                                        identity=c.ident[:]), reads=[qn, c.ident], writes=[k.psT_trk[bi]])
                            p0 = (n % 2) * 4
                            P.op("act", lambda e: e.copy(out=dstT[:, p0:p0 + 4, t * 128:(t + 1) * 128],
                                                         in_=c.psT[:, pso:pso + 512].rearrange("p (k t) -> p k t", k=4)),
                                 reads=[k.psT_trk[bi]], writes=[dstT])
                        if pendB:
                            pendB.pop(0)()
                        pendB.append(stageB)
                    else:
                        P.op("act", lambda e, ps=ps, t=t, n=n: e.copy(out=v_sb[:, t, (n - 4) * 512:(n - 3) * 512], in_=ps[:]),
                             reads=[ps], writes=[v_sb])
            while pendB:
                pendB.pop(0)()
            if sbk:
                units = [(h, Q, ci, cc) for h in range(16) for Q in range(4) for ci, cc in enumerate(range(4 * Q + 3, -1, -1))]

                def geom(i):
                    h, Q, ci, cc = units[i]
                    d = cc - 4 * Q
                    c_lo = 128 * d if d > 0 else 0
                    return h, Q, ci, cc, d, slice(c_lo, 512), slice(Q * 512 + c_lo, (Q + 1) * 512), h // 2, (h % 2) * 64

                def stage1(i):
                    h, Q, ci, cc, d, cs, qs, p, base = geom(i)
                    pz, es, sp = psZ[i % 4], esb[i % 2], spb[i % 2]
                    mm(P, pz[:, cs], kT[base:base + 64, p, cc * 128:(cc + 1) * 128], qT[base:base + 64, p, qs], True, False, [kT, qT], [pz])
                    P.op("act", lambda e: e.activation(out=es[:, cs], in_=pz[:, cs], func=AF.Exp), reads=[pz], writes=[es])
                    P.op("act", lambda e: e.activation(out=sp[:, cs], in_=es[:, cs], func=AF.Ln, bias=1.0), reads=[es], writes=[sp], noself=True)
                    if d >= 0:
                        P.op("dve", lambda e: e.tensor_tensor(out=sp[:, cs], in0=sp[:, cs], in1=masks[:, d, cs], op=ALU.mult),
                             reads=[sp, masks], writes=[sp])

                def stage2(i):
                    h, Q, ci, cc, d, cs, qs, p, base = geom(i)
                    paf, A, sp, ef = psZ[i % 4], Ab[i % 3], spb[i % 2], efb[i % 2]
                    mm(P, paf[:, cs], negtri[:], sp[:, cs], False, ci == 0, [negtri, sp], [paf])
                    Lo, Ln_ = Lbb[(ci + 1) % 2], Lbb[ci % 2]
                    if ci > 0:
                        mm(P, paf[:, cs], negones[:], Lo[:, cs], False, True, [negones, Lo], [paf])
                        P.op("pool", lambda e: e.tensor_tensor(out=Ln_[:, cs], in0=Lo[:, cs], in1=sp[:, cs], op=ALU.add), reads=[Lo, sp], writes=[Ln_])
                    else:
                        P.op("pool", lambda e: e.memset(Lbb[0][:], 0.0), writes=[Lbb[0]])
                        P.op("pool", lambda e: e.memset(Lbb[1][:], 0.0), writes=[Lbb[1]])
                        P.op("pool", lambda e: e.tensor_copy(out=Ln_[:, cs], in_=sp[:, cs]), reads=[sp, Ln_], writes=[Ln_])
                    if d >= 0:
                        P.op("act", lambda e: e.activation(out=ef[:, cs], in_=paf[:, cs], func=AF.Exp), reads=[paf], writes=[ef])
                        P.op("dve", lambda e: e.tensor_tensor(out=A[:, cs], in0=ef[:, cs], in1=masks[:, d, cs], op=ALU.mult), reads=[ef, masks], writes=[A])
                    else:
                        P.op("act", lambda e: e.activation(out=A[:], in_=paf[:], func=AF.Exp), reads=[paf], writes=[A])

                def stage3(i):
                    h, Q, ci, cc, d, cs, qs, p, base = geom(i)
                    A = Ab[i % 3]
                    kwo = dict(tile_position=(0, 64)) if base == 64 else {}
                    if ci == 0:
                        mm(P, psOT[base:base + 64, :], zer[:], masks[:, 0, :], True, False, [zer, masks], [psOT], **kwo)
                    mm(P, psOT[base:base + 64, cs], v_sb[:, cc, h * 64:(h + 1) * 64], A[:, cs], False, cc == 0, [v_sb, A], [psOT], **kwo)
                    if cc == 0:
                        P.op("act", lambda e: e.copy(out=oT[base:base + 64, p, Q * 512:(Q + 1) * 512], in_=psOT[base:base + 64, :]),
                             reads=[psOT], writes=[oT])

                n_u = len(units)
                stage1(0)
                for i in range(n_u):
                    if i + 1 < n_u:
                        stage1(i + 1)
                    stage2(i)
                    if i >= 1:
                        stage3(i - 1)
                stage3(n_u - 1)
            else:
                unitsc = [(h, t, ji, jj, len([x for x in range(5) if t - 4 + x >= 0]))
                          for h in range(16) for t in range(TPS) for ji, jj in enumerate([x for x in range(5) if t - 4 + x >= 0])]

                def cstage1(i):
                    h, t, ji, jj, nj = unitsc[i]
                    p, base = h // 2, (h % 2) * 64
                    kt = t - 4 + jj
                    pz, A = psZ[i % 3], Ab[i % 4]
                    if t == 0 and ji == 0:
                        va = vaug[h % 2]
                        P.op("pool", lambda e: e.tensor_copy(out=va[:, :, base:base + 64], in_=v_sb[:, :, h * 64:(h + 1) * 64]), reads=[v_sb, va], writes=[va])
                    mm(P, pz[:], kT[base:base + 64, p, kt * 128:(kt + 1) * 128], qT[base:base + 64, p, t * 128:(t + 1) * 128], True, True, [kT, qT], [pz])
                    if jj >= 3:
                        sb_ = ssb[i % 2]
                        P.op("dve", lambda e: e.tensor_tensor(out=sb_[:], in0=pz[:], in1=BM[:, h, jj - 3, :], op=ALU.add), reads=[pz, BM], writes=[sb_])
                        P.op("act", lambda e: e.activation(out=A[:], in_=sb_[:], func=AF.Exp), reads=[sb_], writes=[A])
                    elif jj == 0:
                        sb_ = ssb[i % 2]
                        P.op("act", lambda e: e.activation(out=sb_[:], in_=pz[:], func=AF.Exp, bias=c256[:, h:h + 1]), reads=[pz, c256], writes=[sb_])
                        P.op("dve", lambda e: e.tensor_tensor(out=A[:], in0=sb_[:], in1=mask0[:], op=ALU.mult), reads=[sb_, mask0], writes=[A])
                    else:
                        P.op("act", lambda e: e.activation(out=A[:], in_=pz[:], func=AF.Exp, bias=c256[:, h:h + 1]), reads=[pz, c256], writes=[A])

                def cstage2(i):
                    h, t, ji, jj, nj = unitsc[i]
                    p, base = h // 2, (h % 2) * 64
                    kt = t - 4 + jj
                    A = Ab[i % 4]
                    po = psOT[(h * TPS + t) % 2]
                    va = vaug[h % 2]
                    mm(P, po[:], va[:, kt, :], A[:], ji == 0, ji == nj - 1, [va, A], [po])
                    if ji == nj - 1:
                        ob = 64 - base
                        rc = rec[(h * TPS + t) % 2]
                        P.op("dve", lambda e: e.reciprocal(out=rc[ob:ob + 64, :], in_=po[ob:ob + 64, :]), reads=[po], writes=[rc])
                        P.op("dve", lambda e: e.tensor_copy(out=rc[base:base + 64, :], in_=rc[ob:ob + 64, :]), reads=[rc], writes=[rc])
                        P.op("dve", lambda e: e.tensor_tensor(out=oT[base:base + 64, p, t * 128:(t + 1) * 128], in0=po[base:base + 64, :],
                                                              in1=rc[base:base + 64, :], op=ALU.mult), reads=[po, rc], writes=[oT])

                n_c = len(unitsc)
                cstage1(0)
                cstage1(1)
                for i in range(n_c):
                    if i + 2 < n_c:
                        cstage1(i + 2)
                    cstage2(i)
            for half in range(2):
                P.dma("pool", wb[half][:], wov[:, :, half * 512:(half + 1) * 512], writes=[wb[half]])
            for t in range(TPS):
                gt = b * TPS + t
                ap, trk = xsrc(k, first, gt)
                xt = c.xt[c.cnt % 2]
                c.cnt += 1
                P.dma("sp", xt[:], ap, reads=[trk], writes=[xt])
                for half in range(2):
                    ps = psP[cnt[1] % 2]
                    cnt[1] += 1
                    for kk in range(8):
                        mm(P, ps[:], oT[:, kk, t * 128:(t + 1) * 128], wb[half][:, kk, :], kk == 0, kk == 7, [oT, wb[half]], [ps])
                    epilogue_half(P, k, c, xt, ps[:], ps, half)
                store_x(P, k, xt, gt)
        P.barrier_ops()
        P.flush()


GELU_FUNC = AF.Gelu_apprx_tanh


def phase_sg(P, k, layer):
    win = k.sg_win[0].rearrange("(k p) n -> p k n", p=128)
    wout = k.sg_wout[0].rearrange("(k p) n -> p k n", p=128)
    H3 = 3 * D
    with ExitStack() as st:
        c = alloc_common(P, st, k, hb2=True)
        hT = P.sbuf(st, "hT", [128, 8, 512], BF16)
        zb = P.sbuf(st, "zb", [128, 4, 2 * H3], BF16)
        yT = P.sbuf(st, "yT", [128, 24, 512], BF16)
        winb = [P.sbuf(st, f"winb{i}", [128, 8, 512], BF16) for i in range(2)]
        woutb = [P.sbuf(st, f"woutb{i}", [128, 24, 512], BF16) for i in range(2)]
        vgb = P.sbuf(st, "vgb", [128, H3], F32)
        vn = P.sbuf(st, "vn", [128, H3], BF16)
        yb = P.sbuf(st, "yb", [128, H3], BF16)
        wsr = P.sbuf(st, "wsr", [128, 8, 128], BF16)
        wsT = P.sbuf(st, "wsT", [128, 8, 128], BF16)
        bs = P.sbuf(st, "bs", [128, 8], F32)
        ssv = P.sbuf(st, "ssv", [128, 4, 8], F32)
        rs = P.sbuf(st, "rs", [128, 2], F32)
        psZ = [P.psum(st, f"psZ{i}", [128, 512], F32) for i in range(3)]
        psM = [P.psum(st, f"psM{i}", [128, 512], F32) for i in range(2)]
        psY = [P.psum(st, f"psY{i}", [128, 512], F32) for i in range(2)]
        P.dma("sp", vgb[:], k.sg_vg[0:1, :].partition_broadcast(128), writes=[vgb])
        P.dma("pool", wsr[:], k.sg_ws[0].rearrange("g i j -> i g j"), writes=[wsr])
        P.dma("sp", bs[:], k.sg_bs[0].rearrange("g i -> i g"), writes=[bs], allow_slow_non_contiguous=True)
        for g in range(8):
            P.op("pe", lambda e, g=g: e.transpose(out=c.psT[:, g * 128:(g + 1) * 128], in_=wsr[:, g, :], identity=c.ident[:]),
                 reads=[wsr, c.ident], writes=[c.psT])
        P.op("act", lambda e: e.copy(out=wsT[:], in_=c.psT[:].rearrange("p (g i) -> p g i", g=8)), reads=[c.psT], writes=[wsT])
        P.op("dve", lambda e: e.memset(wsT[64:128, :, 0:64], 0.0), reads=[wsT], writes=[wsT])
        cnt = [0, 0, 0, 0]
        cur_b = -1
        for blk in range(8):
            b = blk // 4
            if b != cur_b:
                load_mod(P, k, c, layer, 0, b)
                cur_b = b
            pend = []
            for tt in range(4):
                gt = blk * 4 + tt
                ap, trk = xsrc(k, False, gt)
                prologue_tile(P, k, c, ap, trk, hT, tt * 128, pend)
            flush_pend(pend)
            for n in range(12):
                w = winb[cnt[0] % 2]
                cnt[0] += 1
                P.dma("pool", w[:], win[:, :, n * 512:(n + 1) * 512], writes=[w])
                for tt in range(4):
                    ps = psZ[cnt[1] % 3]
                    cnt[1] += 1
                    for kk in range(8):
                        mm(P, ps[:], hT[:, kk, tt * 128:(tt + 1) * 128], w[:, kk, :], kk == 0, kk == 7, [hT, w], [ps])
                    P.op("act", lambda e, ps=ps, tt=tt, n=n: e.activation(out=zb[:, tt, n * 512:(n + 1) * 512], in_=ps[:], func=GELU_FUNC),
                         reads=[ps], writes=[zb])
                    if n >= 6:
                        P.op("act", lambda e, tt=tt, n=n: e.activation(out=c.junk[:, 0:512], in_=zb[:, tt, n * 512:(n + 1) * 512], func=AF.Square,
                                                                      accum_out=ssv[:, tt, n - 6:n - 5]), reads=[zb], writes=[c.junk, ssv])
            for tt in range(4):
                col = tt % 2
                P.op("dve", lambda e, tt=tt, col=col: e.tensor_reduce(out=rs[:, col:col + 1], in_=ssv[:, tt, 0:6], axis=AX.X, op=ALU.add),
                     reads=[ssv], writes=[rs])
                P.op("act", lambda e, col=col: e.activation(out=rs[:, col:col + 1], in_=rs[:, col:col + 1], func=AF.Sqrt, scale=1.0 / H3, bias=k.epsb[:, 0:1]),
                     reads=[rs, k.epsb], writes=[rs])
                P.op("dve", lambda e, col=col: e.reciprocal(out=rs[:, col:col + 1], in_=rs[:, col:col + 1]), reads=[rs], writes=[rs])
                P.op("dve", lambda e, tt=tt, col=col: e.scalar_tensor_tensor(out=vn[:], in0=zb[:, tt, H3:2 * H3], scalar=rs[:, col:col + 1], in1=vgb[:],
                                                                            op0=ALU.mult, op1=ALU.mult), reads=[zb, rs, vgb], writes=[vn])
                for g in range(8):
                    pm = psM[cnt[2] % 2]
                    cnt[2] += 1
                    mm(P, pm[:, 0:384], wsT[:, g, :], vn[:, g * 384:(g + 1) * 384], True, True, [wsT, vn], [pm])
                    P.op("dve", lambda e, pm=pm, g=g, tt=tt: e.scalar_tensor_tensor(out=yb[:, g * 384:(g + 1) * 384], in0=pm[:, 0:384], scalar=bs[:, g:g + 1],
                                                                                  in1=zb[:, tt, g * 384:(g + 1) * 384], op0=ALU.add, op1=ALU.mult),
                         reads=[pm, bs, zb], writes=[yb])
                for k0 in range(0, 24, 8):
                    for kk in range(8):
                        P.op("pe", lambda e, kk=kk, k0=k0: e.transpose(out=c.psT[:, kk * 128:(kk + 1) * 128], in_=yb[:, (k0 + kk) * 128:(k0 + kk + 1) * 128],
                                                                      identity=c.ident[:]), reads=[yb, c.ident], writes=[c.psT])
                    P.op("act", lambda e, k0=k0, tt=tt: e.copy(out=yT[:, k0:k0 + 8, tt * 128:(tt + 1) * 128], in_=c.psT[:].rearrange("p (k t) -> p k t", k=8)),
                         reads=[c.psT], writes=[yT])
            for half in range(2):
                P.dma("pool", woutb[half][:], wout[:, :, half * 512:(half + 1) * 512], writes=[woutb[half]])
            for tt in range(4):
                gt = blk * 4 + tt
                ap, trk = xsrc(k, False, gt)
                xt = c.xt[c.cnt % 2]
                c.cnt += 1
                P.dma("sp", xt[:], ap, reads=[trk], writes=[xt])
                for half in range(2):
                    ps = psY[cnt[3] % 2]
                    cnt[3] += 1
                    for kk in range(24):
                        mm(P, ps[:], yT[:, kk, tt * 128:(tt + 1) * 128], woutb[half][:, kk, :], kk == 0, kk == 23, [yT, woutb[half]], [ps])
                    epilogue_half(P, k, c, xt, ps[:], ps, half)
                store_x(P, k, xt, gt)
        P.barrier_ops()
        P.flush()


PHASES_ALL = ["mod", "a0", "f0", "a1", "r1", "f1", "a2", "f2", "a3", "r3", "f3"]


def build_program(phases=None):
    phases = PHASES_ALL if phases is None else phases
    nc = bass.Bass("TRN2", target_bir_lowering=False)
    k = K()
    T_ = D * 0 + NSEQ * SEQ

    def inp(name, shape):
        return nc.dram_tensor(name, list(shape), F32, kind="ExternalInput")

    k.x_in = inp("x", [T_, D])
    k.c = inp("c", [NSEQ, D])
    k.norm_g = inp("norm_g", [4, 2, D])
    k.ada_w = inp("ada_w", [4, D, 6 * D])
    k.ada_b = inp("ada_b", [4, 6 * D])
    k.sb_wqkv = inp("sb_wqkv", [2, D, 3 * D])
    k.sb_qg = inp("sb_qg", [2, 64])
    k.sb_kg = inp("sb_kg", [2, 64])
    k.sb_wo = inp("sb_wo", [2, D, D])
    k.sg_win = inp("sg_win", [1, D, 6 * D])
    k.sg_vg = inp("sg_vg", [1, 3 * D])
    k.sg_ws = inp("sg_ws", [1, 8, 128, 128])
    k.sg_bs = inp("sg_bs", [1, 8, 128])
    k.sg_wout = inp("sg_wout", [1, 3 * D, D])
    k.ca_wqkv = inp("ca_wqkv", [1, D, 3 * D])
    k.ca_qg = inp("ca_qg", [1, 64])
    k.ca_kg = inp("ca_kg", [1, 64])
    k.ca_relb = inp("ca_relb", [1, 16, 257])
    k.ca_wo = inp("ca_wo", [1, D, D])
    k.ff_w13 = inp("ff_w13", [2, D, 2 * DFF])
    k.ff_w2 = inp("ff_w2", [2, DFF, D])
    k.moe_wr = inp("moe_wrT", [2, NE, D])
    k.moe_br = inp("moe_br", [2, NE])
    k.moe_w1 = inp("moe_w1", [2, NE, D, DFE])
    k.moe_w3 = inp("moe_w3", [2, NE, D, DFE])
    k.moe_w2 = inp("moe_w2", [2, NE, DFE, D])
    k.y = nc.dram_tensor("y", [T_, D], F32, kind="ExternalOutput")
    k.modd = nc.dram_tensor("modd", [4, NSEQ, 6 * D], F32)
    k.gates_d = nc.dram_tensor("gates_d", [T_, NE], F32)
    k.Fd = nc.dram_tensor("Fd", [16, 128, 383], F32)
    k.modtrk = Trk("modtrk")
    k.gates_trk = Trk("gates_trk")
    k.Ftrk = Trk("Ftrk")
    k.xin_trk = Trk("xin")
    k.ytrk = [Trk(f"y{t}") for t in range(NSEQ * TPS)]
    with ExitStack() as st:
        P = Prog(nc, st)
        for ph in phases:
            if ph == "mod":
                phase_mod(P, k)
            elif ph == "cp":
                for gt in range(NSEQ * TPS):
                    P.dma("sp", k.y[gt * 128:(gt + 1) * 128, :], k.x_in[gt * 128:(gt + 1) * 128, :], writes=[k.ytrk[gt]], key=f"yst{gt % 4}")
                P.barrier_ops()
                P.flush()
            elif ph[0] == "a":
                layer = int(ph[1])
                kind = ["sb", "sg", "ca"][layer % 3]
                if kind == "sg":
                    phase_sg(P, k, layer)
                else:
                    phase_attn(P, k, layer, kind)
            elif ph[0] == "r":
                phase_router(P, k, int(ph[1]))
            elif ph[0] == "f":
                layer = int(ph[1])
                phase_ffn(P, k, layer, layer % 2 == 1)
    return nc


INPUT_NAMES = ["x", "c", "norm_g", "ada_w", "ada_b", "sb_wqkv", "sb_qg", "sb_kg", "sb_wo", "sg_win", "sg_vg", "sg_ws", "sg_bs",
               "sg_wout", "ca_wqkv", "ca_qg", "ca_kg", "ca_relb", "ca_wo", "ff_w13", "ff_w2", "moe_wr", "moe_br",
               "moe_w1", "moe_w3", "moe_w2"]


def make_in_maps(inputs, n_cores=8):
    f = lambda a: np.ascontiguousarray(np.asarray(a, dtype=np.float32))
    shared = {}
    for name in INPUT_NAMES:
        if name in ("x", "c"):
            continue
        if name == "moe_wr":
            shared["moe_wrT"] = f(np.asarray(inputs[name]).transpose(0, 2, 1))
        else:
            shared[name] = f(inputs[name])
    x = np.asarray(inputs["x"], dtype=np.float32)
    c = np.asarray(inputs["c"], dtype=np.float32)
    maps = []
    for i in range(n_cores):
        m = dict(shared)
        m["x"] = f(x[NSEQ * i:NSEQ * (i + 1)].reshape(NSEQ * SEQ, D))
        m["c"] = f(c[NSEQ * i:NSEQ * (i + 1)])
        maps.append(m)
    return maps


def kernel(**inputs):
    nc = build_program()
    maps = make_in_maps(inputs, 8)
    res = run_bass_kernel_spmd(nc, maps, core_ids=list(range(8)))
    out = np.stack([np.asarray(r["y"], dtype=np.float32).reshape(NSEQ, SEQ, D) for r in res.results], axis=0)
    return out.reshape(8 * NSEQ, SEQ, D)
```

```python
import numpy as np
import concourse.bass as bass
import concourse.mybir as mybir
from concourse.bass_utils import run_bass_kernel_spmd
from contextlib import ExitStack

F32 = mybir.dt.float32
BF16 = mybir.dt.bfloat16
AF = mybir.ActivationFunctionType
ALU = mybir.AluOpType
AX = mybir.AxisListType

ENGS = ("pe", "act", "dve", "pool", "sp")
SAME_ENGINE_SYNC = True


class Trk:
    __slots__ = ("w", "r", "name")

    def __init__(self, name=""):
        self.w = None
        self.r = []
        self.name = name


class T:
    def __init__(self, h, name):
        self.h = h
        self.trk = Trk(name)

    def __getitem__(self, k):
        return self.h[k]


class Op:
    __slots__ = ("eng", "fn", "waits", "kind", "key", "val", "needed", "idx")


class Prog:
    def __init__(self, nc, stack):
        self.nc = nc
        self.stack = stack
        self.sems = {e: stack.enter_context(nc.semaphore("s_" + e)) for e in ENGS}
        self.dma_sems = {}
        self.ops = {e: [] for e in ENGS}
        self.nops = {e: 0 for e in ENGS}
        self.allops = {e: [] for e in ENGS}
        self.seen = {e: {} for e in ENGS}
        self.inc_count = {e: 0 for e in ENGS}
        self.uid = 0
        self.last_c = {}

    def sbuf(self, st, name, shape, dtype):
        self.uid += 1
        h = st.enter_context(self.nc.sbuf_tensor(f"{name}_{self.uid}", list(shape), dtype))
        return T(h, name)

    def psum(self, st, name, shape, dtype=F32):
        self.uid += 1
        h = st.enter_context(self.nc.psum_tensor(f"{name}_{self.uid}", list(shape), dtype))
        return T(h, name)

    def _deps(self, reads, writes):
        evs = []
        for t in reads:
            trk = t.trk if isinstance(t, T) else t
            if trk.w is not None:
                evs.append(trk.w)
        for t in writes:
            trk = t.trk if isinstance(t, T) else t
            if trk.w is not None:
                evs.append(trk.w)
            evs.extend(trk.r)
        return evs

    def _mkwaits(self, eng, evs, pe_accum=False, force_self=False):
        waits = []
        seen = self.seen[eng]
        for ev in evs:
            kind, src, v = ev[0], ev[1], ev[2]
            if kind == "eng":
                if src == eng and not force_self and (not SAME_ENGINE_SYNC or pe_accum or eng == "pe"):
                    continue
                if seen.get(("e", src), -1) >= v:
                    continue
                seen[("e", src)] = v
                waits.append(ev)
                self.allops[src][v].needed = True
            else:
                if seen.get(("d", src), -1) >= v:
                    continue
                seen[("d", src)] = v
                waits.append(ev)
        return waits

    def _update(self, ev, reads, writes):
        for t in writes:
            trk = t.trk if isinstance(t, T) else t
            trk.w = ev
            trk.r = []
        for t in reads:
            trk = t.trk if isinstance(t, T) else t
            if trk in [(x.trk if isinstance(x, T) else x) for x in writes]:
                continue
            trk.r.append(ev)
            if len(trk.r) > 64:
                last = {}
                for e in trk.r:
                    k = (e[0], e[1])
                    if k not in last or last[k][2] < e[2]:
                        last[k] = e
                trk.r = list(last.values())

    def op(self, eng, fn, reads=(), writes=(), accum=False, noself=False):
        o = Op()
        o.eng = eng
        o.fn = fn
        o.kind = "c"
        o.needed = False
        o.idx = self.nops[eng]
        evs_ = self._deps(reads, writes)
        if noself:
            evs_ = [ev for ev in evs_ if not (ev[0] == "eng" and ev[1] == eng)]
        o.waits = self._mkwaits(eng, evs_, pe_accum=accum)
        self.nops[eng] += 1
        self.ops[eng].append(o)
        self.allops[eng].append(o)
        ev = ("eng", eng, o.idx)
        self.last_c[eng] = o.idx
        self._update(ev, reads, writes)
        return o

    def dma(self, q, out_ap, in_ap, reads=(), writes=(), key=None, **kw):
        if key is None:
            w0 = writes[0]
            key = (w0.trk if isinstance(w0, T) else w0).name
        if key not in self.dma_sems:
            self.dma_sems[key] = [self.stack.enter_context(self.nc.semaphore("d_" + key)), 0]
        ent = self.dma_sems[key]
        ent[1] += 16
        val = ent[1]
        o = Op()
        o.eng = q
        o.kind = "d"
        o.key = key
        o.val = val
        o.needed = False
        o.idx = self.nops[q]
        sem = ent[0]
        o.fn = lambda e: e.dma_start(out=out_ap, in_=in_ap, **kw).then_inc(sem, 16)
        evs = self._deps(reads, writes)
        if val > 16:
            evs.append(("dma", key, val - 16))
        o.waits = self._mkwaits(q, evs)
        self.nops[q] += 1
        self.ops[q].append(o)
        self.allops[q].append(o)
        ev = ("dma", key, val)
        self._update(ev, reads, writes)
        return o

    def _emit_engine(self, eng, e, ops, incvals):
        sem_self = self.sems[eng]
        for o in ops:
            for ev in o.waits:
                if ev[0] == "eng":
                    e.wait_ge(self.sems[ev[1]], incvals[ev[1]][ev[2]])
                else:
                    e.wait_ge(self.dma_sems[ev[1]][0], ev[2])
            if o.fn is None:
                continue
            ins = o.fn(e)
            if o.kind == "c" and o.needed:
                ins.then_inc(sem_self, 1)

    def flush(self):
        incvals = getattr(self, "_incvals", None)
        if incvals is None:
            incvals = {e: [] for e in ENGS}
            self._incvals = incvals
            self._inc_run = {e: 0 for e in ENGS}
        for eng in ENGS:
            lst = self.allops[eng]
            iv = incvals[eng]
            run = self._inc_run[eng]
            for i in range(len(iv), len(lst)):
                o = lst[i]
                if o.kind == "c" and o.needed:
                    run += 1
                iv.append(run)
            self._inc_run[eng] = run
        ops = self.ops
        self.ops = {e: [] for e in ENGS}
        prog = self
        with self.nc.Block() as block:
            def mk(eng):
                def body(e):
                    prog._emit_engine(eng, e, ops[eng], incvals)
                return body

            block.tensor(mk("pe"))
            block.scalar(mk("act"))
            block.vector(mk("dve"))
            block.gpsimd(mk("pool"))
            block.sync(mk("sp"))

    def barrier_ops(self):
        last = {}
        for eng in ENGS:
            lc = self.last_c.get(eng)
            if lc is not None:
                last[eng] = ("eng", eng, lc)
        dmas = [("dma", k, v[1]) for k, v in self.dma_sems.items() if v[1] > 0]
        for eng in ENGS:
            evs = [ev for s_, ev in last.items()] + dmas
            waits = self._mkwaits(eng, evs, force_self=True)
            if not waits:
                continue
            o = Op()
            o.eng = eng
            o.kind = "b"
            o.needed = False
            o.idx = self.nops[eng]
            o.waits = waits
            o.fn = None
            self.nops[eng] += 1
            self.ops[eng].append(o)
            self.allops[eng].append(o)


D = 1024
SEQ = 2048
NSEQ = 2
TPS = 16
DFF = 2816
DFE = 3584
NE = 8
EPS = 1e-6


class K:
    pass


def mm(P, out_ap, lhsT, rhs, start, stop, reads, writes, **kw):
    P.op("pe", lambda e: e.matmul(out_ap, lhsT=lhsT, rhs=rhs, start=start, stop=stop, **kw),
         reads=reads, writes=writes)


def alloc_common(P, st, k, hb2=False):
    c = K()
    c.ident = P.sbuf(st, "ident", [128, 128], BF16)
    P.op("pool", lambda e: e.memset(c.ident[:], 1.0), writes=[c.ident])
    P.op("pool", lambda e: e.affine_select(out=c.ident[:], in_=c.ident[:], pattern=[[-1, 128]],
                                           compare_op=ALU.is_equal, fill=0.0, base=0, channel_multiplier=1),
         reads=[c.ident], writes=[c.ident])
    c.xt = [P.sbuf(st, f"xt{j}", [128, D], F32) for j in range(2)]
    c.junk = P.sbuf(st, "junk", [128, D], BF16)
    c.t1 = P.sbuf(st, "t1", [128, D], F32)
    c.hb = P.sbuf(st, "hb", [128, D], BF16)
    c.hbs = [c.hb] + ([P.sbuf(st, "hb2", [128, D], BF16)] if hb2 else [])
    c.hbcnt = 0
    c.ssq = P.sbuf(st, "ssq", [128, 2], F32)
    c.rstd = P.sbuf(st, "rstd", [128, 2], F32)
    c.A = P.sbuf(st, "modA", [128, D], F32)
    c.sh = P.sbuf(st, "modsh", [128, D], F32)
    c.g = P.sbuf(st, "modg", [128, D], F32)
    c.psT = P.psum(st, "psT", [128, D], BF16)
    k.epsb = P.sbuf(st, "epsb", [128, 1], F32)
    P.op("pool", lambda e: e.memset(k.epsb[:], EPS), writes=[k.epsb])
    c.cnt = 0
    return c


def load_mod(P, k, c, layer, sub, b):
    o = 3 * sub * D
    P.dma("sp", c.sh[:], k.modd[layer, b:b + 1, o:o + D].partition_broadcast(128), reads=[k.modtrk], writes=[c.sh])
    P.dma("sp", c.t1[:], k.modd[layer, b:b + 1, o + D:o + 2 * D].partition_broadcast(128), reads=[k.modtrk], writes=[c.t1])
    P.dma("sp", c.g[:], k.modd[layer, b:b + 1, o + 2 * D:o + 3 * D].partition_broadcast(128), reads=[k.modtrk], writes=[c.g])
    P.dma("sp", c.A[:], k.norm_g[layer, sub:sub + 1, :].partition_broadcast(128), writes=[c.A])
    P.op("dve", lambda e: e.tensor_tensor(out=c.A[:], in0=c.A[:], in1=c.t1[:], op=ALU.mult),
         reads=[c.A, c.t1], writes=[c.A])


def norm_mod(P, k, c, src_ap, src_trk, out_tile, out_ap):
    xt = c.xt[c.cnt % 2]
    col = c.cnt % 2
    c.cnt += 1
    P.dma("sp", xt[:], src_ap, reads=[src_trk], writes=[xt])
    P.op("act", lambda e: e.activation(out=c.junk[:], in_=xt[:], func=AF.Square, accum_out=c.ssq[:, col:col + 1]),
         reads=[xt], writes=[c.junk, c.ssq])
    P.op("act", lambda e: e.activation(out=c.rstd[:, col:col + 1], in_=c.ssq[:, col:col + 1], func=AF.Sqrt, scale=1.0 / D, bias=k.epsb[:, 0:1]),
         reads=[c.ssq, k.epsb], writes=[c.rstd])
    P.op("dve", lambda e: e.reciprocal(out=c.rstd[:, col:col + 1], in_=c.rstd[:, col:col + 1]), reads=[c.rstd], writes=[c.rstd])
    P.op("dve", lambda e: e.scalar_tensor_tensor(out=c.t1[:], in0=xt[:], scalar=c.rstd[:, col:col + 1], in1=c.A[:],
                                                 op0=ALU.mult, op1=ALU.mult), reads=[xt, c.rstd, c.A], writes=[c.t1])
    P.op("dve", lambda e: e.tensor_tensor(out=out_ap, in0=c.t1[:], in1=c.sh[:], op=ALU.add),
         reads=[c.t1, c.sh], writes=[out_tile])
    return xt


def prologue_tile(P, k, c, src_ap, src_trk, hT, col0, pend=None):
    hb = c.hbs[c.hbcnt % len(c.hbs)]
    c.hbcnt += 1
    norm_mod(P, k, c, src_ap, src_trk, hb, hb[:])

    def stageB():
        for kk in range(8):
            P.op("pe", lambda e, kk=kk: e.transpose(out=c.psT[:, kk * 128:(kk + 1) * 128], in_=hb[:, kk * 128:(kk + 1) * 128],
                                                    identity=c.ident[:]), reads=[hb, c.ident], writes=[c.psT])
        P.op("act", lambda e: e.copy(out=hT[:, :, col0:col0 + 128], in_=c.psT[:].rearrange("p (k t) -> p k t", k=8)),
             reads=[c.psT], writes=[hT])

    if pend is None or len(c.hbs) < 2:
        stageB()
    else:
        if pend:
            pend.pop(0)()
        pend.append(stageB)


def flush_pend(pend):
    while pend:
        pend.pop(0)()


def epilogue_half(P, k, c, xt, y_ap, y_trk, half):
    hs = slice(half * 512, (half + 1) * 512)
    P.op("dve", lambda e: e.tensor_tensor(out=c.t1[:, hs], in0=y_ap, in1=c.g[:, hs], op=ALU.mult),
         reads=[y_trk, c.g], writes=[c.t1])
    P.op("pool", lambda e: e.tensor_tensor(out=xt[:, hs], in0=c.t1[:, hs], in1=xt[:, hs], op=ALU.add),
         reads=[c.t1, xt], writes=[xt])


def xsrc(k, first, gt):
    if first:
        return k.x_in[gt * 128:(gt + 1) * 128, :], k.xin_trk
    return k.y[gt * 128:(gt + 1) * 128, :], k.ytrk[gt]


def store_x(P, k, xt, gt):
    P.dma("sp", k.y[gt * 128:(gt + 1) * 128, :], xt[:], reads=[xt], writes=[k.ytrk[gt]], key=f"yst{gt % 4}")


def phase_mod(P, k):
    with ExitStack() as st:
        cT = P.sbuf(st, "cT", [128, 8, 2], F32)
        condT = P.sbuf(st, "condT", [128, 8, 2], F32)
        wbuf = [P.sbuf(st, f"adaw{j}", [128, 8, 512], F32) for j in range(2)]
        bias2 = P.sbuf(st, "bias2", [2, 6 * D], F32)
        modsb = P.sbuf(st, "modsb", [2, 6 * D], F32)
        ps = [P.psum(st, f"psm{j}", [128, 512], F32) for j in range(2)]
        for b in range(2):
            P.dma("sp", cT[:, :, b], k.c[b].rearrange("(k p) -> p k", p=128), writes=[cT], allow_slow_non_contiguous=True)
        P.op("act", lambda e: e.activation(out=condT[:], in_=cT[:], func=AF.Silu), reads=[cT], writes=[condT])
        it = 0
        for i in range(4):
            P.dma("sp", bias2[:], k.ada_b[i:i + 1, :].partition_broadcast(2), writes=[bias2])
            wv = k.ada_w[i].rearrange("(k p) n -> p k n", p=128)
            for n in range(12):
                wb = wbuf[it % 2]
                pst = ps[it % 2]
                it += 1
                P.dma("sp", wb[:], wv[:, :, n * 512:(n + 1) * 512], writes=[wb])
                for kk in range(8):
                    mm(P, pst[0:2, :], condT[:, kk, :], wb[:, kk, :], kk == 0, kk == 7, [condT, wb], [pst])
                P.op("dve", lambda e, n=n, pst=pst: e.tensor_tensor(out=modsb[:, n * 512:(n + 1) * 512], in0=pst[0:2, :],
                                                                   in1=bias2[:, n * 512:(n + 1) * 512], op=ALU.add),
                     reads=[pst, bias2], writes=[modsb])
            for o in (D, 4 * D):
                P.op("dve", lambda e, o=o: e.tensor_scalar_add(out=modsb[:, o:o + D], in0=modsb[:, o:o + D], scalar1=1.0),
                     reads=[modsb], writes=[modsb])
            P.dma("sp", k.modd[i], modsb[:], reads=[modsb], writes=[k.modtrk], key="modst")
        P.barrier_ops()
        P.flush()


def ffn_body(P, k, c, b, hT, acc, actb, w13b, w2b, psA, psO, wa_ap, wg_ap, w2_ap, F, G, first_acc, gate_ap, st_cnt):
    nch = F // 128
    wav = wa_ap.rearrange("(k p) f -> p k f", p=128)
    wgv = wg_ap.rearrange("(k p) f -> p k f", p=128)
    w2v = w2_ap.rearrange("(c p) n -> p c n", p=128)
    c0 = 0
    while c0 < nch:
        g = min(G, nch - c0)
        w13 = w13b[st_cnt[0] % 2]
        w2 = w2b[st_cnt[0] % 2]
        st_cnt[0] += 1
        P.dma("pool", w13[:, :, 0, 0:g * 128], wav[:, :, c0 * 128:(c0 + g) * 128], writes=[w13])
        P.dma("pool", w13[:, :, 1, 0:g * 128], wgv[:, :, c0 * 128:(c0 + g) * 128], writes=[w13])
        P.dma("pool", w2[:, 0:g, :], w2v[:, c0:c0 + g, :], writes=[w2])
        for cc in range(g):
            for tt in range(4):
                pa = psA[st_cnt[1] % 4]
                pg = psA[(st_cnt[1] + 1) % 4]
                st_cnt[1] += 2
                for kk in range(8):
                    mm(P, pa[:], w13[:, kk, 0, cc * 128:(cc + 1) * 128], hT[:, kk, tt * 512:(tt + 1) * 512], kk == 0, kk == 7, [w13, hT], [pa])
                for kk in range(8):
                    mm(P, pg[:], w13[:, kk, 1, cc * 128:(cc + 1) * 128], hT[:, kk, tt * 512:(tt + 1) * 512], kk == 0, kk == 7, [w13, hT], [pg])
                sa = k.sa[st_cnt[1] // 2 % 2]
                P.op("act", lambda e, sa=sa, pa=pa: e.activation(out=sa[:], in_=pa[:], func=AF.Silu), reads=[pa], writes=[sa])
                P.op("dve", lambda e, sa=sa, pg=pg, cc=cc, tt=tt: e.tensor_tensor(out=actb[:, cc, tt * 512:(tt + 1) * 512], in0=pg[:], in1=sa[:],
                                                                               op=ALU.mult), reads=[pg, sa], writes=[k.act_trk[cc][tt]])
        for t in range(TPS):
            for half in range(2):
                po = psO[st_cnt[2] % 3]
                st_cnt[2] += 1
                for cc in range(g):
                    mm(P, po[:], actb[:, cc, t * 128:(t + 1) * 128], w2[:, cc, half * 512:(half + 1) * 512], cc == 0, cc == g - 1, [k.act_trk[cc][t // 4], w2], [po])
                dst = acc[:, t, half * 512:(half + 1) * 512]
                if first_acc and c0 == 0:
                    if gate_ap is None:
                        P.op("dve", lambda e, dst=dst, po=po: e.tensor_copy(out=dst, in_=po[:]), reads=[po], writes=[k.acc_trk[t][half]])
                    else:
                        P.op("dve", lambda e, dst=dst, po=po, t=t: e.tensor_scalar(out=dst, in0=po[:], scalar1=gate_ap(t), scalar2=None, op0=ALU.mult),
                             reads=[po, k.gates_sb], writes=[k.acc_trk[t][half]])
                else:
                    if gate_ap is None:
                        P.op("dve", lambda e, dst=dst, po=po: e.tensor_tensor(out=dst, in0=po[:], in1=dst, op=ALU.add), reads=[po, k.acc_trk[t][half]], writes=[k.acc_trk[t][half]])
                    else:
                        P.op("dve", lambda e, dst=dst, po=po, t=t: e.scalar_tensor_tensor(out=dst, in0=po[:], scalar=gate_ap(t), in1=dst,
                                                                                      op0=ALU.mult, op1=ALU.add),
                             reads=[po, k.acc_trk[t][half], k.gates_sb], writes=[k.acc_trk[t][half]])
        c0 += g


def phase_ffn(P, k, layer, moe):
    first = False
    m = layer // 2
    G = 4
    with ExitStack() as st:
        c = alloc_common(P, st, k, hb2=True)
        hT = P.sbuf(st, "hT", [128, 8, SEQ], BF16)
        acc = P.sbuf(st, "acc", [128, TPS, D], F32)
        k.acc_trk = [[Trk(f"acc{t}_{h}") for h in range(2)] for t in range(TPS)]
        k.act_trk = [[Trk(f"act{cc}_{tt}") for tt in range(4)] for cc in range(G)]
        actb = P.sbuf(st, "actb", [128, G, SEQ], BF16)
        w13b = [P.sbuf(st, f"w13b{j}", [128, 8, 2, G * 128], BF16) for j in range(2)]
        w2b = [P.sbuf(st, f"w2b{j}", [128, G, D], BF16) for j in range(2)]
        k.sa = [P.sbuf(st, f"sa{j}", [128, 512], F32) for j in range(2)]
        psA = [P.psum(st, f"psA{j}", [128, 512], F32) for j in range(4)]
        psO = [P.psum(st, f"psO{j}", [128, 512], F32) for j in range(3)]
        k.gates_sb = P.sbuf(st, "gates_sb", [128, TPS, NE], F32)
        st_cnt = [0, 0, 0]
        for b in range(NSEQ):
            load_mod(P, k, c, layer, 1, b)
            pend = []
            for t in range(TPS):
                gt = b * TPS + t
                ap, trk = xsrc(k, first, gt)
                prologue_tile(P, k, c, ap, trk, hT, t * 128, pend)
            flush_pend(pend)
            if not moe:
                ffn_body(P, k, c, b, hT, acc, actb, w13b, w2b, psA, psO,
                         k.ff_w13[m][:, 0:DFF], k.ff_w13[m][:, DFF:2 * DFF], k.ff_w2[m], DFF, G, True, None, st_cnt)
            else:
                P.dma("sp", k.gates_sb[:], k.gates_d[b * SEQ:(b + 1) * SEQ, :].rearrange("(t p) e -> p t e", p=128),
                      reads=[k.gates_trk], writes=[k.gates_sb])
                for ex in range(NE):
                    ffn_body(P, k, c, b, hT, acc, actb, w13b, w2b, psA, psO,
                             k.moe_w1[m, ex], k.moe_w3[m, ex], k.moe_w2[m, ex], DFE, G, ex == 0,
                             (lambda t, ex=ex: k.gates_sb[:, t, ex:ex + 1]), st_cnt)
            for t in range(TPS):
                gt = b * TPS + t
                ap, trk = xsrc(k, first, gt)
                xt = c.xt[c.cnt % 2]
                c.cnt += 1
                P.dma("sp", xt[:], ap, reads=[trk], writes=[xt])
                for half in range(2):
                    epilogue_half(P, k, c, xt, acc[:, t, half * 512:(half + 1) * 512], k.acc_trk[t][half], half)
                store_x(P, k, xt, gt)
        P.barrier_ops()
        P.flush()


def phase_router(P, k, layer):
    m = layer // 2
    with ExitStack() as st:
        c = alloc_common(P, st, k)
        wr = P.sbuf(st, "wr", [128, NE, D], F32)
        br = P.sbuf(st, "br", [128, TPS, NE], F32)
        hfs = [P.sbuf(st, f"hf{i}", [128, D], F32) for i in range(2)]
        jk = P.sbuf(st, "jk", [128, D], F32)
        lg = P.sbuf(st, "lg", [128, TPS, NE], F32)
        v1 = P.sbuf(st, "v1", [128, TPS], F32)
        v2 = P.sbuf(st, "v2", [128, TPS], F32)
        ee = P.sbuf(st, "ee", [128, TPS], F32)
        w1 = P.sbuf(st, "w1", [128, TPS], F32)
        w2 = P.sbuf(st, "w2", [128, TPS], F32)
        m1 = P.sbuf(st, "m1", [128, TPS, NE], F32)
        m2 = P.sbuf(st, "m2", [128, TPS, NE], F32)
        l2 = P.sbuf(st, "l2", [128, TPS, NE], F32)
        gt_sb = P.sbuf(st, "gt_sb", [128, TPS, NE], F32)
        for ex in range(NE):
            P.dma("sp", wr[:, ex, :], k.moe_wr[m, ex:ex + 1, :].partition_broadcast(128), writes=[wr])
        for t in range(TPS):
            P.dma("sp", br[:, t, :], k.moe_br[m:m + 1, :].partition_broadcast(128), writes=[br])
        bc = lambda ap: ap.unsqueeze(2).to_broadcast([128, TPS, NE])
        for b in range(NSEQ):
            load_mod(P, k, c, layer, 1, b)
            for t in range(TPS):
                gt = b * TPS + t
                ap, trk = xsrc(k, False, gt)
                hf = hfs[t % 2]
                norm_mod(P, k, c, ap, trk, hf, hf[:])
                for ex in range(NE):
                    P.op("dve", lambda e, ex=ex, hf=hf, t=t: e.scalar_tensor_tensor(out=jk[:], in0=hf[:], scalar=1.0, in1=wr[:, ex, :], op0=ALU.mult, op1=ALU.mult,
                                                                                 accum_out=lg[:, t, ex:ex + 1]), reads=[hf, wr], writes=[jk, lg])
            P.op("dve", lambda e: e.tensor_tensor(out=lg[:], in0=lg[:], in1=br[:], op=ALU.add), reads=[lg, br], writes=[lg])
            P.op("dve", lambda e: e.tensor_reduce(out=v1[:], in_=lg[:], axis=AX.X, op=ALU.max), reads=[lg], writes=[v1])
            P.op("dve", lambda e: e.tensor_tensor(out=m1[:], in0=lg[:], in1=bc(v1[:, :]), op=ALU.is_equal), reads=[lg, v1], writes=[m1])
            P.op("dve", lambda e: e.scalar_tensor_tensor(out=l2[:], in0=m1[:], scalar=-1e30, in1=lg[:], op0=ALU.mult, op1=ALU.add),
                 reads=[m1, lg], writes=[l2])
            P.op("dve", lambda e: e.tensor_reduce(out=v2[:], in_=l2[:], axis=AX.X, op=ALU.max), reads=[l2], writes=[v2])
            P.op("dve", lambda e: e.tensor_tensor(out=m2[:], in0=l2[:], in1=bc(v2[:, :]), op=ALU.is_equal), reads=[l2, v2], writes=[m2])
            P.op("dve", lambda e: e.tensor_tensor(out=ee[:], in0=v2[:], in1=v1[:], op=ALU.subtract), reads=[v1, v2], writes=[ee])
            P.op("act", lambda e: e.activation(out=ee[:], in_=ee[:], func=AF.Exp), reads=[ee], writes=[ee])
            P.op("dve", lambda e: e.tensor_scalar_add(out=w1[:], in0=ee[:], scalar1=1.0), reads=[ee], writes=[w1])
            P.op("dve", lambda e: e.reciprocal(out=w1[:], in_=w1[:]), reads=[w1], writes=[w1])
            P.op("dve", lambda e: e.tensor_tensor(out=w2[:], in0=ee[:], in1=w1[:], op=ALU.mult), reads=[ee, w1], writes=[w2])
            P.op("dve", lambda e: e.tensor_tensor(out=m1[:], in0=m1[:], in1=bc(w1[:, :]), op=ALU.mult), reads=[m1, w1], writes=[m1])
            P.op("dve", lambda e: e.tensor_tensor(out=m2[:], in0=m2[:], in1=bc(w2[:, :]), op=ALU.mult), reads=[m2, w2], writes=[m2])
            P.op("dve", lambda e: e.tensor_tensor(out=gt_sb[:], in0=m1[:], in1=m2[:], op=ALU.add), reads=[m1, m2], writes=[gt_sb])
            P.dma("sp", k.gates_d[b * SEQ:(b + 1) * SEQ, :].rearrange("(t p) e -> p t e", p=128), gt_sb[:], reads=[gt_sb], writes=[k.gates_trk], key="gst")
        P.barrier_ops()
        P.flush()


def phase_attn(P, k, layer, kind):
    first = (layer == 0)
    sbk = (kind == "sb")
    j = layer // 3
    if sbk:
        wqkv, qg, kg, wo = k.sb_wqkv[j], k.sb_qg, k.sb_kg, k.sb_wo[j]
    else:
        wqkv, qg, kg, wo = k.ca_wqkv[0], k.ca_qg, k.ca_kg, k.ca_wo[0]
    wqv = wqkv.rearrange("(k p) n -> p k n", p=128)
    wov = wo.rearrange("(k p) n -> p k n", p=128)
    with ExitStack() as st:
        c = alloc_common(P, st, k)
        hT = P.sbuf(st, "hT", [128, 8, SEQ], BF16)
        oT = hT
        qT = P.sbuf(st, "qT", [128, 8, SEQ], BF16)
        kT = P.sbuf(st, "kT", [128, 8, SEQ], BF16)
        v_sb = P.sbuf(st, "v_sb", [128, TPS, D], BF16)
        wb = [P.sbuf(st, f"wb{i}", [128, 8, 512], BF16) for i in range(2)]
        gq = P.sbuf(st, "gq", [128, 512], F32)
        gk = P.sbuf(st, "gk", [128, 512], F32)
        sq_ = [P.sbuf(st, f"sq{i}", [128, 512], F32) for i in range(2)]
        tmp_ = [P.sbuf(st, f"tmp{i}", [128, 512], F32) for i in range(2)]
        qn_ = [P.sbuf(st, f"qn{i}", [128, 512], BF16) for i in range(2)]
        ssqh_ = [P.sbuf(st, f"ssqh{i}", [128, 8], F32) for i in range(2)]
        rsth_ = [P.sbuf(st, f"rsth{i}", [128, 8], F32) for i in range(2)]
        psP = [P.psum(st, f"psP{i}", [128, 512], F32) for i in range(2)]
        P.dma("sp", gq[:].rearrange("p (h d) -> p h d", h=8), bass.AP(qg, j * 64, [[0, 128], [0, 8], [1, 64]]), writes=[gq])
        P.dma("sp", gk[:].rearrange("p (h d) -> p h d", h=8), bass.AP(kg, j * 64, [[0, 128], [0, 8], [1, 64]]), writes=[gk])
        P.op("dve", lambda e: e.tensor_scalar_mul(out=gq[:], in0=gq[:], scalar1=0.125), reads=[gq], writes=[gq])
        if sbk:
            esb = [P.sbuf(st, f"esb{i}", [128, 512], F32) for i in range(2)]
            spb = [P.sbuf(st, f"spb{i}", [128, 512], BF16) for i in range(2)]
            Lbb = [P.sbuf(st, f"Lb{i}", [128, 512], BF16) for i in range(2)]
            efb = [P.sbuf(st, f"efb{i}", [128, 512], F32) for i in range(2)]
            Ab = [P.sbuf(st, f"Ab{i}", [128, 512], BF16) for i in range(3)]
            masks = P.sbuf(st, "masks", [128, 4, 512], BF16)
            negtri = P.sbuf(st, "negtri", [128, 128], BF16)
            negones = P.sbuf(st, "negones", [128, 128], BF16)
            zer = P.sbuf(st, "zer", [128, 64], BF16)
            psZ = [P.psum(st, f"psZ{i}", [128, 512], F32) for i in range(4)]
            psOT = P.psum(st, "psOT", [128, 512], F32)
            P.op("pool", lambda e: e.memset(masks[:], 1.0), writes=[masks])
            for d in range(4):
                P.op("pool", lambda e, d=d: e.affine_select(out=masks[:, d, :], in_=masks[:, d, :], pattern=[[1, 512]],
                                                           compare_op=ALU.is_gt, fill=0.0, base=-128 * d, channel_multiplier=-1),
                     reads=[masks], writes=[masks])
            P.op("pool", lambda e: e.memset(negtri[:], -1.0), writes=[negtri])
            P.op("pool", lambda e: e.affine_select(out=negtri[:], in_=negtri[:], pattern=[[-1, 128]], compare_op=ALU.is_ge, fill=0.0,
                                                   base=0, channel_multiplier=1), reads=[negtri], writes=[negtri])
            P.op("pool", lambda e: e.memset(negones[:], -1.0), writes=[negones])
            P.op("pool", lambda e: e.memset(zer[:], 0.0), writes=[zer])
        else:
            BM = P.sbuf(st, "BM", [128, 16, 2, 128], BF16)
            mask0 = P.sbuf(st, "mask0", [128, 128], F32)
            c256 = P.sbuf(st, "c256", [128, 16], F32)
            Esb = P.sbuf(st, "Esb", [16, 384], F32)
            ssb = [P.sbuf(st, f"ssb{i}", [128, 128], F32) for i in range(2)]
            Ab = [P.sbuf(st, f"Ab{i}", [128, 128], BF16) for i in range(4)]
            vaug = [P.sbuf(st, f"vaug{a}", [128, TPS, 128], BF16) for a in range(2)]
            rec = [P.sbuf(st, f"rec{i}", [128, 128], F32) for i in range(2)]
            psZ = [P.psum(st, f"psZ{i}", [128, 128], F32) for i in range(3)]
            psOT = [P.psum(st, f"psOT{i}", [128, 128], F32) for i in range(2)]
            P.dma("sp", Esb[:, 0:256], k.ca_relb[0, :, 1:257], writes=[Esb])
            P.op("dve", lambda e: e.tensor_copy(out=Esb[:, 256:384], in_=Esb[:, 255:256].to_broadcast([16, 128])), reads=[Esb], writes=[Esb])
            P.dma("sp", k.Fd[:, :, :], Esb[:, 0:383].unsqueeze(1).to_broadcast([16, 128, 383]), reads=[Esb], writes=[k.Ftrk], key="Fst")
            for jj in (3, 4):
                off = (4 - jj) * 128 + 127
                P.dma("pool", BM[:, :, jj - 3, :], bass.AP(k.Fd, off, [[382, 128], [128 * 383, 16], [1, 128]]), reads=[k.Ftrk], writes=[BM])
            P.dma("sp", c256[:], k.ca_relb[0:1, :, 256:257].rearrange("o h x -> o (h x)").partition_broadcast(128), writes=[c256],
                  allow_slow_non_contiguous=True)
            P.op("dve", lambda e: e.memset(BM[64:128, :, 1, 0:64], -30000.0), reads=[BM], writes=[BM])
            P.op("pool", lambda e: e.memset(mask0[:], 1.0), writes=[mask0])
            P.op("pool", lambda e: e.memset(mask0[0:64, 64:128], 0.0), reads=[mask0], writes=[mask0])
            for a_ in range(2):
                P.op("pool", lambda e, a_=a_: e.memset(vaug[a_][:], 1.0), writes=[vaug[a_]])

        cnt = [0, 0, 0, 0]
        k.psT_trk = [Trk("psTa"), Trk("psTb")]
        for b in range(NSEQ):
            load_mod(P, k, c, layer, 0, b)
            P.op("act", lambda e: e.copy(out=c.junk[:, 0:8], in_=c.junk[:, 8:16]), reads=[k.psT_trk[0], k.psT_trk[1]], writes=[c.psT, c.junk])
            for t in range(TPS):
                gt = b * TPS + t
                ap, trk = xsrc(k, first, gt)
                prologue_tile(P, k, c, ap, trk, hT, t * 128)
            P.op("act", lambda e: e.copy(out=c.junk[:, 0:8], in_=c.psT[:, 0:8]), reads=[c.psT], writes=[c.junk, k.psT_trk[0], k.psT_trk[1]])
            pendB = []
            for n in range(6):
                w = wb[cnt[0] % 2]
                cnt[0] += 1
                P.dma("pool", w[:], wqv[:, :, n * 512:(n + 1) * 512], writes=[w])
                for t in range(TPS):
                    ps = psP[cnt[1] % 2]
                    cnt[1] += 1
                    for kk in range(8):
                        mm(P, ps[:], hT[:, kk, t * 128:(t + 1) * 128], w[:, kk, :], kk == 0, kk == 7, [hT, w], [ps])
                    if n < 4:
                        gain = gq if n < 2 else gk
                        dstT = qT if n < 2 else kT
                        bi = cnt[1] % 2
                        sq, tmp, qn, ssqh, rsth = sq_[bi], tmp_[bi], qn_[bi], ssqh_[bi], rsth_[bi]
                        pso = bi * 512
                        P.op("act", lambda e, ps=ps, sq=sq: e.activation(out=sq[:], in_=ps[:], func=AF.Square), reads=[ps], writes=[sq])
                        P.op("dve", lambda e, sq=sq, ssqh=ssqh: e.tensor_reduce(out=ssqh[:], in_=sq[:].rearrange("p (h d) -> p h d", h=8), axis=AX.X, op=ALU.add),
                             reads=[sq], writes=[ssqh])
                        P.op("act", lambda e, ssqh=ssqh, rsth=rsth: e.activation(out=rsth[:], in_=ssqh[:], func=AF.Sqrt, scale=1.0 / 64, bias=k.epsb[:, 0:1]),
                             reads=[ssqh, k.epsb], writes=[rsth])
                        P.op("dve", lambda e, rsth=rsth: e.reciprocal(out=rsth[:], in_=rsth[:]), reads=[rsth], writes=[rsth])
                        P.op("dve", lambda e, ps=ps, tmp=tmp, rsth=rsth: e.tensor_tensor(out=tmp[:].rearrange("p (h d) -> p h d", h=8),
                                                                                      in0=ps[:].rearrange("p (h d) -> p h d", h=8),
                                                                                      in1=rsth[:, :].unsqueeze(2).to_broadcast([128, 8, 64]), op=ALU.mult),
                             reads=[ps, rsth], writes=[tmp])
                        P.op("pool", lambda e, gain=gain, qn=qn, tmp=tmp: e.tensor_tensor(out=qn[:], in0=tmp[:], in1=gain[:], op=ALU.mult),
                             reads=[tmp, gain], writes=[qn])
                        def stageB(qn=qn, pso=pso, bi=bi, dstT=dstT, t=t, n=n):
                            for pp in range(4):
                                P.op("pe", lambda e, pp=pp: e.transpose(out=c.psT[:, pso + pp * 128:pso + (pp + 1) * 128], in_=qn[:, pp * 128:(pp + 1) * 128],
                                                                        identity=c.ident[:]), reads=[qn, c.ident], writes=[k.psT_trk[bi]])
                            p0 = (n % 2) * 4
                            P.op("act", lambda e: e.copy(out=dstT[:, p0:p0 + 4, t * 128:(t + 1) * 128],
                                                         in_=c.psT[:, pso:pso + 512].rearrange("p (k t) -> p k t", k=4)),
                                 reads=[k.psT_trk[bi]], writes=[dstT])
                        if pendB:
                            pendB.pop(0)()
                        pendB.append(stageB)
                    else:
                        P.op("act", lambda e, ps=ps, t=t, n=n: e.copy(out=v_sb[:, t, (n - 4) * 512:(n - 3) * 512], in_=ps[:]),
                             reads=[ps], writes=[v_sb])
            while pendB:
                pendB.pop(0)()
            if sbk:
                units = [(h, Q, ci, cc) for h in range(16) for Q in range(4) for ci, cc in enumerate(range(4 * Q + 3, -1, -1))]

                def geom(i):
                    h, Q, ci, cc = units[i]
                    d = cc - 4 * Q
                    c_lo = 128 * d if d > 0 else 0
                    return h, Q, ci, cc, d, slice(c_lo, 512), slice(Q * 512 + c_lo, (Q + 1) * 512), h // 2, (h % 2) * 64

                def stage1(i):
                    h, Q, ci, cc, d, cs, qs, p, base = geom(i)
                    pz, es, sp = psZ[i % 4], esb[i % 2], spb[i % 2]
                    mm(P, pz[:, cs], kT[base:base + 64, p, cc * 128:(cc + 1) * 128], qT[base:base + 64, p, qs], True, False, [kT, qT], [pz])
                    P.op("act", lambda e: e.activation(out=es[:, cs], in_=pz[:, cs], func=AF.Exp), reads=[pz], writes=[es])
                    P.op("act", lambda e: e.activation(out=sp[:, cs], in_=es[:, cs], func=AF.Ln, bias=1.0), reads=[es], writes=[sp], noself=True)
                    if d >= 0:
                        P.op("dve", lambda e: e.tensor_tensor(out=sp[:, cs], in0=sp[:, cs], in1=masks[:, d, cs], op=ALU.mult),
                             reads=[sp, masks], writes=[sp])

                def stage2(i):
                    h, Q, ci, cc, d, cs, qs, p, base = geom(i)
                    paf, A, sp, ef = psZ[i % 4], Ab[i % 3], spb[i % 2], efb[i % 2]
                    mm(P, paf[:, cs], negtri[:], sp[:, cs], False, ci == 0, [negtri, sp], [paf])
                    Lo, Ln_ = Lbb[(ci + 1) % 2], Lbb[ci % 2]
                    if ci > 0:
                        mm(P, paf[:, cs], negones[:], Lo[:, cs], False, True, [negones, Lo], [paf])
                        P.op("pool", lambda e: e.tensor_tensor(out=Ln_[:, cs], in0=Lo[:, cs], in1=sp[:, cs], op=ALU.add), reads=[Lo, sp], writes=[Ln_])
                    else:
                        P.op("pool", lambda e: e.memset(Lbb[0][:], 0.0), writes=[Lbb[0]])
                        P.op("pool", lambda e: e.memset(Lbb[1][:], 0.0), writes=[Lbb[1]])
                        P.op("pool", lambda e: e.tensor_copy(out=Ln_[:, cs], in_=sp[:, cs]), reads=[sp, Ln_], writes=[Ln_])
                    if d >= 0:
                        P.op("act", lambda e: e.activation(out=ef[:, cs], in_=paf[:, cs], func=AF.Exp), reads=[paf], writes=[ef])
                        P.op("dve", lambda e: e.tensor_tensor(out=A[:, cs], in0=ef[:, cs], in1=masks[:, d, cs], op=ALU.mult), reads=[ef, masks], writes=[A])
                    else:
                        P.op("act", lambda e: e.activation(out=A[:], in_=paf[:], func=AF.Exp), reads=[paf], writes=[A])

                def stage3(i):
                    h, Q, ci, cc, d, cs, qs, p, base = geom(i)
                    A = Ab[i % 3]
                    kwo = dict(tile_position=(0, 64)) if base == 64 else {}
                    if ci == 0:
                        mm(P, psOT[base:base + 64, :], zer[:], masks[:, 0, :], True, False, [zer, masks], [psOT], **kwo)
                    mm(P, psOT[base:base + 64, cs], v_sb[:, cc, h * 64:(h + 1) * 64], A[:, cs], False, cc == 0, [v_sb, A], [psOT], **kwo)
                    if cc == 0:
                        P.op("act", lambda e: e.copy(out=oT[base:base + 64, p, Q * 512:(Q + 1) * 512], in_=psOT[base:base + 64, :]),
                             reads=[psOT], writes=[oT])

                n_u = len(units)
                stage1(0)
                for i in range(n_u):
                    if i + 1 < n_u:
                        stage1(i + 1)
                    stage2(i)
                    if i >= 1:
                        stage3(i - 1)
                stage3(n_u - 1)
            else:
                unitsc = [(h, t, ji, jj, len([x for x in range(5) if t - 4 + x >= 0]))
                          for h in range(16) for t in range(TPS) for ji, jj in enumerate([x for x in range(5) if t - 4 + x >= 0])]

                def cstage1(i):
                    h, t, ji, jj, nj = unitsc[i]
                    p, base = h // 2, (h % 2) * 64
                    kt = t - 4 + jj
                    pz, A = psZ[i % 3], Ab[i % 4]
                    if t == 0 and ji == 0:
                        va = vaug[h % 2]
                        P.op("pool", lambda e: e.tensor_copy(out=va[:, :, base:base + 64], in_=v_sb[:, :, h * 64:(h + 1) * 64]), reads=[v_sb, va], writes=[va])
                    mm(P, pz[:], kT[base:base + 64, p, kt * 128:(kt + 1) * 128], qT[base:base + 64, p, t * 128:(t + 1) * 128], True, True, [kT, qT], [pz])
                    if jj >= 3:
                        sb_ = ssb[i % 2]
                        P.op("dve", lambda e: e.tensor_tensor(out=sb_[:], in0=pz[:], in1=BM[:, h, jj - 3, :], op=ALU.add), reads=[pz, BM], writes=[sb_])
                        P.op("act", lambda e: e.activation(out=A[:], in_=sb_[:], func=AF.Exp), reads=[sb_], writes=[A])
                    elif jj == 0:
                        sb_ = ssb[i % 2]
                        P.op("act", lambda e: e.activation(out=sb_[:], in_=pz[:], func=AF.Exp, bias=c256[:, h:h + 1]), reads=[pz, c256], writes=[sb_])
                        P.op("dve", lambda e: e.tensor_tensor(out=A[:], in0=sb_[:], in1=mask0[:], op=ALU.mult), reads=[sb_, mask0], writes=[A])
                    else:
                        P.op("act", lambda e: e.activation(out=A[:], in_=pz[:], func=AF.Exp, bias=c256[:, h:h + 1]), reads=[pz, c256], writes=[A])

                def cstage2(i):
                    h, t, ji, jj, nj = unitsc[i]
                    p, base = h // 2, (h % 2) * 64
                    kt = t - 4 + jj
                    A = Ab[i % 4]
                    po = psOT[(h * TPS + t) % 2]
                    va = vaug[h % 2]
                    mm(P, po[:], va[:, kt, :], A[:], ji == 0, ji == nj - 1, [va, A], [po])
                    if ji == nj - 1:
                        ob = 64 - base
                        rc = rec[(h * TPS + t) % 2]
                        P.op("dve", lambda e: e.reciprocal(out=rc[ob:ob + 64, :], in_=po[ob:ob + 64, :]), reads=[po], writes=[rc])
                        P.op("dve", lambda e: e.tensor_copy(out=rc[base:base + 64, :], in_=rc[ob:ob + 64, :]), reads=[rc], writes=[rc])
                        P.op("dve", lambda e: e.tensor_tensor(out=oT[base:base + 64, p, t * 128:(t + 1) * 128], in0=po[base:base + 64, :],
                                                              in1=rc[base:base + 64, :], op=ALU.mult), reads=[po, rc], writes=[oT])

                n_c = len(unitsc)
                cstage1(0)
                cstage1(1)
                for i in range(n_c):
                    if i + 2 < n_c:
                        cstage1(i + 2)
                    cstage2(i)
            for half in range(2):
                P.dma("pool", wb[half][:], wov[:, :, half * 512:(half + 1) * 512], writes=[wb[half]])
            for t in range(TPS):
                gt = b * TPS + t
                ap, trk = xsrc(k, first, gt)
                xt = c.xt[c.cnt % 2]
                c.cnt += 1
                P.dma("sp", xt[:], ap, reads=[trk], writes=[xt])
                for half in range(2):
                    ps = psP[cnt[1] % 2]
                    cnt[1] += 1
                    for kk in range(8):
                        mm(P, ps[:], oT[:, kk, t * 128:(t + 1) * 128], wb[half][:, kk, :], kk == 0, kk == 7, [oT, wb[half]], [ps])
                    epilogue_half(P, k, c, xt, ps[:], ps, half)
                store_x(P, k, xt, gt)
        P.barrier_ops()
        P.flush()


GELU_FUNC = AF.Gelu_apprx_tanh


def phase_sg(P, k, layer):
    win = k.sg_win[0].rearrange("(k p) n -> p k n", p=128)
    wout = k.sg_wout[0].rearrange("(k p) n -> p k n", p=128)
    H3 = 3 * D
    with ExitStack() as st:
        c = alloc_common(P, st, k, hb2=True)
        hT = P.sbuf(st, "hT", [128, 8, 512], BF16)
        zb = P.sbuf(st, "zb", [128, 4, 2 * H3], BF16)
        yT = P.sbuf(st, "yT", [128, 24, 512], BF16)
        winb = [P.sbuf(st, f"winb{i}", [128, 8, 512], BF16) for i in range(2)]
        woutb = [P.sbuf(st, f"woutb{i}", [128, 24, 512], BF16) for i in range(2)]
        vgb = P.sbuf(st, "vgb", [128, H3], F32)
        vn = P.sbuf(st, "vn", [128, H3], BF16)
        yb = P.sbuf(st, "yb", [128, H3], BF16)
        wsr = P.sbuf(st, "wsr", [128, 8, 128], BF16)
        wsT = P.sbuf(st, "wsT", [128, 8, 128], BF16)
        bs = P.sbuf(st, "bs", [128, 8], F32)
        ssv = P.sbuf(st, "ssv", [128, 4, 8], F32)
        rs = P.sbuf(st, "rs", [128, 2], F32)
        psZ = [P.psum(st, f"psZ{i}", [128, 512], F32) for i in range(3)]
        psM = [P.psum(st, f"psM{i}", [128, 512], F32) for i in range(2)]
        psY = [P.psum(st, f"psY{i}", [128, 512], F32) for i in range(2)]
        P.dma("sp", vgb[:], k.sg_vg[0:1, :].partition_broadcast(128), writes=[vgb])
        P.dma("pool", wsr[:], k.sg_ws[0].rearrange("g i j -> i g j"), writes=[wsr])
        P.dma("sp", bs[:], k.sg_bs[0].rearrange("g i -> i g"), writes=[bs], allow_slow_non_contiguous=True)
        for g in range(8):
            P.op("pe", lambda e, g=g: e.transpose(out=c.psT[:, g * 128:(g + 1) * 128], in_=wsr[:, g, :], identity=c.ident[:]),
                 reads=[wsr, c.ident], writes=[c.psT])
        P.op("act", lambda e: e.copy(out=wsT[:], in_=c.psT[:].rearrange("p (g i) -> p g i", g=8)), reads=[c.psT], writes=[wsT])
        P.op("dve", lambda e: e.memset(wsT[64:128, :, 0:64], 0.0), reads=[wsT], writes=[wsT])
        cnt = [0, 0, 0, 0]
        cur_b = -1
        for blk in range(8):
            b = blk // 4
            if b != cur_b:
                load_mod(P, k, c, layer, 0, b)
                cur_b = b
            pend = []
            for tt in range(4):
                gt = blk * 4 + tt
                ap, trk = xsrc(k, False, gt)
                prologue_tile(P, k, c, ap, trk, hT, tt * 128, pend)
            flush_pend(pend)
            for n in range(12):
                w = winb[cnt[0] % 2]
                cnt[0] += 1
                P.dma("pool", w[:], win[:, :, n * 512:(n + 1) * 512], writes=[w])
                for tt in range(4):
                    ps = psZ[cnt[1] % 3]
                    cnt[1] += 1
                    for kk in range(8):
                        mm(P, ps[:], hT[:, kk, tt * 128:(tt + 1) * 128], w[:, kk, :], kk == 0, kk == 7, [hT, w], [ps])
                    P.op("act", lambda e, ps=ps, tt=tt, n=n: e.activation(out=zb[:, tt, n * 512:(n + 1) * 512], in_=ps[:], func=GELU_FUNC),
                         reads=[ps], writes=[zb])
                    if n >= 6:
                        P.op("act", lambda e, tt=tt, n=n: e.activation(out=c.junk[:, 0:512], in_=zb[:, tt, n * 512:(n + 1) * 512], func=AF.Square,
                                                                      accum_out=ssv[:, tt, n - 6:n - 5]), reads=[zb], writes=[c.junk, ssv], noself=True)
            for tt in range(4):
                col = tt % 2
                P.op("dve", lambda e, tt=tt, col=col: e.tensor_reduce(out=rs[:, col:col + 1], in_=ssv[:, tt, 0:6], axis=AX.X, op=ALU.add),
                     reads=[ssv], writes=[rs])
                P.op("act", lambda e, col=col: e.activation(out=rs[:, col:col + 1], in_=rs[:, col:col + 1], func=AF.Sqrt, scale=1.0 / H3, bias=k.epsb[:, 0:1]),
                     reads=[rs, k.epsb], writes=[rs])
                P.op("dve", lambda e, col=col: e.reciprocal(out=rs[:, col:col + 1], in_=rs[:, col:col + 1]), reads=[rs], writes=[rs])
                P.op("dve", lambda e, tt=tt, col=col: e.scalar_tensor_tensor(out=vn[:], in0=zb[:, tt, H3:2 * H3], scalar=rs[:, col:col + 1], in1=vgb[:],
                                                                            op0=ALU.mult, op1=ALU.mult), reads=[zb, rs, vgb], writes=[vn])
                for g in range(8):
                    pm = psM[cnt[2] % 2]
                    cnt[2] += 1
                    mm(P, pm[:, 0:384], wsT[:, g, :], vn[:, g * 384:(g + 1) * 384], True, True, [wsT, vn], [pm])
                    P.op("dve", lambda e, pm=pm, g=g, tt=tt: e.scalar_tensor_tensor(out=yb[:, g * 384:(g + 1) * 384], in0=pm[:, 0:384], scalar=bs[:, g:g + 1],
                                                                                  in1=zb[:, tt, g * 384:(g + 1) * 384], op0=ALU.add, op1=ALU.mult),
                         reads=[pm, bs, zb], writes=[yb])
                for k0 in range(0, 24, 8):
                    for kk in range(8):
                        P.op("pe", lambda e, kk=kk, k0=k0: e.transpose(out=c.psT[:, kk * 128:(kk + 1) * 128], in_=yb[:, (k0 + kk) * 128:(k0 + kk + 1) * 128],
                                                                      identity=c.ident[:]), reads=[yb, c.ident], writes=[c.psT])
                    P.op("act", lambda e, k0=k0, tt=tt: e.copy(out=yT[:, k0:k0 + 8, tt * 128:(tt + 1) * 128], in_=c.psT[:].rearrange("p (k t) -> p k t", k=8)),
                         reads=[c.psT], writes=[yT])
            for half in range(2):
                P.dma("pool", woutb[half][:], wout[:, :, half * 512:(half + 1) * 512], writes=[woutb[half]])
            for tt in range(4):
                gt = blk * 4 + tt
                ap, trk = xsrc(k, False, gt)
                xt = c.xt[c.cnt % 2]
                c.cnt += 1
                P.dma("sp", xt[:], ap, reads=[trk], writes=[xt])
                for half in range(2):
                    ps = psY[cnt[3] % 2]
                    cnt[3] += 1
                    for kk in range(24):
                        mm(P, ps[:], yT[:, kk, tt * 128:(tt + 1) * 128], woutb[half][:, kk, :], kk == 0, kk == 23, [yT, woutb[half]], [ps])
                    epilogue_half(P, k, c, xt, ps[:], ps, half)
                store_x(P, k, xt, gt)
        P.barrier_ops()
        P.flush()


PHASES_ALL = ["mod", "a0", "f0", "a1", "r1", "f1", "a2", "f2", "a3", "r3", "f3"]


def build_program(phases=None):
    phases = PHASES_ALL if phases is None else phases
    nc = bass.Bass("TRN2", target_bir_lowering=False)
    k = K()
    T_ = D * 0 + NSEQ * SEQ

    def inp(name, shape):
        return nc.dram_tensor(name, list(shape), F32, kind="ExternalInput")

    k.x_in = inp("x", [T_, D])
    k.c = inp("c", [NSEQ, D])
    k.norm_g = inp("norm_g", [4, 2, D])
    k.ada_w = inp("ada_w", [4, D, 6 * D])
    k.ada_b = inp("ada_b", [4, 6 * D])
    k.sb_wqkv = inp("sb_wqkv", [2, D, 3 * D])
    k.sb_qg = inp("sb_qg", [2, 64])
    k.sb_kg = inp("sb_kg", [2, 64])
    k.sb_wo = inp("sb_wo", [2, D, D])
    k.sg_win = inp("sg_win", [1, D, 6 * D])
    k.sg_vg = inp("sg_vg", [1, 3 * D])
    k.sg_ws = inp("sg_ws", [1, 8, 128, 128])
    k.sg_bs = inp("sg_bs", [1, 8, 128])
    k.sg_wout = inp("sg_wout", [1, 3 * D, D])
    k.ca_wqkv = inp("ca_wqkv", [1, D, 3 * D])
    k.ca_qg = inp("ca_qg", [1, 64])
    k.ca_kg = inp("ca_kg", [1, 64])
    k.ca_relb = inp("ca_relb", [1, 16, 257])
    k.ca_wo = inp("ca_wo", [1, D, D])
    k.ff_w13 = inp("ff_w13", [2, D, 2 * DFF])
    k.ff_w2 = inp("ff_w2", [2, DFF, D])
    k.moe_wr = inp("moe_wrT", [2, NE, D])
    k.moe_br = inp("moe_br", [2, NE])
    k.moe_w1 = inp("moe_w1", [2, NE, D, DFE])
    k.moe_w3 = inp("moe_w3", [2, NE, D, DFE])
    k.moe_w2 = inp("moe_w2", [2, NE, DFE, D])
    k.y = nc.dram_tensor("y", [T_, D], F32, kind="ExternalOutput")
    k.modd = nc.dram_tensor("modd", [4, NSEQ, 6 * D], F32)
    k.gates_d = nc.dram_tensor("gates_d", [T_, NE], F32)
    k.Fd = nc.dram_tensor("Fd", [16, 128, 383], F32)
    k.modtrk = Trk("modtrk")
    k.gates_trk = Trk("gates_trk")
    k.Ftrk = Trk("Ftrk")
    k.xin_trk = Trk("xin")
    k.ytrk = [Trk(f"y{t}") for t in range(NSEQ * TPS)]
    with ExitStack() as st:
        P = Prog(nc, st)
        for ph in phases:
            if ph == "mod":
                phase_mod(P, k)
            elif ph == "cp":
                for gt in range(NSEQ * TPS):
                    P.dma("sp", k.y[gt * 128:(gt + 1) * 128, :], k.x_in[gt * 128:(gt + 1) * 128, :], writes=[k.ytrk[gt]], key=f"yst{gt % 4}")
                P.barrier_ops()
                P.flush()
            elif ph[0] == "a":
                layer = int(ph[1])
                kind = ["sb", "sg", "ca"][layer % 3]
                if kind == "sg":
                    phase_sg(P, k, layer)
                else:
                    phase_attn(P, k, layer, kind)
            elif ph[0] == "r":
                phase_router(P, k, int(ph[1]))
            elif ph[0] == "f":
                layer = int(ph[1])
                phase_ffn(P, k, layer, layer % 2 == 1)
    return nc


INPUT_NAMES = ["x", "c", "norm_g", "ada_w", "ada_b", "sb_wqkv", "sb_qg", "sb_kg", "sb_wo", "sg_win", "sg_vg", "sg_ws", "sg_bs",
               "sg_wout", "ca_wqkv", "ca_qg", "ca_kg", "ca_relb", "ca_wo", "ff_w13", "ff_w2", "moe_wr", "moe_br",
               "moe_w1", "moe_w3", "moe_w2"]


def make_in_maps(inputs, n_cores=8):
    f = lambda a: np.ascontiguousarray(np.asarray(a, dtype=np.float32))
    shared = {}
    for name in INPUT_NAMES:
        if name in ("x", "c"):
            continue
        if name == "moe_wr":
            shared["moe_wrT"] = f(np.asarray(inputs[name]).transpose(0, 2, 1))
        else:
            shared[name] = f(inputs[name])
    x = np.asarray(inputs["x"], dtype=np.float32)
    c = np.asarray(inputs["c"], dtype=np.float32)
    maps = []
    for i in range(n_cores):
        m = dict(shared)
        m["x"] = f(x[NSEQ * i:NSEQ * (i + 1)].reshape(NSEQ * SEQ, D))
        m["c"] = f(c[NSEQ * i:NSEQ * (i + 1)])
        maps.append(m)
    return maps


def kernel(**inputs):
    nc = build_program()
    maps = make_in_maps(inputs, 8)
    res = run_bass_kernel_spmd(nc, maps, core_ids=list(range(8)))
    out = np.stack([np.asarray(r["y"], dtype=np.float32).reshape(NSEQ, SEQ, D) for r in res.results], axis=0)
    return out.reshape(8 * NSEQ, SEQ, D)
```
